# Optimizing a Trainium2 kernel written in Bass

```python
import jax, jax.numpy as jnp
from jax import lax
import numpy as np

D_MODEL = 1024
BATCH = 4
SEQ = 4096
DEPTH = 4

MIX_WIDTH = D_MODEL
N_GROUPS = 4
GROUP_WIDTH = MIX_WIDTH // N_GROUPS
EPS = 1e-6
ROPE_THETA = 10000.0

MLA_HEADS = 4
MLA_NOPE = 64
MLA_ROPE = 32
MLA_V = GROUP_WIDTH // MLA_HEADS
MLA_Q_LORA = 256
MLA_KV_LORA = 128
ATTN_BLOCK = 128

SG_HEADS = 4
SG_CHUNK = 128
SG_HALF = GROUP_WIDTH

HG_HEADS = 4
HG_KEY = 128
HG_VAL = GROUP_WIDTH // HG_HEADS
HG_CHUNK = 64

RET_HEADS = 4
RET_QK = 64
RET_V = GROUP_WIDTH // RET_HEADS
RET_CHUNK = 128

D_FF = 2816
PLE_DIM = 256

IN_SPLITS = (MLA_Q_LORA, MLA_KV_LORA, MLA_ROPE,
             SG_HALF, SG_HALF,
             HG_HEADS * HG_KEY, HG_HEADS * HG_KEY, HG_HEADS * HG_VAL, HG_HEADS * HG_VAL,
             RET_HEADS * RET_QK, RET_HEADS * RET_QK, RET_HEADS * RET_V, RET_HEADS * RET_V)
IN_WIDTH = sum(IN_SPLITS)

kernel_name = 'hybrid_parallel_mla_sgu_hgrn2_retention_macaron'


def rmsnorm(x, g):
    xf = x.astype(jnp.float32)
    y = xf * lax.rsqrt(jnp.mean(xf * xf, axis=-1, keepdims=True) + EPS)
    return (y * g.astype(jnp.float32)).astype(x.dtype)


def layernorm(x, g):
    xf = x.astype(jnp.float32)
    xc = xf - jnp.mean(xf, axis=-1, keepdims=True)
    y = xc * lax.rsqrt(jnp.mean(xc * xc, axis=-1, keepdims=True) + EPS)
    return (y * g.astype(jnp.float32)).astype(x.dtype)


def swiglu(x, w_gate, w_up, w_down):
    return (jax.nn.silu(x @ w_gate) * (x @ w_up)) @ w_down


def rope(x, positions):
    d = x.shape[-1]
    inv = ROPE_THETA ** (-jnp.arange(0, d, 2, dtype=jnp.float32) / d)
    ang = positions.astype(jnp.float32)[..., None] * inv
    cos = jnp.cos(ang)[:, :, None, :]
    sin = jnp.sin(ang)[:, :, None, :]
    x1, x2 = jnp.split(x.astype(jnp.float32), 2, axis=-1)
    return jnp.concatenate([x1 * cos - x2 * sin, x1 * sin + x2 * cos], axis=-1).astype(x.dtype)


def mla_mixer(c_q, c_kv, k_pe, positions, q_norm, w_uq, kv_norm, w_ukv):
    B, S, _ = c_q.shape
    q = (rmsnorm(c_q, q_norm) @ w_uq).reshape(B, S, MLA_HEADS, MLA_NOPE + MLA_ROPE)
    q = jnp.concatenate([q[..., :MLA_NOPE], rope(q[..., MLA_NOPE:], positions)], axis=-1)
    kv = (rmsnorm(c_kv, kv_norm) @ w_ukv).reshape(B, S, MLA_HEADS, MLA_NOPE + MLA_V)
    k_rot = rope(k_pe[:, :, None, :], positions)
    k = jnp.concatenate([kv[..., :MLA_NOPE],
                         jnp.broadcast_to(k_rot, (B, S, MLA_HEADS, MLA_ROPE))], axis=-1)
    v = kv[..., MLA_NOPE:]
    scale = (MLA_NOPE + MLA_ROPE) ** -0.5
    nb = S // ATTN_BLOCK
    qb = (q * scale).reshape(B, nb, ATTN_BLOCK, MLA_HEADS, MLA_NOPE + MLA_ROPE).transpose(1, 0, 3, 2, 4)
    kh = k.transpose(0, 2, 1, 3)
    vh = v.transpose(0, 2, 1, 3)
    key_idx = jnp.arange(S)

    def query_block(args):
        q_blk, bi = args
        s = jnp.einsum('bhqd,bhkd->bhqk', q_blk, kh).astype(jnp.float32)
        q_idx = bi * ATTN_BLOCK + jnp.arange(ATTN_BLOCK)
        s = jnp.where(key_idx[None, :] <= q_idx[:, None], s, -jnp.inf)
        pr = jax.nn.softmax(s, axis=-1).astype(vh.dtype)
        return jnp.einsum('bhqk,bhkd->bhqd', pr, vh)

    o = lax.map(query_block, (qb, jnp.arange(nb)))
    return o.transpose(1, 0, 3, 2, 4).reshape(B, S, MLA_HEADS * MLA_V)


def spatial_gating_mixer(z_u, z_v, ln_g, w_s, b_s):
    B, S, _ = z_u.shape
    u = jax.nn.gelu(z_u, approximate=False)
    v = layernorm(jax.nn.gelu(z_v, approximate=False), ln_g)
    n = S // SG_CHUNK
    vh = v.reshape(B, n, SG_CHUNK, SG_HEADS, SG_HALF // SG_HEADS)
    causal = jnp.tril(jnp.ones((SG_CHUNK, SG_CHUNK), dtype=bool))
    w = jnp.where(causal[None], w_s, 0.0).astype(v.dtype)
    mixed = jnp.einsum('hts,bnshd->bnthd', w, vh) + b_s.T[None, None, :, :, None]
    return u * mixed.reshape(B, S, SG_HALF)


def hgrn2_mixer(q, f, i, g, lb, norm_g):
    B, S, _ = q.shape
    f32 = jnp.float32
    forget = lb + (1.0 - lb) * jax.nn.sigmoid(f.astype(f32))
    log_f = jnp.log(forget)
    k = 1.0 - forget
    n = S // HG_CHUNK

    def chunks(t, d):
        return t.astype(f32).reshape(B, n, HG_CHUNK, HG_HEADS, d).transpose(1, 0, 3, 2, 4)

    qc, kc, gc = chunks(q, HG_KEY), chunks(k, HG_KEY), chunks(log_f, HG_KEY)
    vc = chunks(i, HG_VAL)
    causal = jnp.tril(jnp.ones((HG_CHUNK, HG_CHUNK), dtype=bool))[None, None, :, :, None]

    def step(state, inp):
        qt, kt, vt, gt = inp
        b = jnp.cumsum(gt, axis=2)
        diff = jnp.where(causal, b[:, :, :, None, :] - b[:, :, None, :, :], -jnp.inf)
        scores = jnp.einsum('bhtk,bhtsk,bhsk->bhts', qt, jnp.exp(diff), kt)
        o = (jnp.einsum('bhts,bhsv->bhtv', scores, vt)
             + jnp.einsum('bhtk,bhkv->bhtv', qt * jnp.exp(b), state))
        b_last = b[:, :, -1:, :]
        new_state = (jnp.exp(b_last[:, :, 0, :])[..., None] * state
                     + jnp.einsum('bhsk,bhsv->bhkv', kt * jnp.exp(b_last - b), vt))
        return new_state, o

    state0 = jnp.zeros((B, HG_HEADS, HG_KEY, HG_VAL), f32)
    _, o = lax.scan(step, state0, (qc, kc, vc, gc))
    o = o.transpose(1, 0, 3, 2, 4).reshape(B, S, HG_HEADS, HG_VAL)
    o = o * lax.rsqrt(jnp.mean(o * o, axis=-1, keepdims=True) + EPS) * norm_g.astype(f32).reshape(HG_HEADS, HG_VAL)
    gate = jax.nn.silu(g.astype(f32)).reshape(B, S, HG_HEADS, HG_VAL)
    return (o * gate).reshape(B, S, HG_HEADS * HG_VAL).astype(q.dtype)


def retention_mixer(q, k, v, g, positions, norm_g):
    B, S, _ = q.shape
    f32 = jnp.float32
    H, C = RET_HEADS, RET_CHUNK
    n = S // C
    qr = rope(q.reshape(B, S, H, RET_QK), positions).astype(f32)
    kr = rope(k.reshape(B, S, H, RET_QK), positions).astype(f32) * (RET_QK ** -0.5)
    vr = v.reshape(B, S, H, RET_V).astype(f32)

    def chunks(t):
        return t.reshape(B, n, C, H, t.shape[-1]).transpose(0, 3, 1, 2, 4)

    qh, kh, vh = chunks(qr), chunks(kr), chunks(vr)
    lg = jnp.log1p(-jnp.power(2.0, -(5.0 + jnp.arange(H, dtype=f32))))
    pos = jnp.arange(C, dtype=f32)
    rel = pos[:, None] - pos[None, :]
    decay = jnp.where(rel >= 0, jnp.exp(jnp.maximum(rel, 0.0)[None] * lg[:, None, None]), 0.0)
    scores = jnp.einsum('bhntd,bhnsd->bhnts', qh, kh) * decay[None, :, None]
    o = jnp.einsum('bhnts,bhnse->bhnte', scores, vh)
    k_dec = kh * jnp.exp((C - 1.0 - pos)[None, :] * lg[:, None])[None, :, None, :, None]
    kv = jnp.einsum('bhnsd,bhnse->bhnde', k_dec, vh)
    cidx = jnp.arange(n, dtype=f32)
    gap = cidx[:, None] - 1.0 - cidx[None, :]
    cross = jnp.where(gap >= 0, jnp.exp(C * jnp.maximum(gap, 0.0)[None] * lg[:, None, None]), 0.0)
    state = jnp.einsum('hnm,bhmde->bhnde', cross, kv)
    q_dec = qh * jnp.exp((pos + 1.0)[None, :] * lg[:, None])[None, :, None, :, None]
    o = o + jnp.einsum('bhntd,bhnde->bhnte', q_dec, state)
    o = o.transpose(0, 2, 3, 1, 4).reshape(B, S, H, RET_V)
    oc = o - jnp.mean(o, axis=-1, keepdims=True)
    o = oc * lax.rsqrt(jnp.mean(oc * oc, axis=-1, keepdims=True) + EPS) * norm_g.astype(f32).reshape(H, RET_V)
    gate = jax.nn.silu(g.astype(f32)).reshape(B, S, H, RET_V)
    return (o * gate).reshape(B, S, H * RET_V).astype(q.dtype)


def setup_inputs(seed: int = 0) -> dict:
    key = jax.random.key(seed)
    ks = iter(jax.random.split(key, 40))
    f32 = jnp.float32

    def nrm(shape, scale):
        return jax.random.normal(next(ks), shape, f32) * scale

    def gain(shape):
        return 1.0 + 0.01 * jax.random.normal(next(ks), shape, f32)

    L, D = DEPTH, D_MODEL
    return {
        'x': nrm((BATCH, SEQ, D), 1.0),
        'p': nrm((DEPTH, BATCH, SEQ, PLE_DIM), 1.0),
        'positions': jnp.broadcast_to(jnp.arange(SEQ, dtype=jnp.int32), (BATCH, SEQ)),
        'ffn1_norm': gain((L, D)),
        'ffn1_w_gate': nrm((L, D, D_FF), D ** -0.5),
        'ffn1_w_up': nrm((L, D, D_FF), D ** -0.5),
        'ffn1_w_down': nrm((L, D_FF, D), D_FF ** -0.5),
        'mix_norm': gain((L, D)),
        'w_in': nrm((L, D, IN_WIDTH), D ** -0.5),
        'mla_q_norm': gain((L, MLA_Q_LORA)),
        'mla_w_uq': nrm((L, MLA_Q_LORA, MLA_HEADS * (MLA_NOPE + MLA_ROPE)), MLA_Q_LORA ** -0.5),
        'mla_kv_norm': gain((L, MLA_KV_LORA)),
        'mla_w_ukv': nrm((L, MLA_KV_LORA, MLA_HEADS * (MLA_NOPE + MLA_V)), MLA_KV_LORA ** -0.5),
        'sg_ln': gain((L, SG_HALF)),
        'sg_w_s': nrm((L, SG_HEADS, SG_CHUNK, SG_CHUNK), 0.5 * SG_CHUNK ** -0.5),
        'sg_b_s': 1.0 + nrm((L, SG_HEADS, SG_CHUNK), 0.1),
        'hg_lb_logits': nrm((L, HG_HEADS * HG_KEY), 1.0),
        'hg_norm': gain((L, HG_HEADS * HG_VAL)),
        'ret_norm': gain((L, RET_HEADS * RET_V)),
        'w_out': nrm((L, MIX_WIDTH, D), MIX_WIDTH ** -0.5),
        'ffn2_norm': gain((L, D)),
        'ffn2_w_gate': nrm((L, D, D_FF), D ** -0.5),
        'ffn2_w_up': nrm((L, D, D_FF), D ** -0.5),
        'ffn2_w_down': nrm((L, D_FF, D), D_FF ** -0.5),
        'ple_norm': gain((L, D)),
        'ple_w_proj': nrm((L, PLE_DIM, D), PLE_DIM ** -0.5),
        'ple_w_gate': nrm((L, D, D), D ** -0.5),
        'final_norm': gain((D,)),
    }


def reference(x, p, positions, ffn1_norm, ffn1_w_gate, ffn1_w_up, ffn1_w_down,
              mix_norm, w_in, mla_q_norm, mla_w_uq, mla_kv_norm, mla_w_ukv,
              sg_ln, sg_w_s, sg_b_s, hg_lb_logits, hg_norm, ret_norm, w_out,
              ffn2_norm, ffn2_w_gate, ffn2_w_up, ffn2_w_down,
              ple_norm, ple_w_proj, ple_w_gate, final_norm):
    split_points = np.cumsum(IN_SPLITS)[:-1].tolist()
    lb_all = jnp.cumsum(jax.nn.softmax(hg_lb_logits.astype(jnp.float32), axis=0), axis=0)
    lb_all = lb_all - lb_all[0:1]
    h = x
    for l in range(DEPTH):
        h = h + 0.5 * swiglu(rmsnorm(h, ffn1_norm[l]), ffn1_w_gate[l], ffn1_w_up[l], ffn1_w_down[l])
        z = rmsnorm(h, mix_norm[l]) @ w_in[l]
        (c_q, c_kv, k_pe, z_u, z_v, hq, hf, hi, hg,
         rq, rk, rv, rg) = jnp.split(z, split_points, axis=-1)
        y_a = mla_mixer(c_q, c_kv, k_pe, positions, mla_q_norm[l], mla_w_uq[l], mla_kv_norm[l], mla_w_ukv[l])
        y_b = spatial_gating_mixer(z_u, z_v, sg_ln[l], sg_w_s[l], sg_b_s[l])
        y_c = hgrn2_mixer(hq, hf, hi, hg, lb_all[l], hg_norm[l])
        y_d = retention_mixer(rq, rk, rv, rg, positions, ret_norm[l])
        h = h + jnp.concatenate([y_a, y_b, y_c, y_d], axis=-1) @ w_out[l]
        h = h + 0.5 * swiglu(rmsnorm(h, ffn2_norm[l]), ffn2_w_gate[l], ffn2_w_up[l], ffn2_w_down[l])
        gate = jax.nn.sigmoid(rmsnorm(h, ple_norm[l]) @ ple_w_gate[l])
        h = h + (p[l] @ ple_w_proj[l]) * gate
    return rmsnorm(h, final_norm)
```

```python
import numpy as np
import concourse.bass as bass
import concourse.mybir as mybir
from concourse.bass_utils import run_bass_kernel_spmd
from contextlib import ExitStack

F32 = mybir.dt.float32
BF16 = mybir.dt.bfloat16
I32 = mybir.dt.int32
ALU = mybir.AluOpType
AF = mybir.ActivationFunctionType

ENGS = ["tensor", "vector", "scalar", "gpsimd", "sync"]
NDMA_SEM = 8

D = 1024
L = 4
T = 2048
TT = 512
NT = T // TT
NB = T // 128
DFF = 2816
NFC = DFF // 128
EPS = 1e-6
INW = 3488
C_CQ, C_CKV, C_KPE, C_ZU, C_ZV, C_HQ, C_HF, C_HI, C_HG, C_RQ, C_RK, C_RV, C_RG = (
    0, 256, 384, 416, 672, 928, 1440, 1952, 2208, 2464, 2720, 2976, 3232)


class Buf:
    __slots__ = ("name", "lw", "rd")

    def __init__(self, name=""):
        self.name = name
        self.lw = None
        self.rd = {}


class Prog:
    def __init__(self, nc):
        self.nc = nc
        self.es = ExitStack()
        self.ops = {e: [] for e in ENGS}
        self.cnt = {}
        self.sems = {}
        self.mult = {}
        self.waited = {e: {} for e in ENGS}
        self.dma_i = {e: 0 for e in ENGS}
        for e in ENGS:
            if e != "sync":
                self._mksem(e, 1)
        for e in ("sync", "gpsimd", "scalar"):
            for j in range(NDMA_SEM):
                self._mksem(("dma", e, j), 16)

    def _mksem(self, key, mult):
        name = key if isinstance(key, str) else "_".join(str(k) for k in key)
        self.sems[key] = self.es.enter_context(self.nc.semaphore("s_" + name))
        self.cnt[key] = 0
        self.mult[key] = mult

    def sb(self, name, shape, dt):
        return self.es.enter_context(self.nc.sbuf_tensor("sb_" + name, list(shape), dt))

    def ps(self, name, shape, dt=F32):
        return self.es.enter_context(self.nc.psum_tensor(name, list(shape), dt))

    def _deps(self, eng, reads, writes):
        deps = {}

        def add(k, s):
            if deps.get(k, 0) < s:
                deps[k] = s
        for b in reads:
            if b.lw is not None:
                add(*b.lw)
        for b in writes:
            if b.lw is not None:
                add(*b.lw)
            for k, s in b.rd.items():
                add(k, s)
        waits = []
        w = self.waited[eng]
        for k, s in deps.items():
            if k == "tensor" and eng == "tensor":
                continue
            if w.get(k, 0) >= s:
                continue
            w[k] = s
            waits.append((k, s))
        return waits

    def _mark(self, key, seq, reads, writes):
        for b in reads:
            if b.rd.get(key, 0) < seq:
                b.rd[key] = seq
        for b in writes:
            b.lw = (key, seq)
            b.rd = {}

    def op(self, eng, fn, r=(), w=(), inc=True):
        waits = self._deps(eng, r, w)
        seq = self.cnt[eng] + 1
        if inc:
            self.cnt[eng] = seq
        self._mark(eng, seq, r, w)
        self.ops[eng].append((fn, waits, eng if inc else None))

    def dma(self, eng, fn, r=(), w=()):
        i = self.dma_i[eng]
        self.dma_i[eng] = i + 1
        key = ("dma", eng, i % NDMA_SEM)
        waits = self._deps(eng, r, w)
        prev = self.cnt[key]
        if prev > 0 and self.waited[eng].get(key, 0) < prev:
            self.waited[eng][key] = prev
            waits.append((key, prev))
        seq = prev + 1
        self.cnt[key] = seq
        self._mark(key, seq, r, w)
        self.ops[eng].append((fn, waits, key))

    def wait_all(self, eng, bufs):
        waits = self._deps(eng, bufs, ())
        self.ops[eng].append((None, waits, None))

    def emit(self):
        nc = self.nc
        with nc.Block() as block:
            for e in ENGS:
                ops = self.ops[e]

                def body(engine, ops=ops):
                    for fn, waits, inckey in ops:
                        for k, s in waits:
                            engine.wait_ge(self.sems[k], s * self.mult[k])
                        if fn is None:
                            continue
                        inst = fn(engine)
                        if inckey is not None:
                            inst.then_inc(self.sems[inckey], self.mult[inckey])
                getattr(block, e)(body)
        self.es.close()


class Arena:
    def __init__(self, tile_bf16, nbytes):
        self.t = tile_bf16
        self.n = nbytes
        self.regs = []
        self.names = {}

    def at(self, off, size, dt, name=""):
        assert off % 4 == 0 and size % 4 == 0 and off + size <= self.n, (name, off, size, self.n)
        b = Buf(name)
        self.names[name] = (off, size, dt)
        keep = []
        for (o, s, ob) in self.regs:
            if o < off + size and off < o + s:
                merge_into(b, ob)
                if not (off <= o and o + s <= off + size):
                    keep.append((o, s, ob))
            else:
                keep.append((o, s, ob))
        self.regs = keep
        self.regs.append((off, size, b))
        ap = self.t[:, off // 2:(off + size) // 2]
        if dt != BF16:
            ap = ap.bitcast(dt)
        return ap, b


class Stack:
    def __init__(self, arena, start, end):
        self.a = arena
        self.start = start
        self.end = end
        self.lo = start
        self.hi = end

    def plo(self, size, dt, name=""):
        size = (size + 31) // 32 * 32
        assert self.lo + size <= self.hi, ("arena overflow lo", name, self.lo, size, self.hi)
        r = self.a.at(self.lo, size, dt, name)
        self.lo += size
        return r

    def phi(self, size, dt, name=""):
        size = (size + 31) // 32 * 32
        assert self.hi - size >= self.lo, ("arena overflow hi", name, self.lo, size, self.hi)
        self.hi -= size
        return self.a.at(self.hi, size, dt, name)

    def phi_off(self, size):
        size = (size + 31) // 32 * 32
        assert self.hi - size >= self.lo, ("arena overflow hi(off)", self.lo, size, self.hi)
        self.hi -= size
        return self.hi

    def reset_hi(self):
        self.hi = self.end

    def reset(self):
        self.lo = self.start
        self.hi = self.end


def merge_into(dst, src):
    for k, v in src.rd.items():
        if dst.rd.get(k, 0) < v:
            dst.rd[k] = v
    if src.lw is not None:
        k, v = src.lw
        if dst.rd.get(k, 0) < v:
            dst.rd[k] = v


def _esize(dt):
    return 2 if dt == BF16 else 4


class Builder:
    def __init__(self, n_layers=L, dbg=None, use_cc=False, stages=3, stop_at=None, halves=(0, 1)):
        self.nl = n_layers
        self.halves = halves
        self.stages = stages
        self.stop_at = stop_at
        self.stopped = False
        self.dbg = dbg or []
        self.use_cc = use_cc
        self.nc = nc = bass.Bass("TRN2", target_bir_lowering=False)
        self.P = Prog(nc)
        self.dram = {}
        self.outs = {}

    def din(self, name, shape, dt=F32):
        t = self.nc.dram_tensor(name, list(shape), dt, kind="ExternalInput").ap()
        self.dram[name] = t
        return t

    def dout(self, name, shape, dt=F32):
        t = self.nc.dram_tensor(name, list(shape), dt, kind="ExternalOutput").ap()
        self.outs[name] = t
        return t

    def V(self, fn, r=(), w=()):
        self.P.op("vector", fn, r, w)

    def A(self, fn, r=(), w=()):
        self.P.op("scalar", fn, r, w)

    def G(self, fn, r=(), w=()):
        self.P.op("gpsimd", fn, r, w)

    def MM(self, out, lhsT, rhs, start, stop, r=(), w=(), inc=True):
        self.P.op("tensor", lambda e: e.matmul(out, lhsT, rhs, start=start, stop=stop), r, w, inc=inc)

    def dump(self, name, ap, buf, shape, dt=F32):
        if name not in self.dbg:
            return
        o = self.dout("dbg_" + name, shape, dt)
        self.P.dma("sync", lambda e: e.dma_start(out=o, in_=ap), r=[buf], w=[self.Bout])

    def build(self):
        nc, P = self.nc, self.P
        nl = self.nl
        xT = self.din("xT", [D, 2 * T])
        pT = self.din("pT", [L, 256, 2 * T])
        pos = self.din("pos", [1, 2 * T], I32)
        pp = self.din("pp", [128, PPW])
        cst = self.din("cst", [128, CSTW])
        W = {}
        for n in ("ffn1_w_gate", "ffn1_w_up", "ffn2_w_gate", "ffn2_w_up"):
            W[n] = self.din(n, [L, D, DFF])
        for n in ("ffn1_w_down", "ffn2_w_down"):
            W[n] = self.din(n, [L, DFF, D])
        W["w_in"] = self.din("w_in", [L, D, INW])
        W["w_in_sw"] = self.din("w_in_sw", [L, D, 544])
        W["w_uq"] = self.din("w_uq", [L, 256, 384])
        W["w_uq_sw"] = self.din("w_uq_sw", [L, 256, 128])
        W["w_ukv"] = self.din("w_ukv", [L, 128, 512])
        W["sg_ln"] = self.din("sg_ln", [L, 256])
        W["sg_wT"] = self.din("sg_wT", [L, 4, 128, 128])
        W["sg_b"] = self.din("sg_b", [L, 4, 128])
        W["w_out"] = self.din("w_out", [L, D, D])
        W["ple_w_proj"] = self.din("ple_w_proj", [L, 256, D])
        W["ple_w_gate"] = self.din("ple_w_gate", [L, D, D])
        self.W = W
        outT = self.dout("outT", [D, 2 * T])
        self.Bout = Buf("out")
        hsp = nc.dram_tensor("hspill", [D, T], F32).ap()
        self.hsp = hsp
        self.Bhsp = [Buf("hspill%d" % i) for i in range(NT)]

        AR_BYTES = 148 * 1024
        self.AR_BYTES = AR_BYTES
        ar_t = P.sb("arena", [128, AR_BYTES // 2], BF16)
        self.arena = Arena(ar_t, AR_BYTES)
        self.H0 = 4 * 2 * T * 2
        self.H_BYTES = 8 * T * 4
        xn_t = P.sb("xn", [128, 8 * T], BF16)
        self.xn = xn_t[:].rearrange("p (c t) -> p c t", c=8)
        self.Bxn = [Buf("xn%d" % i) for i in range(NT)]
        pp_t = P.sb("pp", [128, PPW], F32)
        self.pp = pp_t
        self.Bpp = Buf("pp")
        cst_t = P.sb("cst", [128, CSTW], F32)
        self.cst = cst_t
        self.Bcst = Buf("cst")
        cb_t = P.sb("cstb", [128, CSTBW], BF16)
        self.cb = cb_t
        self.Bcb = Buf("cstb")
        pb_t = P.sb("pbias", [128, 1], F32)
        self.pbias = pb_t
        self.Bpbias = Buf("pbias")
        self.pb = [P.ps("pb%d" % i, [128, 512], F32) for i in range(8)]
        self.Bpb = [Buf("pb%d" % i) for i in range(8)]
        self._rr = 0
        self._cpi = 0

        P.dma("sync", lambda e: e.dma_start(out=pp_t[:], in_=pp), w=[self.Bpp])
        P.dma("sync", lambda e: e.dma_start(out=cst_t[:], in_=cst), w=[self.Bcst])
        self.V(lambda e: e.tensor_copy(out=cb_t[:, 0:CSTBW], in_=cst_t[:, 0:CSTBW]), r=[self.Bcst], w=[self.Bcb])
        self.V(lambda e: e.tensor_scalar(out=pb_t[:, 0:1], in0=pp_t[:, PP_FLAG:PP_FLAG + 1], scalar1=-1.0, scalar2=30000.0,
                                         op0=ALU.add, op1=ALU.mult), r=[self.Bpp], w=[self.Bpbias])

        self.hs = [nc.dram_tensor("hs%d" % i, [D, T], F32).ap() for i in range(2)]
        self.Bhs = [[Buf("hs0_%d" % i) for i in range(NT)], [Buf("hs1_%d" % i) for i in range(NT)]]
        self.tabd = [nc.dram_tensor("tabd%d" % i, [128, 4 * T], BF16).ap() for i in range(2)]
        self.Btabd = [Buf("tabd0"), Buf("tabd1")]
        self.xsd = {}
        self.h_ap, self.hb = self.arena.at(self.H0, self.H_BYTES, F32, "h")
        self.h = self.h_ap.rearrange("p (c t) -> p c t", c=8)
        self.Bh = [Buf("h%d" % i) for i in range(NT)]
        self.stk = Stack(self.arena, self.H0 + self.H_BYTES, AR_BYTES)
        self.pos = pos
        if self.stages >= 2:
            self.alloc_tables()
            for half in range(2):
                self.setup_tables(pos, half)
                P.dma("sync", lambda e, half=half: e.dma_start(out=self.tabd[half][:, :], in_=self.tab_t[:, :]),
                      r=[self.Btab], w=[self.Btabd[half]])
            self.setup_lb()
        for l in range(nl):
            for half in self.halves:
                self.half = half
                c0 = half * T
                self.h_ap, self.hb = self.arena.at(self.H0, self.H_BYTES, F32, "h")
                self.h = self.h_ap.rearrange("p (c t) -> p c t", c=8)
                self.Bh = [Buf("h%d" % i) for i in range(NT)]
                for bb_ in self.Bh:
                    merge_into(bb_, self.hb)
                src = xT[:, c0:c0 + T] if l == 0 else self.hs[half][:, :]
                for tt in range(NT):
                    ts = slice(tt * TT, (tt + 1) * TT)
                    P.dma("sync", lambda e, ts=ts, src=src: e.dma_start(out=self.h[:, :, ts], in_=src[:, ts].rearrange("(c p) t -> p c t", p=128)),
                          r=([] if l == 0 else [self.Bhs[half][tt]]), w=[self.Bh[tt]])
                if self.stages >= 2:
                    P.dma("sync", lambda e, half=half: e.dma_start(out=self.tab_t[:, :], in_=self.tabd[half][:, :]),
                          r=[self.Btabd[half]], w=[self.Btab])
                self.norm(PP_FFN1 + l * 8, 1.0 / D)
                self.ffn(W["ffn1_w_gate"], W["ffn1_w_up"], W["ffn1_w_down"], l)
                if "h_ffn1" in self.dbg and l == 0 and half == 0:
                    o = self.dout("dbg_h_ffn1", [D, T])
                    for ch in range(8):
                        P.dma("sync", lambda e, ch=ch: e.dma_start(out=o[ch * 128:(ch + 1) * 128, :], in_=self.h[:, ch, :]),
                              r=self.Bh, w=[self.Bout])
                if self.stages >= 2:
                    self.mixers(l, half)
                    if self.stopped:
                        break
                    if "h_mix" in self.dbg and l == 0 and half == getattr(self, "dbg_half", 0):
                        o = self.dout("dbg_h_mix", [D, T])
                        for ch in range(8):
                            P.dma("sync", lambda e, ch=ch, o=o: e.dma_start(out=o[ch * 128:(ch + 1) * 128, :], in_=self.h[:, ch, :]),
                                  r=self.Bh, w=[self.Bout])
                if self.stages >= 3:
                    self.norm(PP_FFN2 + l * 8, 1.0 / D)
                    self.ffn(W["ffn2_w_gate"], W["ffn2_w_up"], W["ffn2_w_down"], l)
                    self.ple(l, pT, c0)
                if l == nl - 1:
                    self.final_out(outT, c0)
                else:
                    for tt in range(NT):
                        ts = slice(tt * TT, (tt + 1) * TT)
                        P.dma("sync", lambda e, ts=ts, half=half: e.dma_start(out=self.hs[half][:, ts].rearrange("(c p) t -> p c t", p=128), in_=self.h[:, :, ts]),
                              r=[self.Bh[tt]], w=[self.Bhs[half][tt]])
                    for b in self.Bh:
                        merge_into(self.hb, b)
            if self.stopped:
                break
        P.wait_all("sync", [self.Bout])
        for j in range(NDMA_SEM):
            key = ("dma", "sync", j)
            if P.cnt[key] > 0:
                P.ops["sync"].append((None, [(key, P.cnt[key])], None))
        P.emit()
        return nc

    def norm(self, gcol, inv_n):
        stk = self.stk
        stk.reset_hi()
        ones = self.cb[:, CB_ONES:CB_ONES + 128]
        sq, Bsq = stk.phi(8 * TT * 2, BF16, "sq")
        rs, Brs = stk.phi(TT * 4, F32, "rstd")
        for tt in range(NT):
            ts = slice(tt * TT, (tt + 1) * TT)
            sq3 = sq.rearrange("p (c t) -> p c t", c=8)
            bank = 5 + (tt % 2)
            ps, Bps = self.pb[bank], self.Bpb[bank]
            for ch in range(8):
                if ch % 2 == 0:
                    self.G(lambda e, ch=ch, ts=ts: e.tensor_tensor(out=sq3[:, ch, :], in0=self.h[:, ch, ts],
                                                                  in1=self.h[:, ch, ts], op=ALU.mult),
                           r=[self.Bh[tt]], w=[Bsq])
                else:
                    self.A(lambda e, ch=ch, ts=ts: e.activation(out=sq3[:, ch, :], in_=self.h[:, ch, ts], func=AF.Square),
                           r=[self.Bh[tt]], w=[Bsq])
            for ch in range(8):
                self.MM(ps[:, :], ones, sq3[:, ch, :], ch == 0, ch == 7, r=[Bsq, self.Bcb], w=[Bps], inc=(ch == 7))
            epsb = self.cst[:, CS_EPS:CS_EPS + 1]
            self.A(lambda e, ps=ps: e.activation(out=rs, in_=ps[:, :], func=AF.Ln, bias=epsb, scale=inv_n),
                   r=[Bps, self.Bcst], w=[Brs])
            self.A(lambda e: e.activation(out=rs, in_=rs, func=AF.Exp, scale=-0.5), r=[Brs], w=[Brs])
            for ch in range(8):
                self.V(lambda e, ch=ch, ts=ts: e.scalar_tensor_tensor(
                    out=self.xn[:, ch, ts], in0=self.h[:, ch, ts], scalar=self.pp[:, gcol + ch:gcol + ch + 1],
                    in1=rs, op0=ALU.mult, op1=ALU.mult), r=[self.Bh[tt], Brs, self.Bpp], w=[self.Bxn[tt]])

    def ffn(self, wg, wu, wd, l):
        stk = self.stk
        stk.reset_hi()
        NG = NFC // 2
        NSLOT = 3
        slots = []
        for s in range(NSLOT):
            g_ap, Bg = stk.phi(8 * 256 * 2, BF16, "wg%d" % s)
            u_ap, Bu = stk.phi(8 * 256 * 2, BF16, "wu%d" % s)
            d_ap, Bd = stk.phi(2 * D * 2, BF16, "wd%d" % s)
            slots.append((g_ap.rearrange("p (c f) -> p c f", c=8), Bg, u_ap.rearrange("p (c f) -> p c f", c=8), Bu,
                          d_ap.rearrange("p (c d) -> p c d", c=2), Bd))
        NA = 6
        abuf = [stk.phi(TT * 2, BF16, "A%d" % i) for i in range(NA)]
        sgb = [stk.phi(TT * 2, BF16, "sg%d" % i) for i in range(3)]

        def load(g):
            g3, Bg, u3, Bu, d3, Bd = slots[g % NSLOT]
            c0 = g * 256
            self.P.dma("gpsimd", lambda e: e.dma_start(
                out=g3, in_=wg[l, :, c0:c0 + 256].rearrange("(c p) f -> p c f", p=128)), w=[Bg])
            self.P.dma("gpsimd", lambda e: e.dma_start(
                out=u3, in_=wu[l, :, c0:c0 + 256].rearrange("(c p) f -> p c f", p=128)), w=[Bu])
            self.P.dma("gpsimd", lambda e: e.dma_start(
                out=d3, in_=wd[l, c0:c0 + 256, :].rearrange("(c p) d -> p c d", p=128)), w=[Bd])
        load(0)
        load(1)
        ai = 0
        gi = 0
        yi = 0
        YB = (4, 5, 6, 7)

        def down(item):
            nonlocal yi
            d3, Bd, Aj, tt = item
            ts = slice(tt * TT, (tt + 1) * TT)
            for dc in range(8):
                b = YB[yi % 4]
                py, Bpy = self.pb[b], self.Bpb[b]
                yi += 1
                for j in range(2):
                    self.MM(py[:, :], d3[:, j, dc * 128:(dc + 1) * 128], Aj[j][0], j == 0, j == 1,
                            r=[Bd, Aj[j][1]], w=[Bpy], inc=(j == 1))
                self.V(lambda e, py=py, dc=dc, ts=ts: e.scalar_tensor_tensor(
                    out=self.h[:, dc, ts], in0=py[:, :], scalar=0.5, in1=self.h[:, dc, ts],
                    op0=ALU.mult, op1=ALU.add), r=[Bpy, self.Bh[tt]], w=[self.Bh[tt]])
        prev = None
        for g in range(NG):
            g3, Bg, u3, Bu, d3, Bd = slots[g % NSLOT]
            for tt in range(NT):
                ts = slice(tt * TT, (tt + 1) * TT)
                Aj = []
                for j in range(2):
                    pg, Bpg = self.pb[gi % 2], self.Bpb[gi % 2]
                    pu, Bpu = self.pb[2 + gi % 2], self.Bpb[2 + gi % 2]
                    gi += 1
                    for ch in range(8):
                        self.MM(pg[:, :], g3[:, ch, j * 128:(j + 1) * 128], self.xn[:, ch, ts], ch == 0, ch == 7,
                                r=[Bg, self.Bxn[tt]], w=[Bpg], inc=(ch == 7))
                    for ch in range(8):
                        self.MM(pu[:, :], u3[:, ch, j * 128:(j + 1) * 128], self.xn[:, ch, ts], ch == 0, ch == 7,
                                r=[Bu, self.Bxn[tt]], w=[Bpu], inc=(ch == 7))
                    sg, Bsg = sgb[ai % len(sgb)]
                    a_ap, Ba = abuf[ai % NA]
                    ai += 1
                    self.A(lambda e, sg=sg, pg=pg: e.activation(out=sg, in_=pg[:, :], func=AF.Silu), r=[Bpg], w=[Bsg])
                    self.V(lambda e, a_ap=a_ap, sg=sg, pu=pu: e.tensor_tensor(out=a_ap, in0=pu[:, :], in1=sg, op=ALU.mult),
                           r=[Bpu, Bsg], w=[Ba])
                    Aj.append((a_ap, Ba))
                if prev is not None:
                    down(prev)
                prev = (d3, Bd, Aj, tt)
                if tt == 0 and g + 2 < NG:
                    load(g + 2)
        down(prev)

    def bank(self):
        i = (0, 1, 2, 3)[self._rr % 4]
        self._rr += 1
        return self.pb[i], self.Bpb[i]

    def cp(self, out, in_, r, w):
        self._cpi += 1
        if self._cpi % 2 == 0:
            self.V(lambda e: e.tensor_copy(out=out, in_=in_), r=r, w=w)
        else:
            self.A(lambda e: e.activation(out=out, in_=in_, func=AF.Copy), r=r, w=w)

    def load_w(self, stk, wap2d, kc, ncols, name):
        ap, B = stk.phi(kc * ncols * 2, BF16, name)
        w3 = ap.rearrange("p (c f) -> p c f", c=kc)
        self.P.dma("gpsimd", lambda e: e.dma_start(out=w3, in_=wap2d.rearrange("(c p) f -> p c f", p=128)), w=[B])
        return w3, B

    def load_w_at(self, off, wap2d, kc, ncols, name):
        ap, B = self.arena.at(off, kc * ncols * 2, BF16, name)
        w3 = ap.rearrange("p (c f) -> p c f", c=kc)
        self.P.dma("gpsimd", lambda e: e.dma_start(out=w3, in_=wap2d.rearrange("(c p) f -> p c f", p=128)), w=[B])
        return w3, B

    def proj(self, ps_ap, Bps, w3, Bw, c0, M, tt):
        ts = slice(tt * TT, (tt + 1) * TT)
        for ch in range(8):
            self.MM(ps_ap, w3[:, ch, c0:c0 + M], self.xn[:, ch, ts], ch == 0, ch == 7,
                    r=[Bw, self.Bxn[tt]], w=[Bps], inc=(ch == 7))

    def proj_tm(self, ps_ap, Bps, w3, Bw, c0, n, tb):
        tt = tb // 4
        for ch in range(8):
            self.MM(ps_ap, self.xn[:, ch, tb * 128:(tb + 1) * 128], w3[:, ch, c0:c0 + n], ch == 0, ch == 7,
                    r=[Bw, self.Bxn[tt]], w=[Bps], inc=(ch == 7))

    def rstd_from(self, out, Bout, in_, Bin, scale):
        epsb = self.cst[:, CS_EPS:CS_EPS + 1]
        self.A(lambda e: e.activation(out=out, in_=in_, func=AF.Ln, bias=epsb, scale=scale), r=[Bin, self.Bcst], w=[Bout])
        self.A(lambda e: e.activation(out=out, in_=out, func=AF.Exp, scale=-0.5), r=[Bout], w=[Bout])

    def alloc_tables(self):
        tb_t = self.P.sb("ropetab", [128, 4 * T], BF16)
        self.tab_t = tb_t
        self.cosM, self.sinM = tb_t[:, 0:T], tb_t[:, T:2 * T]
        self.cosR, self.sinR = tb_t[:, 2 * T:3 * T], tb_t[:, 3 * T:4 * T]
        self.Btab = Buf("tab")

    def setup_tables(self, pos, half):
        P = self.P
        stk = self.stk
        stk.reset_hi()
        pi, Bpi = stk.phi(T * 4, I32, "posi")
        pf, Bpf = stk.phi(T * 4, F32, "posf")
        a, Ba = stk.phi(T * 4, F32, "ang")
        k, Bk = stk.phi(T * 4, F32, "kf")
        ki, Bki = stk.phi(T * 4, I32, "ki")
        P.dma("sync", lambda e: e.dma_start(out=pi, in_=pos[0, half * T:(half + 1) * T].partition_broadcast(128)), w=[Bpi])
        self.V(lambda e: e.tensor_copy(out=pf, in_=pi), r=[Bpi], w=[Bpf])
        PI = float(np.pi)
        TWO_PI = 2.0 * PI
        PI_LO = 3.1415925
        cst = self.cst
        for (invc, sgnc, cos_t, sin_t) in ((CS_INVM, CS_SGNM, self.cosM, self.sinM),
                                           (CS_INVR, CS_SGNR, self.cosR, self.sinR)):
            for which in ("sin", "cos"):
                if which == "sin":
                    self.V(lambda e, invc=invc: e.tensor_scalar(out=a, in0=pf, scalar1=cst[:, invc:invc + 1], scalar2=None,
                                                                op0=ALU.mult), r=[Bpf, self.Bcst], w=[Ba])
                else:
                    self.V(lambda e, invc=invc: e.tensor_scalar(out=a, in0=pf, scalar1=cst[:, invc:invc + 1],
                                                                scalar2=PI / 2, op0=ALU.mult, op1=ALU.add),
                           r=[Bpf, self.Bcst], w=[Ba])
                self.V(lambda e: e.tensor_scalar(out=k, in0=a, scalar1=1.0 / TWO_PI, scalar2=None, op0=ALU.mult),
                       r=[Ba], w=[Bk])
                self.V(lambda e: e.tensor_copy(out=ki, in_=k), r=[Bk], w=[Bki])
                self.V(lambda e: e.tensor_copy(out=k, in_=ki), r=[Bki], w=[Bk])
                self.V(lambda e: e.scalar_tensor_tensor(out=a, in0=k, scalar=-TWO_PI, in1=a, op0=ALU.mult, op1=ALU.add),
                       r=[Bk, Ba], w=[Ba])
                self.V(lambda e: e.tensor_scalar(out=k, in0=a, scalar1=PI, scalar2=-TWO_PI, op0=ALU.is_gt, op1=ALU.mult),
                       r=[Ba], w=[Bk])
                self.V(lambda e: e.tensor_tensor(out=a, in0=a, in1=k, op=ALU.add), r=[Ba, Bk], w=[Ba])
                self.V(lambda e: e.tensor_scalar(out=k, in0=a, scalar1=-PI, scalar2=TWO_PI, op0=ALU.is_lt, op1=ALU.mult),
                       r=[Ba], w=[Bk])
                self.V(lambda e: e.tensor_tensor(out=a, in0=a, in1=k, op=ALU.add), r=[Ba, Bk], w=[Ba])
                self.V(lambda e: e.tensor_scalar(out=a, in0=a, scalar1=PI_LO, scalar2=-PI_LO, op0=ALU.min, op1=ALU.max),
                       r=[Ba], w=[Ba])
                if which == "sin":
                    self.A(lambda e: e.activation(out=k, in_=a, func=AF.Sin), r=[Ba], w=[Bk])
                    self.V(lambda e, sgnc=sgnc, sin_t=sin_t: e.tensor_scalar(
                        out=sin_t, in0=k, scalar1=cst[:, sgnc:sgnc + 1], scalar2=None, op0=ALU.mult),
                        r=[Bk, self.Bcst], w=[self.Btab])
                else:
                    self.A(lambda e, cos_t=cos_t: e.activation(out=cos_t, in_=a, func=AF.Sin), r=[Ba], w=[self.Btab])

    def setup_lb(self):
        P = self.P
        lbt = P.sb("lbt", [128, 52], F32)
        self.lb = lbt[:, 0:16]
        self.omlb = lbt[:, 16:32]
        self.Blb = Buf("lb")
        e_ = lbt[:, 32:48]
        s4 = lbt[:, 48:52]
        B = self.Blb
        e3 = e_.rearrange("p (h l) -> p h l", l=4)
        lb3 = self.lb.rearrange("p (h l) -> p h l", l=4)
        self.A(lambda e: e.activation(out=e_, in_=self.pp[:, PP_LBL:PP_LBL + 16], func=AF.Exp), r=[self.Bpp], w=[B])
        self.V(lambda e: e.tensor_reduce(out=s4, in_=e3, axis=mybir.AxisListType.X, op=ALU.add), r=[B], w=[B])
        self.V(lambda e: e.reciprocal(out=s4, in_=s4), r=[B], w=[B])
        self.V(lambda e: e.tensor_tensor(out=e3, in0=e3, in1=s4.rearrange("p (h o) -> p h o", o=1).broadcast_to([128, 4, 4]),
                                         op=ALU.mult), r=[B], w=[B])
        self.V(lambda e: e.memset(self.lb, 0.0), r=[B], w=[B])
        for l in range(1, 4):
            self.V(lambda e, l=l: e.tensor_tensor(out=lb3[:, :, l], in0=lb3[:, :, l - 1], in1=e3[:, :, l], op=ALU.add),
                   r=[B], w=[B])
        self.V(lambda e: e.tensor_scalar(out=self.omlb, in0=self.lb, scalar1=-1.0, scalar2=1.0, op0=ALU.mult, op1=ALU.add),
               r=[B], w=[B])

    def mixers(self, l, half):
        P = self.P
        self.norm(PP_MIX + l * 8, 1.0 / D)
        for tt in range(NT):
            ts = slice(tt * TT, (tt + 1) * TT)
            P.dma("sync", lambda e, ts=ts: e.dma_start(out=self.hsp[:, ts].rearrange("(c p) t -> p c t", p=128), in_=self.h[:, :, ts]),
                  r=[self.Bh[tt]], w=[self.Bhsp[tt]])
        for b in self.Bh:
            merge_into(self.hb, b)
        M = Stack(self.arena, self.H0, self.AR_BYTES)
        self.M = M
        self.ycat = []
        self.Bycat = []
        for i in range(4):
            ap, B = self.arena.at(i * 2 * T * 2, 2 * T * 2, BF16, "ycat%d" % i)
            self.ycat.append(ap.rearrange("p (c t) -> p c t", c=2))
            self.Bycat.append(B)
        def stop(tag):
            if self.stop_at == tag:
                self.stopped = True
            return self.stopped
        if stop("spill"):
            return
        self.sg(l, M)
        M.reset()
        if stop("sg"):
            return
        self.hgrn2_s1(l, M)
        M.reset_hi()
        if self.stopped or stop("hg1"):
            return
        pred = (half == 1)
        if not pred:
            self.put("hg", l, [(self.hg_Sfin, 128, 256, self.Bhg_Sfin)], F32)
        else:
            self.hg_Spred, self.Bhg_Spred = self.load_state(M, "hg", l)
        self.hgrn2_s2(l, M, pred)
        M.reset()
        if stop("hg2"):
            return
        self.ret_s1(l, M)
        M.reset_hi()
        if stop("rt1"):
            return
        if not pred:
            self.put("rt", l, [(self.rt_Sfin, 128, 256, self.Brt_Sfin)], F32)
        else:
            self.rt_Spred, self.Brt_Spred = self.load_state(M, "rt", l)
        self.ret_s2(l, M, pred)
        M.reset()
        if stop("rt2"):
            return
        self.mla_s1(l, M)
        M.reset_hi()
        if stop("ml1"):
            return
        if not pred:
            self.put("ml", l, [(self.ml_lat[:, T:2 * T], 128, T, self.Bml_lat), (self.ml_kr[:, T:2 * T], 128, T, self.Bml_kr)], BF16)
        else:
            xs, Bxs = self.xsd[("ml", l)]
            P.dma("sync", lambda e: e.dma_start(out=self.ml_lat[:, 0:T], in_=xs[0:128, 0:T]), r=[Bxs], w=[self.Bml_lat])
            P.dma("sync", lambda e: e.dma_start(out=self.ml_kr[0:32, 0:T], in_=xs[0:32, T:2 * T]), r=[Bxs], w=[self.Bml_kr])
        self.mla_s2(l, M, pred)
        M.reset()
        if stop("ml2"):
            return
        if l == 0 and half == getattr(self, "dbg_half", 0):
            for i, nm in enumerate(("ya", "yb", "yc", "yd")):
                if nm in self.dbg:
                    o = self.dout("dbg_" + nm, [256, T], BF16)
                    for c in range(2):
                        P.dma("sync", lambda e, c=c, i=i, o=o: e.dma_start(out=o[c * 128:(c + 1) * 128, :], in_=self.ycat[i][:, c, :]),
                              r=[self.Bycat[i]], w=[self.Bout])
        wo, Bwo = self.load_w(M, self.W["w_out"][l, :, :], 8, D, "w_out")
        self.wout(l, wo, Bwo)

    def put(self, name, l, pieces, dt):
        P = self.P
        XW = sum(p[2] for p in pieces)
        xs = self.nc.dram_tensor("xs_%s%d" % (name, l), [128, XW], dt).ap()
        Bxs = Buf("xs")
        self.xsd[(name, l)] = (xs, Bxs)
        c0 = 0
        for (ap, rows, width, B) in pieces:
            P.dma("sync", lambda e, ap=ap, rows=rows, c0=c0, width=width: e.dma_start(out=xs[0:rows, c0:c0 + width], in_=ap), r=[B], w=[Bxs])
            c0 += width

    def load_state(self, M, name, l):
        xs, Bxs = self.xsd[(name, l)]
        sp, Bsp = M.phi(256 * 4, F32, name + "_sp32")
        self.P.dma("sync", lambda e: e.dma_start(out=sp, in_=xs[0:128, 0:256]), r=[Bxs], w=[Bsp])
        hs, Bhs = M.plo(256 * 2, BF16, name + "_Spred")
        self.V(lambda e: e.tensor_copy(out=hs, in_=sp), r=[Bsp], w=[Bhs])
        return hs, Bhs

    def sg(self, l, M):
        P = self.P
        W_in = self.W["w_in"]
        yb, Byb = self.ycat[1], self.Bycat[1]
        uT, Bu = M.phi(2 * T * 2, BF16, "sg_u")
        uT3 = uT.rearrange("p (c t) -> p c t", c=2)
        w3, Bw = self.load_w(M, W_in[l, :, C_ZU:C_ZU + 256], 8, 256, "w_zu")
        wv, Bwv = self.load_w(M, W_in[l, :, C_ZV:C_ZV + 256], 8, 256, "w_zv")
        for cc in range(2):
            for tt in range(NT):
                ts = slice(tt * TT, (tt + 1) * TT)
                ps, Bps = self.bank()
                self.proj(ps[:, :], Bps, w3, Bw, cc * 128, 128, tt)
                self.A(lambda e, ps=ps, cc=cc, ts=ts: e.activation(out=uT3[:, cc, ts], in_=ps[:, :], func=AF.Gelu),
                       r=[Bps], w=[Bu])
        gv, Bgv = M.phi(NB * 256 * 4, F32, "sg_gv")
        gv3 = gv.rearrange("p (b c) -> p b c", b=NB)
        st, Bst = M.phi(NB * 6 * 4, F32, "sg_st")
        mv, Bmv = M.phi(NB * 2 * 4, F32, "sg_mv")
        mv3 = mv.rearrange("p (b two) -> p b two", two=2)
        rsd, Brsd = M.phi(NB * 4, F32, "sg_rsd")
        lng, Blng = M.phi(256 * 4, F32, "sg_lng")
        vtm, Bvtm = M.phi(NB * 256 * 2, BF16, "sg_vtm")
        vtm3 = vtm.rearrange("p (b c) -> p b c", b=NB)
        wsf, Bwsf = M.phi(4 * 128 * 4, F32, "sg_wsf")
        wsf3 = wsf.rearrange("p (h t) -> p h t", h=4)
        wm, Bwm = M.phi(4 * 128 * 2, BF16, "sg_wm")
        wm3 = wm.rearrange("p (h t) -> p h t", h=4)
        bsf, Bbsf = M.phi(512 * 4, F32, "sg_bsf")
        bsb, Bbsb = M.phi(512 * 2, BF16, "sg_bsb")
        P.dma("sync", lambda e: e.dma_start(out=lng, in_=self.W["sg_ln"][l, :].partition_broadcast(128)), w=[Blng])
        P.dma("sync", lambda e: e.dma_start(out=wsf3, in_=self.W["sg_wT"][l].rearrange("h s t -> s h t")), w=[Bwsf])
        P.dma("sync", lambda e: e.dma_start(out=bsf[0:1, :], in_=self.W["sg_b"][l:l + 1].rearrange("o h t -> o (h t)")), w=[Bbsf])
        caus = self.cst[:, CB_CAUS:CB_CAUS + 128]
        for hh in range(4):
            self.V(lambda e, hh=hh: e.tensor_tensor(out=wm3[:, hh, :], in0=wsf3[:, hh, :], in1=caus, op=ALU.mult),
                   r=[Bwsf, self.Bcst], w=[Bwm])
        self.V(lambda e: e.tensor_copy(out=bsb[0:1, :], in_=bsf[0:1, :]), r=[Bbsf], w=[Bbsb])
        for tb in range(NB):
            ps, Bps = self.bank()
            self.proj_tm(ps[:, 0:256], Bps, wv, Bwv, 0, 256, tb)
            self.A(lambda e, ps=ps, tb=tb: e.activation(out=gv3[:, tb, :], in_=ps[:, 0:256], func=AF.Gelu), r=[Bps], w=[Bgv])
            self.V(lambda e, tb=tb: e.bn_stats(out=st[:, tb * 6:(tb + 1) * 6], in_=gv3[:, tb, :]), r=[Bgv], w=[Bst])
            self.V(lambda e, tb=tb: e.bn_aggr(out=mv[:, tb * 2:(tb + 1) * 2], in_=st[:, tb * 6:(tb + 1) * 6]), r=[Bst], w=[Bmv])
        self.rstd_from(rsd, Brsd, mv3[:, :, 1], Bmv, 1.0)
        for tb in range(NB):
            self.V(lambda e, tb=tb: e.tensor_scalar(out=gv3[:, tb, :], in0=gv3[:, tb, :], scalar1=mv3[:, tb, 0:1],
                                                    scalar2=rsd[:, tb:tb + 1], op0=ALU.subtract, op1=ALU.mult),
                   r=[Bgv, Bmv, Brsd], w=[Bgv])
            self.G(lambda e, tb=tb: e.tensor_tensor(out=vtm3[:, tb, :], in0=gv3[:, tb, :], in1=lng, op=ALU.mult),
                   r=[Bgv, Blng], w=[Bvtm])
        onesrow = self.cb[0:1, CB_ONES:CB_ONES + 64]
        for tt in range(NT):
            ts = slice(tt * TT, (tt + 1) * TT)
            for pr in range(2):
                ps, Bps = self.bank()
                for bi in range(4):
                    tb = tt * 4 + bi
                    for hp in range(2):
                        hh = pr * 2 + hp
                        o = ps[hp * 64:(hp + 1) * 64, bi * 128:(bi + 1) * 128]
                        self.MM(o, vtm3[:, tb, hh * 64:(hh + 1) * 64], wm3[:, hh, :], True, False, r=[Bvtm, Bwm], w=[Bps], inc=False)
                        self.MM(o, onesrow, bsb[0:1, hh * 128:(hh + 1) * 128], False, True, r=[Bbsb, self.Bcb], w=[Bps],
                                inc=(bi == 3 and hp == 1))
                self.V(lambda e, ps=ps, pr=pr, ts=ts: e.tensor_tensor(out=yb[:, pr, ts], in0=ps[:, :], in1=uT3[:, pr, ts], op=ALU.mult),
                       r=[Bps, Bu], w=[Byb])
    def hgrn2_s1(self, l, M):
        P = self.P
        W_in = self.W["w_in"]
        oloc, Bol = M.plo(2 * T * 2, BF16, "hg_oloc")
        self.hg_oloc, self.Bhg_oloc = oloc.rearrange("p (c t) -> p c t", c=2), Bol
        qB, BqB = M.plo(4 * T * 2, BF16, "hg_qB")
        self.hg_qB, self.Bhg_qB = qB.rearrange("p (h t) -> p h t", h=4), BqB
        gate, Bgate = M.plo(2 * T * 2, BF16, "hg_gate")
        self.hg_gate, self.Bhg_gate = gate.rearrange("p (c t) -> p c t", c=2), Bgate
        wg3, Bwg = self.load_w(M, W_in[l, :, C_HG:C_HG + 256], 8, 256, "w_hg")
        wv3, Bwv = self.load_w(M, W_in[l, :, C_HI:C_HI + 256], 8, 256, "w_hi")
        for cc in range(2):
            for tt in range(NT):
                ts = slice(tt * TT, (tt + 1) * TT)
                ps, Bps = self.bank()
                self.proj(ps[:, :], Bps, wg3, Bwg, cc * 128, 128, tt)
                self.A(lambda e, ps=ps, cc=cc, ts=ts: e.activation(out=self.hg_gate[:, cc, ts], in_=ps[:, :], func=AF.Silu),
                       r=[Bps], w=[Bgate])
        vtm, Bvtm = M.phi(NB * 256 * 2, BF16, "hg_vtm")
        vtm3 = vtm.rearrange("p (b c) -> p b c", b=NB)
        for tb in range(NB):
            ps, Bps = self.bank()
            self.proj_tm(ps[:, 0:256], Bps, wv3, Bwv, 0, 256, tb)
            self.cp(vtm3[:, tb, :], ps[:, 0:256], [Bps], [Bvtm])
        if self.stop_at == "hg1a":
            self.stopped = True
            return
        fg, Bfg = M.phi(T * 4, F32, "hg_fg")
        bb, Bbb = M.phi(T * 4, F32, "hg_b")
        tmp, Btmp = M.phi(T * 4, F32, "hg_tmp")
        qT, BqT = M.phi(T * 2, BF16, "hg_qT")
        kT, BkT = M.phi(T * 2, BF16, "hg_kT")
        qtl, Bqtl = M.phi(T * 2, BF16, "hg_qtl")
        ktl, Bktl = M.phi(T * 2, BF16, "hg_ktl")
        khT, BkhT = M.phi(T * 2, BF16, "hg_khT")
        khtm, Bkhtm = M.phi(NB * 128 * 2, BF16, "hg_khtm")
        khtm3 = khtm.rearrange("p (b k) -> p b k", b=NB)
        ebl, Bebl = M.phi(32 * 4, F32, "hg_ebl")
        S32, BS32 = M.phi(64 * 4, F32, "hg_S32")
        Sb = [M.phi(64 * 2, BF16, "hg_Sb%d" % i) for i in range(2)]
        msk = [M.phi(128 * 2, BF16, "hg_msk%d" % i) for i in range(2)]
        rmask, Brm = M.phi(T * 2, BF16, "hg_rmask")
        onesT, Bon = M.phi(T * 2, BF16, "hg_ones")
        self.G(lambda e: e.memset(rmask, 1.0), w=[Brm])
        self.G(lambda e: e.memset(rmask.rearrange("p (c j) -> p c j", j=64)[:, :, 0:1], 0.0), w=[Brm])
        self.G(lambda e: e.memset(onesT, 1.0), w=[Bon])
        Sfin, BSfin = M.plo(256 * 4, F32, "hg_Sfin")
        self.hg_Sfin, self.Bhg_Sfin = Sfin, BSfin
        bd = self.cb[:, CB_BD64:CB_BD64 + 128]
        ident = self.cb[:, CB_IDENT:CB_IDENT + 128]
        wf_off = M.phi_off(8 * 128 * 2)
        wq_off = M.phi_off(8 * 128 * 2)
        for hh in range(4):
            pr, hp = hh // 2, hh % 2
            wf3, Bwf = self.load_w_at(wf_off, W_in[l, :, C_HF + hh * 128:C_HF + (hh + 1) * 128], 8, 128, "w_hf")
            wq3, Bwq = self.load_w_at(wq_off, W_in[l, :, C_HQ + hh * 128:C_HQ + (hh + 1) * 128], 8, 128, "w_hq")
            ci = hh * 4 + l
            for tt in range(NT):
                ts = slice(tt * TT, (tt + 1) * TT)
                ps, Bps = self.bank()
                self.proj(ps[:, :], Bps, wf3, Bwf, 0, 128, tt)
                self.A(lambda e, ps=ps, ts=ts: e.activation(out=fg[:, ts], in_=ps[:, :], func=AF.Sigmoid), r=[Bps], w=[Bfg])
                self.V(lambda e, ts=ts, ci=ci: e.tensor_scalar(out=fg[:, ts], in0=fg[:, ts], scalar1=self.omlb[:, ci:ci + 1],
                                                               scalar2=self.lb[:, ci:ci + 1], op0=ALU.mult, op1=ALU.add),
                       r=[Bfg, self.Blb], w=[Bfg])
                self.G(lambda e, ts=ts: e.tensor_scalar(out=kT[:, ts], in0=fg[:, ts], scalar1=-1.0, scalar2=1.0,
                                                        op0=ALU.mult, op1=ALU.add), r=[Bfg], w=[BkT])
                self.A(lambda e, ts=ts: e.activation(out=fg[:, ts], in_=fg[:, ts], func=AF.Ln), r=[Bfg, BkT], w=[Bfg])
                ps2, Bps2 = self.bank()
                self.proj(ps2[:, :], Bps2, wq3, Bwq, 0, 128, tt)
                self.cp(qT[:, ts], ps2[:, :], [Bps2], [BqT])
            self.V(lambda e: e.tensor_tensor_scan(out=bb, data0=rmask, data1=fg, initial=0.0, op0=ALU.mult, op1=ALU.add),
                   r=[Brm, Bfg], w=[Bbb])
            self.V(lambda e: e.tensor_tensor_scan(out=tmp, data0=onesT, data1=fg, initial=0.0, op0=ALU.mult, op1=ALU.add),
                   r=[Bon, Bfg], w=[Btmp])
            self.A(lambda e: e.activation(out=tmp, in_=tmp, func=AF.Exp), r=[Btmp], w=[Btmp])
            self.G(lambda e, hh=hh: e.tensor_tensor(out=self.hg_qB[:, hh, :], in0=qT, in1=tmp, op=ALU.mult),
                   r=[BqT, Btmp], w=[BqB])
            self.A(lambda e: e.activation(out=tmp, in_=bb, func=AF.Exp), r=[Bbb, BqB], w=[Btmp])
            self.G(lambda e: e.tensor_tensor(out=qtl, in0=qT, in1=tmp, op=ALU.mult), r=[BqT, Btmp], w=[Bqtl])
            self.A(lambda e: e.activation(out=tmp, in_=bb, func=AF.Exp, scale=-1.0), r=[Bbb, Bqtl], w=[Btmp])
            self.G(lambda e: e.tensor_tensor(out=ktl, in0=kT, in1=tmp, op=ALU.mult), r=[BkT, Btmp], w=[Bktl])
            b3 = bb.rearrange("p (c j) -> p c j", j=64)
            self.A(lambda e: e.activation(out=ebl, in_=b3[:, :, 63], func=AF.Exp), r=[Bbb], w=[Bebl])
            self.G(lambda e: e.tensor_tensor(out=khT.rearrange("p (c j) -> p c j", j=64), in0=ktl.rearrange("p (c j) -> p c j", j=64),
                                             in1=ebl.rearrange("p (c o) -> p c o", o=1).broadcast_to([128, 32, 64]), op=ALU.mult),
                   r=[Bktl, Bebl], w=[BkhT])
            if self.stop_at == "hg1b":
                self.stopped = True
                return
            for tb in range(NB):
                psx, Bpt = self.bank()
                pt = psx[:, 0:64].bitcast(BF16)
                self.P.op("tensor", lambda e, pt=pt, tb=tb: e.transpose(out=pt, in_=khT[:, tb * 128:(tb + 1) * 128], identity=ident),
                          r=[BkhT, self.Bcb], w=[Bpt])
                self.cp(khtm3[:, tb, :], pt, [Bpt], [Bkhtm])
            if self.stop_at == "hg1c":
                self.stopped = True
                return
            self.V(lambda e: e.memset(S32, 0.0), w=[BS32])
            self.G(lambda e: e.memset(Sb[0][0], 0.0), w=[Sb[0][1]])
            si = 0
            import os as _os
            SK = set(_os.environ.get("KSKIP", "").split(","))
            for tg in range(NT):
                po, Bpo = self.pb[4 + (tg % 2)], self.Bpb[4 + (tg % 2)]
                pkv, Bpkv = self.pb[6], self.Bpb[6]
                for bi in range(4):
                    tb = tg * 4 + bi
                    for c in range(2):
                        cidx = bi * 2 + c
                        if "kv" in SK:
                            continue
                        self.MM(self.pb[6 + c][:, bi * 64:(bi + 1) * 64], khtm3[c * 64:(c + 1) * 64, tb, :],
                                vtm3[c * 64:(c + 1) * 64, tb, hh * 64:(hh + 1) * 64], True, True,
                                r=[Bkhtm, Bvtm], w=[self.Bpb[6 + c]], inc=(bi == 3))
                for bi in range(4):
                    tb = tg * 4 + bi
                    blk = slice(tb * 128, (tb + 1) * 128)
                    ps, Bps = self.bank()
                    m_ap, Bm = msk[tb % 2]
                    o = po[hp * 64:(hp + 1) * 64, bi * 128:(bi + 1) * 128]
                    if "sc" not in SK:
                        self.MM(ps[:, 0:128], ktl[:, blk], qtl[:, blk], True, True, r=[Bktl, Bqtl], w=[Bps])
                        self.V(lambda e, ps=ps, m_ap=m_ap: e.tensor_tensor(out=m_ap, in0=ps[:, 0:128], in1=bd, op=ALU.mult),
                               r=[Bps, self.Bcb], w=[Bm])
                        self.MM(o, vtm3[:, tb, hh * 64:(hh + 1) * 64], m_ap, True, "st" in SK, r=[Bvtm, Bm], w=[Bpo], inc=("st" in SK))
                    for c in range(2):
                        cidx = bi * 2 + c
                        gc = tb * 2 + c
                        s_ap, Bs = Sb[si % 2]
                        oc = po[hp * 64:(hp + 1) * 64, bi * 128 + c * 64:bi * 128 + (c + 1) * 64]
                        if "st" not in SK:
                            self.MM(oc, s_ap, qtl[:, tb * 128 + c * 64:tb * 128 + (c + 1) * 64], False, c == 1,
                                    r=[Bs, Bqtl], w=[Bpo], inc=True)
                        if "up" not in SK:
                            self.V(lambda e, gc=gc, bi=bi, c=c: e.scalar_tensor_tensor(
                                out=S32, in0=S32, scalar=ebl[:, gc:gc + 1], in1=self.pb[6 + c][:, bi * 64:(bi + 1) * 64],
                                op0=ALU.mult, op1=ALU.add), r=[BS32, Bebl, self.Bpb[6 + c]], w=[BS32])
                        si += 1
                        s2, Bs2 = Sb[si % 2]
                        if "cast" not in SK:
                            self.V(lambda e, s2=s2: e.tensor_copy(out=s2, in_=S32), r=[BS32], w=[Bs2])
                ts = slice(tg * TT, (tg + 1) * TT)
                if "ev" not in SK:
                    self.cp(self.hg_oloc[hp * 64:(hp + 1) * 64, pr, ts], po[hp * 64:(hp + 1) * 64, :], [Bpo], [Bol])
            self.V(lambda e, hh=hh: e.tensor_copy(out=Sfin[:, hh * 64:(hh + 1) * 64], in_=S32), r=[BS32], w=[BSfin])

    def hgrn2_s2(self, l, M, pred):
        yc, Byc = self.ycat[2], self.Bycat[2]
        o32 = [M.phi(TT * 4, F32, "hg2_o%d" % i) for i in range(2)]
        sq = [M.phi(TT * 2, BF16, "hg2_sq%d" % i) for i in range(2)]
        rs = [M.phi(TT * 4, F32, "hg2_rs%d" % i) for i in range(2)]
        bones = self.cb[:, CB_BONES:CB_BONES + 128]
        if pred:
            Sp, BSp = self.hg_Spred, self.Bhg_Spred
        i = 0
        for pr in range(2):
            for tt in range(NT):
                ts = slice(tt * TT, (tt + 1) * TT)
                o_ap, Bo = o32[i % 2]
                sq_ap, Bsq = sq[i % 2]
                rs_ap, Brs = rs[i % 2]
                i += 1
                if pred:
                    ps, Bps = self.bank()
                    for hp in range(2):
                        hh = pr * 2 + hp
                        self.MM(ps[hp * 64:(hp + 1) * 64, :], Sp[:, hh * 64:(hh + 1) * 64], self.hg_qB[:, hh, ts], True, True,
                                r=[BSp, self.Bhg_qB], w=[Bps], inc=(hp == 1))
                    self.V(lambda e, ps=ps, o_ap=o_ap, pr=pr, ts=ts: e.tensor_tensor(out=o_ap, in0=ps[:, :], in1=self.hg_oloc[:, pr, ts], op=ALU.add),
                           r=[Bps, self.Bhg_oloc], w=[Bo])
                else:
                    self.V(lambda e, o_ap=o_ap, pr=pr, ts=ts: e.tensor_copy(out=o_ap, in_=self.hg_oloc[:, pr, ts]),
                           r=[self.Bhg_oloc], w=[Bo])
                self.G(lambda e, o_ap=o_ap, sq_ap=sq_ap: e.tensor_tensor(out=sq_ap, in0=o_ap, in1=o_ap, op=ALU.mult), r=[Bo], w=[Bsq])
                ps2, Bps2 = self.bank()
                self.MM(ps2[:, :], bones, sq_ap, True, True, r=[Bsq, self.Bcb], w=[Bps2])
                self.rstd_from(rs_ap, Brs, ps2[:, :], Bps2, 1.0 / 64)
                gcol = PP_HGN + l * 2 + pr
                self.V(lambda e, o_ap=o_ap, rs_ap=rs_ap, gcol=gcol: e.scalar_tensor_tensor(
                    out=o_ap, in0=o_ap, scalar=self.pp[:, gcol:gcol + 1], in1=rs_ap, op0=ALU.mult, op1=ALU.mult),
                    r=[Bo, Brs, self.Bpp], w=[Bo])
                self.G(lambda e, o_ap=o_ap, pr=pr, ts=ts: e.tensor_tensor(out=yc[:, pr, ts], in0=o_ap, in1=self.hg_gate[:, pr, ts], op=ALU.mult),
                       r=[Bo, self.Bhg_gate], w=[Byc])

    def ret_s1(self, l, M):
        W_in, W_sw = self.W["w_in"], self.W["w_in_sw"]
        oloc, Bol = M.plo(2 * T * 2, BF16, "rt_oloc")
        self.rt_oloc, self.Brt_oloc = oloc.rearrange("p (c t) -> p c t", c=2), Bol
        qseg, Bqseg = M.plo(2 * T * 2, BF16, "rt_qseg")
        self.rt_qseg, self.Brt_qseg = qseg.rearrange("p (c t) -> p c t", c=2), Bqseg
        gate, Bgate = M.plo(2 * T * 2, BF16, "rt_gate")
        self.rt_gate, self.Brt_gate = gate.rearrange("p (c t) -> p c t", c=2), Bgate
        Sfin, BSfin = M.plo(256 * 4, F32, "rt_Sfin")
        self.rt_Sfin, self.Brt_Sfin = Sfin, BSfin
        qr, Bqr = M.phi(2 * T * 2, BF16, "rt_qr")
        qr3 = qr.rearrange("p (c t) -> p c t", c=2)
        kr, Bkr = M.phi(2 * T * 2, BF16, "rt_kr")
        kr3 = kr.rearrange("p (c t) -> p c t", c=2)
        qd, Bqd = M.phi(2 * T * 2, BF16, "rt_qd")
        qd3 = qd.rearrange("p (c t) -> p c t", c=2)
        vtm, Bvtm = M.phi(NB * 256 * 2, BF16, "rt_vtm")
        vtm3 = vtm.rearrange("p (b c) -> p b c", b=NB)
        kdtm, Bkdtm = M.phi(NB * 256 * 2, BF16, "rt_kdtm")
        kdtm3 = kdtm.rearrange("p (b c) -> p b c", b=NB)
        t1 = [M.phi(TT * 4, F32, "rt_t1%d" % i) for i in range(2)]
        t2 = [M.phi(TT * 4, F32, "rt_t2%d" % i) for i in range(2)]
        S32, BS32 = M.phi(256 * 4, F32, "rt_S32")
        Sb = [M.phi(256 * 2, BF16, "rt_Sb%d" % i) for i in range(2)]
        msk = [M.phi(128 * 2, BF16, "rt_msk%d" % i) for i in range(3)]
        wg3, Bwg = self.load_w(M, W_in[l, :, C_RG:C_RG + 256], 8, 256, "w_rg")
        wv3, Bwv = self.load_w(M, W_in[l, :, C_RV:C_RV + 256], 8, 256, "w_rv")
        wq3, Bwq = self.load_w(M, W_in[l, :, C_RQ:C_RQ + 256], 8, 256, "w_rq")
        wqs3, Bwqs = self.load_w(M, W_sw[l, :, 32:288], 8, 256, "w_rqs")
        wk3, Bwk = self.load_w(M, W_in[l, :, C_RK:C_RK + 256], 8, 256, "w_rk")
        wks3, Bwks = self.load_w(M, W_sw[l, :, 288:544], 8, 256, "w_rks")
        for cc in range(2):
            for tt in range(NT):
                ts = slice(tt * TT, (tt + 1) * TT)
                ps, Bps = self.bank()
                self.proj(ps[:, :], Bps, wg3, Bwg, cc * 128, 128, tt)
                self.A(lambda e, ps=ps, cc=cc, ts=ts: e.activation(out=self.rt_gate[:, cc, ts], in_=ps[:, :], func=AF.Silu),
                       r=[Bps], w=[Bgate])
        for tb in range(NB):
            ps, Bps = self.bank()
            self.proj_tm(ps[:, 0:256], Bps, wv3, Bwv, 0, 256, tb)
            self.cp(vtm3[:, tb, :], ps[:, 0:256], [Bps], [Bvtm])
        ri = 0
        for (w3, Bw, ws3, Bws, dst3, Bdst) in ((wq3, Bwq, wqs3, Bwqs, qr3, Bqr), (wk3, Bwk, wks3, Bwks, kr3, Bkr)):
            for cc in range(2):
                for tt in range(NT):
                    ts = slice(tt * TT, (tt + 1) * TT)
                    ps, Bps = self.bank()
                    self.proj(ps[:, :], Bps, w3, Bw, cc * 128, 128, tt)
                    ps2, Bps2 = self.bank()
                    self.proj(ps2[:, :], Bps2, ws3, Bws, cc * 128, 128, tt)
                    a1, Ba1 = t1[ri % 2]
                    a2, Ba2 = t2[ri % 2]
                    ri += 1
                    self.V(lambda e, ps=ps, a1=a1, ts=ts: e.tensor_tensor(out=a1, in0=ps[:, :], in1=self.cosR[:, ts], op=ALU.mult),
                           r=[Bps, self.Btab], w=[Ba1])
                    self.V(lambda e, ps2=ps2, a2=a2, ts=ts: e.tensor_tensor(out=a2, in0=ps2[:, :], in1=self.sinR[:, ts], op=ALU.mult),
                           r=[Bps2, self.Btab], w=[Ba2])
                    self.G(lambda e, a1=a1, a2=a2, dst3=dst3, cc=cc, ts=ts: e.tensor_tensor(out=dst3[:, cc, ts], in0=a1, in1=a2, op=ALU.add),
                           r=[Ba1, Ba2], w=[Bdst])
        for pr in range(2):
            qdm = self.cb[:, CB_QD + pr * 128:CB_QD + (pr + 1) * 128]
            sgd = self.cb[:, CB_SEGD + pr * 16:CB_SEGD + (pr + 1) * 16]
            self.G(lambda e, pr=pr, qdm=qdm: e.tensor_tensor(
                out=qd3[:, pr, :].rearrange("p (b j) -> p b j", j=128), in0=qr3[:, pr, :].rearrange("p (b j) -> p b j", j=128),
                in1=qdm.rearrange("p (o j) -> p o j", o=1).broadcast_to([128, NB, 128]), op=ALU.mult),
                r=[Bqr, self.Bcb], w=[Bqd])
            self.G(lambda e, pr=pr, sgd=sgd: e.tensor_tensor(
                out=self.rt_qseg[:, pr, :].rearrange("p (b j) -> p b j", j=128), in0=qd3[:, pr, :].rearrange("p (b j) -> p b j", j=128),
                in1=sgd.rearrange("p (b o) -> p b o", o=1).broadcast_to([128, NB, 128]), op=ALU.mult),
                r=[Bqd, self.Bcb], w=[Bqseg])
        ident = self.cb[:, CB_IDENT:CB_IDENT + 128]
        ti = 0
        for pr in range(2):
            for tb in range(NB):
                psx, Bpt = self.bank()
                pt = psx[:, 0:64].bitcast(BF16)
                self.P.op("tensor", lambda e, pt=pt, tb=tb, pr=pr: e.transpose(out=pt, in_=kr3[:, pr, tb * 128:(tb + 1) * 128], identity=ident),
                          r=[Bkr, self.Bcb], w=[Bpt])
                for hp in range(2):
                    hh = pr * 2 + hp
                    kc = self.cst[:, CS_KDEC + hh:CS_KDEC + hh + 1]
                    if hp == 0:
                        self.V(lambda e, pt=pt, tb=tb, hh=hh, hp=hp, kc=kc: e.tensor_scalar(
                            out=kdtm3[:, tb, hh * 64:(hh + 1) * 64], in0=pt[:, hp * 64:(hp + 1) * 64], scalar1=kc, scalar2=None, op0=ALU.mult),
                            r=[Bpt, self.Bcst], w=[Bkdtm])
                    else:
                        self.A(lambda e, pt=pt, tb=tb, hh=hh, hp=hp, kc=kc: e.activation(
                            out=kdtm3[:, tb, hh * 64:(hh + 1) * 64], in_=pt[:, hp * 64:(hp + 1) * 64], func=AF.Copy, scale=kc),
                            r=[Bpt, self.Bcst], w=[Bkdtm])
        self.V(lambda e: e.memset(S32, 0.0), w=[BS32])
        self.G(lambda e: e.memset(Sb[0][0], 0.0), w=[Sb[0][1]])
        mi = 0
        for tg in range(NT):
            pos_ = [(self.pb[4], self.Bpb[4]), (self.pb[5], self.Bpb[5])]
            for bi in range(4):
                tb = tg * 4 + bi
                blk = slice(tb * 128, (tb + 1) * 128)
                s_ap, Bs = Sb[tb % 2]
                s2, Bs2 = Sb[(tb + 1) % 2]
                pkv, Bpkv = self.pb[6], self.Bpb[6]
                for hh in range(4):
                    pr, hp = hh // 2, hh % 2
                    rows = slice(hp * 64, (hp + 1) * 64)
                    po, Bpo = pos_[pr]
                    ps, Bps = self.bank()
                    self.MM(ps[:, 0:128], kr3[rows, pr, blk], qr3[rows, pr, blk], True, True, r=[Bkr, Bqr], w=[Bps])
                    m_ap, Bm = msk[mi % 3]
                    mi += 1
                    dm = self.cst[:, CS_DM + hh * 128:CS_DM + (hh + 1) * 128]
                    self.V(lambda e, ps=ps, m_ap=m_ap, dm=dm: e.tensor_tensor(out=m_ap, in0=ps[:, 0:128], in1=dm, op=ALU.mult),
                           r=[Bps, self.Bcst], w=[Bm])
                    o = po[rows, bi * 128:(bi + 1) * 128]
                    self.MM(o, vtm3[:, tb, hh * 64:(hh + 1) * 64], m_ap, True, False, r=[Bvtm, Bm], w=[Bpo], inc=False)
                    self.MM(o, s_ap[rows, hh * 64:(hh + 1) * 64], qd3[rows, pr, blk], False, True, r=[Bs, Bqd], w=[Bpo])
                    self.MM(pkv[rows, hh * 64:(hh + 1) * 64], kdtm3[:, tb, hh * 64:(hh + 1) * 64], vtm3[:, tb, hh * 64:(hh + 1) * 64],
                            True, True, r=[Bkdtm, Bvtm], w=[Bpkv], inc=(hh == 3))
                for hh in range(4):
                    hp = hh % 2
                    rows = slice(hp * 64, (hp + 1) * 64)
                    g128 = RET_G[hh] ** 128
                    self.V(lambda e, rows=rows, hh=hh, g128=g128, pkv=pkv: e.scalar_tensor_tensor(
                        out=S32[rows, hh * 64:(hh + 1) * 64], in0=S32[rows, hh * 64:(hh + 1) * 64], scalar=g128,
                        in1=pkv[rows, hh * 64:(hh + 1) * 64], op0=ALU.mult, op1=ALU.add), r=[BS32, Bpkv], w=[BS32])
                self.V(lambda e, s2=s2: e.tensor_copy(out=s2, in_=S32), r=[BS32], w=[Bs2])
            ts = slice(tg * TT, (tg + 1) * TT)
            for pr in range(2):
                po, Bpo = pos_[pr]
                self.cp(self.rt_oloc[:, pr, ts], po[:, :], [Bpo], [Bol])
        self.V(lambda e: e.tensor_copy(out=Sfin, in_=S32), r=[BS32], w=[BSfin])

    def ret_s2(self, l, M, pred):
        yd, Byd = self.ycat[3], self.Bycat[3]
        o32 = [M.phi(TT * 4, F32, "rt2_o%d" % i) for i in range(2)]
        ob = [M.phi(TT * 2, BF16, "rt2_ob%d" % i) for i in range(2)]
        sq = [M.phi(TT * 2, BF16, "rt2_sq%d" % i) for i in range(2)]
        rs = [M.phi(TT * 4, F32, "rt2_rs%d" % i) for i in range(2)]
        bones = self.cb[:, CB_BONES:CB_BONES + 128]
        if pred:
            Sp, BSp = self.rt_Spred, self.Brt_Spred
        i = 0
        for pr in range(2):
            for tt in range(NT):
                ts = slice(tt * TT, (tt + 1) * TT)
                o_ap, Bo = o32[i % 2]
                ob_ap, Bob = ob[i % 2]
                sq_ap, Bsq = sq[i % 2]
                rs_ap, Brs = rs[i % 2]
                i += 1
                if not pred:
                    self.V(lambda e, o_ap=o_ap, pr=pr, ts=ts: e.tensor_copy(out=o_ap, in_=self.rt_oloc[:, pr, ts]),
                           r=[self.Brt_oloc], w=[Bo])
                for hp in (range(2) if pred else ()):
                    hh = pr * 2 + hp
                    rows = slice(hp * 64, (hp + 1) * 64)
                    ps, Bps = self.bank()
                    self.MM(ps[rows, :], Sp[rows, hh * 64:(hh + 1) * 64], self.rt_qseg[rows, pr, ts], True, True,
                            r=[BSp, self.Brt_qseg], w=[Bps])
                    self.V(lambda e, ps=ps, o_ap=o_ap, pr=pr, ts=ts, rows=rows: e.tensor_tensor(
                        out=o_ap[rows, :], in0=ps[rows, :], in1=self.rt_oloc[rows, pr, ts], op=ALU.add),
                        r=[Bps, self.Brt_oloc], w=[Bo])
                self.G(lambda e, o_ap=o_ap, ob_ap=ob_ap: e.tensor_copy(out=ob_ap, in_=o_ap), r=[Bo], w=[Bob])
                pm, Bpm = self.bank()
                self.MM(pm[:, :], bones, ob_ap, True, True, r=[Bob, self.Bcb], w=[Bpm])
                self.V(lambda e, pm=pm, o_ap=o_ap: e.scalar_tensor_tensor(out=o_ap, in0=pm[:, :], scalar=-1.0 / 64, in1=o_ap,
                                                                          op0=ALU.mult, op1=ALU.add), r=[Bpm, Bo], w=[Bo])
                self.G(lambda e, o_ap=o_ap, sq_ap=sq_ap: e.tensor_tensor(out=sq_ap, in0=o_ap, in1=o_ap, op=ALU.mult), r=[Bo], w=[Bsq])
                ps2, Bps2 = self.bank()
                self.MM(ps2[:, :], bones, sq_ap, True, True, r=[Bsq, self.Bcb], w=[Bps2])
                self.rstd_from(rs_ap, Brs, ps2[:, :], Bps2, 1.0 / 64)
                gcol = PP_RTN + l * 2 + pr
                self.V(lambda e, o_ap=o_ap, rs_ap=rs_ap, gcol=gcol: e.scalar_tensor_tensor(
                    out=o_ap, in0=o_ap, scalar=self.pp[:, gcol:gcol + 1], in1=rs_ap, op0=ALU.mult, op1=ALU.mult),
                    r=[Bo, Brs, self.Bpp], w=[Bo])
                self.G(lambda e, o_ap=o_ap, pr=pr, ts=ts: e.tensor_tensor(out=yd[:, pr, ts], in0=o_ap, in1=self.rt_gate[:, pr, ts], op=ALU.mult),
                       r=[Bo, self.Brt_gate], w=[Byd])
    def mla_s1(self, l, M):
        W_in, W_sw = self.W["w_in"], self.W["w_in_sw"]
        qT, BqT = M.plo(4 * T * 2, BF16, "ml_qT")
        self.ml_qT, self.Bml_qT = qT.rearrange("p (h t) -> p h t", h=4), BqT
        lat, Blat = M.plo(2 * T * 2, BF16, "ml_lat")
        self.ml_lat, self.Bml_lat = lat, Blat
        kr, Bkr = M.plo(2 * T * 2, BF16, "ml_kr")
        self.ml_kr, self.Bml_kr = kr, Bkr
        self.G(lambda e: e.memset(kr, 0.0), w=[Bkr])
        wcq, Bwcq = self.load_w(M, W_in[l, :, C_CQ:C_CQ + 256], 8, 256, "w_cq")
        wckv, Bwckv = self.load_w(M, W_in[l, :, C_CKV:C_CKV + 128], 8, 128, "w_ckv")
        wkpe, Bwkpe = self.load_w(M, W_in[l, :, C_KPE:C_KPE + 32], 8, 32, "w_kpe")
        wkpes, Bwkpes = self.load_w(M, W_sw[l, :, 0:32], 8, 32, "w_kpes")
        wuq, Bwuq = self.load_w(M, self.W["w_uq"][l, :, :], 2, 384, "w_uq")
        wuqs, Bwuqs = self.load_w(M, self.W["w_uq_sw"][l, :, :], 2, 128, "w_uqs")
        cq = [M.phi(2 * TT * 4, F32, "ml_cq%d" % i) for i in range(2)]
        sq = [M.phi(2 * TT * 2, BF16, "ml_sq%d" % i) for i in range(2)]
        rs = [M.phi(TT * 4, F32, "ml_rs%d" % i) for i in range(2)]
        cqn = [M.phi(2 * TT * 2, BF16, "ml_cqn%d" % i) for i in range(2)]
        t1 = [M.phi(TT * 4, F32, "ml_t1%d" % i) for i in range(2)]
        t2 = [M.phi(TT * 4, F32, "ml_t2%d" % i) for i in range(2)]
        ones = self.cb[:, CB_ONES:CB_ONES + 128]
        ri = 0
        for tt in range(NT):
            ts = slice(tt * TT, (tt + 1) * TT)
            cq_ap, Bcq = cq[tt % 2]
            cq3 = cq_ap.rearrange("p (c t) -> p c t", c=2)
            sq_ap, Bsq = sq[tt % 2]
            sq3 = sq_ap.rearrange("p (c t) -> p c t", c=2)
            rs_ap, Brs = rs[tt % 2]
            cqn_ap, Bcqn = cqn[tt % 2]
            cqn3 = cqn_ap.rearrange("p (c t) -> p c t", c=2)
            for cc in range(2):
                ps, Bps = self.bank()
                self.proj(ps[:, :], Bps, wcq, Bwcq, cc * 128, 128, tt)
                self.cp(cq3[:, cc, :], ps[:, :], [Bps], [Bcq])
            self.G(lambda e, sq_ap=sq_ap, cq_ap=cq_ap: e.tensor_tensor(out=sq_ap, in0=cq_ap, in1=cq_ap, op=ALU.mult), r=[Bcq], w=[Bsq])
            ps, Bps = self.bank()
            for cc in range(2):
                self.MM(ps[:, :], ones, sq3[:, cc, :], cc == 0, cc == 1, r=[Bsq, self.Bcb], w=[Bps], inc=(cc == 1))
            self.rstd_from(rs_ap, Brs, ps[:, :], Bps, 1.0 / 256)
            for cc in range(2):
                gcol = PP_QN + l * 2 + cc
                self.V(lambda e, cc=cc, gcol=gcol, cqn3=cqn3, cq3=cq3, rs_ap=rs_ap: e.scalar_tensor_tensor(
                    out=cqn3[:, cc, :], in0=cq3[:, cc, :], scalar=self.pp[:, gcol:gcol + 1], in1=rs_ap, op0=ALU.mult, op1=ALU.mult),
                    r=[Bcq, Brs, self.Bpp], w=[Bcqn])
            for hh in range(4):
                psA, BpsA = self.bank()
                for kc in range(2):
                    self.MM(psA[0:96, :], wuq[:, kc, hh * 96:(hh + 1) * 96], cqn3[:, kc, :], kc == 0, kc == 1,
                            r=[Bwuq, Bcqn], w=[BpsA], inc=(kc == 1))
                psB, BpsB = self.bank()
                for kc in range(2):
                    self.MM(psB[64:96, :], wuqs[:, kc, hh * 32:(hh + 1) * 32], cqn3[:, kc, :], kc == 0, kc == 1,
                            r=[Bwuqs, Bcqn], w=[BpsB], inc=(kc == 1))
                self.cp(self.ml_qT[0:64, hh, ts], psA[0:64, :], [BpsA], [BqT])
                a1, Ba1 = t1[ri % 2]
                a2, Ba2 = t2[ri % 2]
                ri += 1
                self.V(lambda e, psA=psA, a1=a1, ts=ts: e.tensor_tensor(out=a1[64:96, :], in0=psA[64:96, :], in1=self.cosM[64:96, ts], op=ALU.mult),
                       r=[BpsA, self.Btab], w=[Ba1])
                self.V(lambda e, psB=psB, a2=a2, ts=ts: e.tensor_tensor(out=a2[64:96, :], in0=psB[64:96, :], in1=self.sinM[64:96, ts], op=ALU.mult),
                       r=[BpsB, self.Btab], w=[Ba2])
                self.V(lambda e, a1=a1, a2=a2, hh=hh, ts=ts: e.tensor_tensor(out=self.ml_qT[64:96, hh, ts], in0=a1[64:96, :], in1=a2[64:96, :], op=ALU.add),
                       r=[Ba1, Ba2], w=[BqT])
            ck_ap, Bck = cq[(tt + 1) % 2]
            ck = ck_ap[:, 0:TT]
            ps, Bps = self.bank()
            self.proj(ps[:, :], Bps, wckv, Bwckv, 0, 128, tt)
            self.cp(ck, ps[:, :], [Bps], [Bck])
            sk_ap, Bsk = sq[(tt + 1) % 2]
            sk = sk_ap[:, 0:TT]
            self.G(lambda e, sk=sk, ck=ck: e.tensor_tensor(out=sk, in0=ck, in1=ck, op=ALU.mult), r=[Bck], w=[Bsk])
            ps2, Bps2 = self.bank()
            self.MM(ps2[:, :], ones, sk, True, True, r=[Bsk, self.Bcb], w=[Bps2])
            rk_ap, Brk = rs[(tt + 1) % 2]
            self.rstd_from(rk_ap, Brk, ps2[:, :], Bps2, 1.0 / 128)
            gcol = PP_KVN + l
            self.V(lambda e, ck=ck, rk_ap=rk_ap, gcol=gcol, tt=tt: e.scalar_tensor_tensor(
                out=lat[:, T + tt * TT:T + (tt + 1) * TT], in0=ck, scalar=self.pp[:, gcol:gcol + 1], in1=rk_ap, op0=ALU.mult, op1=ALU.mult),
                r=[Bck, Brk, self.Bpp], w=[Blat])
            psA, BpsA = self.bank()
            self.proj(psA[0:32, :], BpsA, wkpe, Bwkpe, 0, 32, tt)
            psB, BpsB = self.bank()
            self.proj(psB[0:32, :], BpsB, wkpes, Bwkpes, 0, 32, tt)
            a1, Ba1 = t1[ri % 2]
            a2, Ba2 = t2[ri % 2]
            ri += 1
            self.V(lambda e, psA=psA, a1=a1, ts=ts: e.tensor_tensor(out=a1[0:32, :], in0=psA[0:32, :], in1=self.cosM[0:32, ts], op=ALU.mult),
                   r=[BpsA, self.Btab], w=[Ba1])
            self.V(lambda e, psB=psB, a2=a2, ts=ts: e.tensor_tensor(out=a2[0:32, :], in0=psB[0:32, :], in1=self.sinM[0:32, ts], op=ALU.mult),
                   r=[BpsB, self.Btab], w=[Ba2])
            self.V(lambda e, a1=a1, a2=a2, tt=tt: e.tensor_tensor(out=kr[0:32, T + tt * TT:T + (tt + 1) * TT], in0=a1[0:32, :], in1=a2[0:32, :], op=ALU.add),
                   r=[Ba1, Ba2], w=[Bkr])

    def cc_gather(self, xs, Bxs, xr, Bxr):
        P = self.P
        groups = [[0, 1], [2, 3], [4, 5], [6, 7]]
        key = ("cc",)
        if key not in P.sems:
            P._mksem(key, 16)
        waits = P._deps("gpsimd", [Bxs], [Bxr])
        seq = P.cnt[key] + 1
        P.cnt[key] = seq
        P._mark(key, seq, [Bxs], [Bxr])
        P.ops["gpsimd"].append((lambda e: e.collective_compute("AllGather", ALU.bypass, replica_groups=groups,
                                                               ins=[xs[:, :]], outs=[xr[:, :]]), waits, key))

    def mla_s2(self, l, M, pred):
        ya, Bya = self.ycat[0], self.Bycat[0]
        CT = 2 * T
        wukv, Bwukv = self.load_w(M, self.W["w_ukv"][l, :, :], 1, 512, "w_ukv")
        KT = [M.phi(CT * 2, BF16, "ml_KT%d" % i) for i in range(2)]
        VH = [M.phi(32 * 128 * 2, BF16, "ml_VH%d" % i) for i in range(2)]
        PT = [M.phi(TT * 2, BF16, "ml_PT%d" % i) for i in range(5)]
        RC = [M.phi(TT * 4, F32, "ml_RC%d" % i) for i in range(2)]
        for i in range(2):
            v3 = VH[i][0].rearrange("p (k c) -> p k c", k=32)
            self.G(lambda e, v3=v3: e.memset(v3[:, :, 64:128], 1.0), w=[VH[i][1]])
        caus = self.cb[:, CB_CAUS:CB_CAUS + 128]
        SC = 96.0 ** -0.5
        lat, Blat, kr, Bkr = self.ml_lat, self.Bml_lat, self.ml_kr, self.Bml_kr
        pi = 0
        qi = 0
        for hh in range(4):
            pr, hp = hh // 2, hh % 2
            kt, Bkt = KT[hh % 2]
            vh, Bvh = VH[hh % 2]
            vh3 = vh.rearrange("p (k c) -> p k c", k=32)
            for ct in range(0 if pred else 4, CT // TT):
                ps, Bps = self.bank()
                self.MM(ps[0:64, :], wukv[:, 0, hh * 128:hh * 128 + 64], lat[:, ct * TT:(ct + 1) * TT], True, True,
                        r=[Bwukv, Blat], w=[Bps])
                self.cp(kt[0:64, ct * TT:(ct + 1) * TT], ps[0:64, :], [Bps], [Bkt])
            k0 = 0 if pred else T
            self.V(lambda e, kt=kt, k0=k0: e.tensor_copy(out=kt[64:96, k0:], in_=kr[0:32, k0:]), r=[Bkr], w=[Bkt])
            for kg in range(0 if pred else 2, 4):
                ps, Bps = self.bank()
                for j in range(8):
                    kti = kg * 8 + j
                    self.MM(ps[:, j * 64:(j + 1) * 64], lat[:, kti * 128:(kti + 1) * 128], wukv[:, 0, hh * 128 + 64:hh * 128 + 128],
                            True, True, r=[Bwukv, Blat], w=[Bps], inc=(j == 7))
                self.cp(vh3[:, kg * 8:(kg + 1) * 8, 0:64], ps[:, :].rearrange("p (k c) -> p k c", k=8), [Bps], [Bvh])
            for Q in range(NT):
                po, Bpo = self.pb[4 + (qi % 2)], self.Bpb[4 + (qi % 2)]
                qi += 1
                keys = [(k_, 0, False, False) for k_ in range(16)] if pred else []
                for j in range(4 * Q + 4):
                    keys.append((16 + j, max(0, j - 4 * Q) * 128, False, j >= 4 * Q))
                LA = 2
                pend = []

                def emit_pv(item, first, last):
                    kti_, c0_, pt_, Bpt_ = item
                    self.MM(po[:, c0_:TT], vh3[:, kti_, :], pt_[:, c0_:TT], first, last, r=[Bvh, Bpt_], w=[Bpo])
                npv = 0
                for idx, (kti, c0, usebias, diag) in enumerate(keys):
                    ps, Bps = self.bank()
                    self.MM(ps[:, c0:TT], kt[0:96, kti * 128:(kti + 1) * 128], self.ml_qT[0:96, hh, Q * TT + c0:(Q + 1) * TT], True, True,
                            r=[Bkt, self.Bml_qT], w=[Bps])
                    pt, Bpt = PT[pi % len(PT)]
                    pi += 1
                    self.A(lambda e, ps=ps, pt=pt, c0=c0: e.activation(out=pt[:, c0:TT], in_=ps[:, c0:TT], func=AF.Exp, scale=SC),
                           r=[Bps], w=[Bpt])
                    if diag:
                        self.G(lambda e, pt=pt, c0=c0: e.tensor_tensor(out=pt[:, c0:c0 + 128], in0=pt[:, c0:c0 + 128], in1=caus, op=ALU.mult),
                               r=[Bpt, self.Bcb], w=[Bpt])
                    pend.append((kti, c0, pt, Bpt))
                    if len(pend) > LA:
                        emit_pv(pend.pop(0), npv == 0, False)
                        npv += 1
                while pend:
                    emit_pv(pend.pop(0), npv == 0, len(pend) == 0)
                    npv += 1
                rc, Brc = RC[qi % 2]
                self.V(lambda e, rc=rc, po=po: e.reciprocal(out=rc[64:128, :], in_=po[64:128, :]), r=[Bpo], w=[Brc])
                self.V(lambda e, rc=rc, po=po, hp=hp, pr=pr, Q=Q: e.tensor_tensor(
                    out=ya[hp * 64:(hp + 1) * 64, pr, Q * TT:(Q + 1) * TT], in0=po[0:64, :], in1=rc[64:128, :], op=ALU.mult),
                    r=[Bpo, Brc], w=[Bya])

    def wout(self, l, wo, Bwo):
        P = self.P
        self.h_ap, self.hb = self.arena.at(self.H0, self.H_BYTES, F32, "h")
        self.h = self.h_ap.rearrange("p (c t) -> p c t", c=8)
        self.Bh = [Buf("h%d" % i) for i in range(NT)]
        for b in self.Bh:
            merge_into(b, self.hb)
        for tt in range(NT):
            ts = slice(tt * TT, (tt + 1) * TT)
            P.dma("sync", lambda e, ts=ts: e.dma_start(out=self.h[:, :, ts], in_=self.hsp[:, ts].rearrange("(c p) t -> p c t", p=128)),
                  r=[self.Bhsp[tt]], w=[self.Bh[tt]])
        for tt in range(NT):
            ts = slice(tt * TT, (tt + 1) * TT)
            for dc in range(8):
                ps, Bps = self.bank()
                for kc in range(8):
                    self.MM(ps[:, :], wo[:, kc, dc * 128:(dc + 1) * 128], self.ycat[kc // 2][:, kc % 2, ts], kc == 0, kc == 7,
                            r=[Bwo, self.Bycat[kc // 2]], w=[Bps], inc=(kc == 7))
                self.V(lambda e, ps=ps, dc=dc, ts=ts: e.tensor_tensor(out=self.h[:, dc, ts], in0=ps[:, :], in1=self.h[:, dc, ts], op=ALU.add),
                       r=[Bps, self.Bh[tt]], w=[self.Bh[tt]])

    def ple(self, l, pT, c0):
        P = self.P
        self.norm(PP_PLE + l * 8, 1.0 / D)
        stk = self.stk
        stk.reset_hi()
        wpg, Bwpg = self.load_w(stk, self.W["ple_w_gate"][l, :, :], 8, D, "w_pg")
        wpp, Bwpp = self.load_w(stk, self.W["ple_w_proj"][l, :, :], 2, D, "w_pp")
        pt = [stk.phi(2 * TT * 2, BF16, "ple_pT%d" % i) for i in range(2)]
        gt = [stk.phi(TT * 4, F32, "ple_g%d" % i) for i in range(2)]
        tm = [stk.phi(TT * 4, F32, "ple_t%d" % i) for i in range(2)]
        i = 0
        for tt in range(NT):
            ts = slice(tt * TT, (tt + 1) * TT)
            p_ap, Bp = pt[tt % 2]
            p3 = p_ap.rearrange("p (c t) -> p c t", c=2)
            P.dma("gpsimd", lambda e, p3=p3, ts=ts: e.dma_start(out=p3, in_=pT[l, :, c0 + ts.start:c0 + ts.stop].rearrange("(c p) t -> p c t", p=128)), w=[Bp])
            for dc in range(8):
                psg, Bpsg = self.bank()
                for kc in range(8):
                    self.MM(psg[:, :], wpg[:, kc, dc * 128:(dc + 1) * 128], self.xn[:, kc, ts], kc == 0, kc == 7,
                            r=[Bwpg, self.Bxn[tt]], w=[Bpsg], inc=(kc == 7))
                g_ap, Bg = gt[i % 2]
                t_ap, Bt = tm[i % 2]
                i += 1
                self.A(lambda e, psg=psg, g_ap=g_ap: e.activation(out=g_ap, in_=psg[:, :], func=AF.Sigmoid), r=[Bpsg], w=[Bg])
                psp, Bpsp = self.bank()
                for kc in range(2):
                    self.MM(psp[:, :], wpp[:, kc, dc * 128:(dc + 1) * 128], p3[:, kc, :], kc == 0, kc == 1,
                            r=[Bwpp, Bp], w=[Bpsp], inc=(kc == 1))
                self.V(lambda e, psp=psp, g_ap=g_ap, t_ap=t_ap: e.tensor_tensor(out=t_ap, in0=psp[:, :], in1=g_ap, op=ALU.mult),
                       r=[Bpsp, Bg], w=[Bt])
                self.G(lambda e, t_ap=t_ap, dc=dc, ts=ts: e.tensor_tensor(out=self.h[:, dc, ts], in0=self.h[:, dc, ts], in1=t_ap, op=ALU.add),
                       r=[Bt, self.Bh[tt]], w=[self.Bh[tt]])

    def final_out(self, outT, c0):
        stk = self.stk
        stk.reset_hi()
        ones = self.cb[:, CB_ONES:CB_ONES + 128]
        sq, Bsq = stk.phi(8 * TT * 2, BF16, "sq")
        sq3 = sq.rearrange("p (c t) -> p c t", c=8)
        rs, Brs = stk.phi(TT * 4, F32, "rstd")
        ob = [stk.phi(TT * 4, F32, "ob%d" % i) for i in range(4)]
        oi = 0
        for tt in range(NT):
            ts = slice(tt * TT, (tt + 1) * TT)
            bank = 5 + (tt % 2)
            ps, Bps = self.pb[bank], self.Bpb[bank]
            for ch in range(8):
                if ch % 2 == 0:
                    self.G(lambda e, ch=ch, ts=ts: e.tensor_tensor(out=sq3[:, ch, :], in0=self.h[:, ch, ts],
                                                                  in1=self.h[:, ch, ts], op=ALU.mult),
                           r=[self.Bh[tt]], w=[Bsq])
                else:
                    self.A(lambda e, ch=ch, ts=ts: e.activation(out=sq3[:, ch, :], in_=self.h[:, ch, ts], func=AF.Square),
                           r=[self.Bh[tt]], w=[Bsq])
            for ch in range(8):
                self.MM(ps[:, :], ones, sq3[:, ch, :], ch == 0, ch == 7, r=[Bsq, self.Bcb], w=[Bps], inc=(ch == 7))
            epsb = self.cst[:, CS_EPS:CS_EPS + 1]
            self.A(lambda e, ps=ps: e.activation(out=rs, in_=ps[:, :], func=AF.Ln, bias=epsb, scale=1.0 / D),
                   r=[Bps, self.Bcst], w=[Brs])
            self.A(lambda e: e.activation(out=rs, in_=rs, func=AF.Exp, scale=-0.5), r=[Brs], w=[Brs])
            for ch in range(8):
                o_ap, Bo = ob[oi % 4]
                oi += 1
                self.V(lambda e, ch=ch, ts=ts, o_ap=o_ap: e.scalar_tensor_tensor(
                    out=o_ap, in0=self.h[:, ch, ts], scalar=self.pp[:, PP_FINAL + ch:PP_FINAL + ch + 1],
                    in1=rs, op0=ALU.mult, op1=ALU.mult), r=[self.Bh[tt], Brs, self.Bpp], w=[Bo])
                self.P.dma("sync", lambda e, ch=ch, ts=ts, o_ap=o_ap: e.dma_start(
                    out=outT[ch * 128:(ch + 1) * 128, c0 + ts.start:c0 + ts.stop], in_=o_ap), r=[Bo], w=[self.Bout])


PP_FFN1 = 0
PP_MIX = PP_FFN1 + L * 8
PP_FFN2 = PP_MIX + L * 8
PP_PLE = PP_FFN2 + L * 8
PP_FINAL = PP_PLE + L * 8
PP_QN = PP_FINAL + 8
PP_KVN = PP_QN + L * 2
PP_HGN = PP_KVN + L
PP_RTN = PP_HGN + L * 2
PP_LBL = PP_RTN + L * 2
PP_FLAG = PP_LBL + 4 * L
PPW = PP_FLAG + 1

CB_ONES = 0
CB_IDENT = 128
CB_BONES = 256
CB_CAUS = 384
CB_BD64 = 512
CB_QD = 640
CB_SEGD = 896
CSTBW = 928
CS_EPS = 928
CS_INVM = 929
CS_INVR = 930
CS_SGNM = 931
CS_SGNR = 932
CS_KDEC = 933
CS_DM = 937
CSTW = CS_DM + 512
RET_G = [1.0 - 2.0 ** (-(5.0 + h)) for h in range(4)]


def _consts():
    c = np.zeros((128, CSTW), np.float64)
    c[:, CB_ONES:CB_ONES + 128] = 1.0
    c[:, CB_IDENT:CB_IDENT + 128] = np.eye(128)
    bo = np.zeros((128, 128))
    bo[:64, :64] = 1.0
    bo[64:, 64:] = 1.0
    c[:, CB_BONES:CB_BONES + 128] = bo
    s = np.arange(128)[:, None]
    t = np.arange(128)[None, :]
    caus = (t >= s).astype(np.float64)
    c[:, CB_CAUS:CB_CAUS + 128] = caus
    c[:, CB_BD64:CB_BD64 + 128] = caus * bo
    p = np.arange(128)
    for pr in range(2):
        for hp in range(2):
            g = RET_G[pr * 2 + hp]
            c[hp * 64:(hp + 1) * 64, CB_QD + pr * 128:CB_QD + (pr + 1) * 128] = (g ** (np.arange(128) + 1.0) / 8.0)[None, :]
            c[hp * 64:(hp + 1) * 64, CB_SEGD + pr * 16:CB_SEGD + (pr + 1) * 16] = (g ** (128.0 * np.arange(16)))[None, :]
    c[:, CS_EPS] = EPS
    c[:, CS_INVM] = (np.float32(10000.0) ** (-(np.arange(0, 32, 2, dtype=np.float32)) / np.float32(32)))[p % 16]
    c[:, CS_INVR] = (np.float32(10000.0) ** (-(np.arange(0, 64, 2, dtype=np.float32)) / np.float32(64)))[p % 32]
    c[:, CS_SGNM] = np.where((p % 32) < 16, -1.0, 1.0)
    c[:, CS_SGNR] = np.where((p % 64) < 32, -1.0, 1.0)
    for h in range(4):
        g = RET_G[h]
        c[:, CS_KDEC + h] = g ** (127.0 - p)
        c[:, CS_DM + h * 128:CS_DM + (h + 1) * 128] = np.where(t >= s, g ** np.maximum(t - s, 0) / 8.0, 0.0)
    return c.astype(np.float32)


def _pack_pp(inp, has_pred):
    pp = np.zeros((128, PPW), np.float32)

    def fm(v):
        return np.ascontiguousarray(np.asarray(v, np.float32).reshape(-1, 128).T)
    for l in range(L):
        pp[:, PP_FFN1 + l * 8:PP_FFN1 + l * 8 + 8] = fm(inp["ffn1_norm"][l])
        pp[:, PP_MIX + l * 8:PP_MIX + l * 8 + 8] = fm(inp["mix_norm"][l])
        pp[:, PP_FFN2 + l * 8:PP_FFN2 + l * 8 + 8] = fm(inp["ffn2_norm"][l])
        pp[:, PP_PLE + l * 8:PP_PLE + l * 8 + 8] = fm(inp["ple_norm"][l])
        pp[:, PP_QN + l * 2:PP_QN + l * 2 + 2] = fm(inp["mla_q_norm"][l])
        pp[:, PP_KVN + l:PP_KVN + l + 1] = fm(inp["mla_kv_norm"][l])
        pp[:, PP_HGN + l * 2:PP_HGN + l * 2 + 2] = fm(inp["hg_norm"][l])
        pp[:, PP_RTN + l * 2:PP_RTN + l * 2 + 2] = fm(inp["ret_norm"][l])
        for h in range(4):
            pp[:, PP_LBL + h * L + l] = np.asarray(inp["hg_lb_logits"][l], np.float32)[h * 128:(h + 1) * 128]
    pp[:, PP_FINAL:PP_FINAL + 8] = fm(inp["final_norm"])
    pp[:, PP_FLAG] = 1.0 if has_pred else 0.0
    return pp


def _swap_half(w, c0, width, hd):
    blk = np.asarray(w)[..., c0:c0 + width]
    sh = blk.shape[:-1]
    b = blk.reshape(sh + (width // hd, 2, hd // 2))
    return np.ascontiguousarray(b[..., ::-1, :].reshape(sh + (width,)))


def _prep_shared(inp):
    f = lambda k: np.ascontiguousarray(np.asarray(inp[k], np.float32))
    sh = {}
    for n in ("ffn1_w_gate", "ffn1_w_up", "ffn2_w_gate", "ffn2_w_up", "ffn1_w_down", "ffn2_w_down", "w_in",
              "w_out", "ple_w_proj", "ple_w_gate", "sg_ln"):
        sh[n] = f(n)
    w_in = sh["w_in"]
    sh["w_in_sw"] = np.ascontiguousarray(np.concatenate(
        [_swap_half(w_in, C_KPE, 32, 32), _swap_half(w_in, C_RQ, 256, 64), _swap_half(w_in, C_RK, 256, 64)], axis=-1))
    uq = f("mla_w_uq").reshape(L, 256, 4, 96)
    sh["w_uq"] = np.ascontiguousarray(uq.reshape(L, 256, 384))
    sh["w_uq_sw"] = np.ascontiguousarray(_swap_half(uq[..., 64:96].reshape(L, 256, 128), 0, 128, 32))
    sh["w_ukv"] = f("mla_w_ukv")
    sh["sg_wT"] = np.ascontiguousarray(f("sg_w_s").transpose(0, 1, 3, 2))
    sh["sg_b"] = f("sg_b_s")
    sh["cst"] = _consts()
    return sh


def _core_inputs(inp, sh, c):
    b = c
    m = dict(sh)
    m["xT"] = np.ascontiguousarray(np.asarray(inp["x"], np.float32)[b].T)
    m["pT"] = np.ascontiguousarray(np.asarray(inp["p"], np.float32)[:, b].transpose(0, 2, 1))
    m["pos"] = np.ascontiguousarray(np.asarray(inp["positions"], np.int32)[b][None, :])
    m["pp"] = _pack_pp(inp, False)
    return m


def run(inputs, n_layers=L, dbg=None, cores=4, use_cc=False, stages=3, stop_at=None, halves=(0, 1)):
    bld = Builder(n_layers=n_layers, dbg=dbg, use_cc=use_cc, stages=stages, stop_at=stop_at, halves=halves)
    nc = bld.build()
    sh = _prep_shared(inputs)
    in_maps = []
    for c in range(cores):
        m = _core_inputs(inputs, sh, c)
        in_maps.append({k: m[k] for k in bld.dram})
    res = run_bass_kernel_spmd(nc, in_maps, core_ids=list(range(cores)))
    return res.results


def kernel(**inputs):
    res = run(inputs)
    out = np.empty((4, 2 * T, D), np.float32)
    for c in range(4):
        out[c] = res[c]["outT"].T
    return out
```

```python
import numpy as np
import concourse.bass as bass
import concourse.mybir as mybir
from concourse.bass_utils import run_bass_kernel_spmd
from contextlib import ExitStack

F32 = mybir.dt.float32
BF16 = mybir.dt.bfloat16
I32 = mybir.dt.int32
ALU = mybir.AluOpType
AF = mybir.ActivationFunctionType

ENGS = ["tensor", "vector", "scalar", "gpsimd", "sync"]
NDMA_SEM = 8

D = 1024
L = 4
T = 2048
TT = 512
NT = T // TT
NB = T // 128
DFF = 2816
NFC = DFF // 128
EPS = 1e-6
INW = 3488
C_CQ, C_CKV, C_KPE, C_ZU, C_ZV, C_HQ, C_HF, C_HI, C_HG, C_RQ, C_RK, C_RV, C_RG = (
    0, 256, 384, 416, 672, 928, 1440, 1952, 2208, 2464, 2720, 2976, 3232)


class Buf:
    __slots__ = ("name", "lw", "rd")

    def __init__(self, name=""):
        self.name = name
        self.lw = None
        self.rd = {}


class Prog:
    def __init__(self, nc):
        self.nc = nc
        self.es = ExitStack()
        self.ops = {e: [] for e in ENGS}
        self.cnt = {}
        self.sems = {}
        self.mult = {}
        self.waited = {e: {} for e in ENGS}
        self.dma_i = {e: 0 for e in ENGS}
        for e in ENGS:
            if e != "sync":
                self._mksem(e, 1)
        for e in ("sync", "gpsimd", "scalar"):
            for j in range(NDMA_SEM):
                self._mksem(("dma", e, j), 16)

    def _mksem(self, key, mult):
        name = key if isinstance(key, str) else "_".join(str(k) for k in key)
        self.sems[key] = self.es.enter_context(self.nc.semaphore("s_" + name))
        self.cnt[key] = 0
        self.mult[key] = mult

    def sb(self, name, shape, dt):
        return self.es.enter_context(self.nc.sbuf_tensor("sb_" + name, list(shape), dt))

    def ps(self, name, shape, dt=F32):
        return self.es.enter_context(self.nc.psum_tensor(name, list(shape), dt))

    def _deps(self, eng, reads, writes):
        deps = {}

        def add(k, s):
            if deps.get(k, 0) < s:
                deps[k] = s
        for b in reads:
            if b.lw is not None:
                add(*b.lw)
        for b in writes:
            if b.lw is not None:
                add(*b.lw)
            for k, s in b.rd.items():
                add(k, s)
        waits = []
        w = self.waited[eng]
        for k, s in deps.items():
            if k == "tensor" and eng == "tensor":
                continue
            if w.get(k, 0) >= s:
                continue
            w[k] = s
            waits.append((k, s))
        return waits

    def _mark(self, key, seq, reads, writes):
        for b in reads:
            if b.rd.get(key, 0) < seq:
                b.rd[key] = seq
        for b in writes:
            b.lw = (key, seq)
            b.rd = {}

    def op(self, eng, fn, r=(), w=(), inc=True):
        waits = self._deps(eng, r, w)
        seq = self.cnt[eng] + 1
        if inc:
            self.cnt[eng] = seq
        self._mark(eng, seq, r, w)
        self.ops[eng].append((fn, waits, eng if inc else None))

    def dma(self, eng, fn, r=(), w=()):
        i = self.dma_i[eng]
        self.dma_i[eng] = i + 1
        key = ("dma", eng, i % NDMA_SEM)
        waits = self._deps(eng, r, w)
        prev = self.cnt[key]
        if prev > 0 and self.waited[eng].get(key, 0) < prev:
            self.waited[eng][key] = prev
            waits.append((key, prev))
        seq = prev + 1
        self.cnt[key] = seq
        self._mark(key, seq, r, w)
        self.ops[eng].append((fn, waits, key))

    def wait_all(self, eng, bufs):
        waits = self._deps(eng, bufs, ())
        self.ops[eng].append((None, waits, None))

    def emit(self):
        nc = self.nc
        with nc.Block() as block:
            for e in ENGS:
                ops = self.ops[e]

                def body(engine, ops=ops):
                    for fn, waits, inckey in ops:
                        for k, s in waits:
                            engine.wait_ge(self.sems[k], s * self.mult[k])
                        if fn is None:
                            continue
                        inst = fn(engine)
                        if inckey is not None:
                            inst.then_inc(self.sems[inckey], self.mult[inckey])
                getattr(block, e)(body)
        self.es.close()


class Arena:
    def __init__(self, tile_bf16, nbytes):
        self.t = tile_bf16
        self.n = nbytes
        self.regs = []
        self.names = {}

    def at(self, off, size, dt, name=""):
        assert off % 4 == 0 and size % 4 == 0 and off + size <= self.n, (name, off, size, self.n)
        b = Buf(name)
        self.names[name] = (off, size, dt)
        keep = []
        for (o, s, ob) in self.regs:
            if o < off + size and off < o + s:
                merge_into(b, ob)
                if not (off <= o and o + s <= off + size):
                    keep.append((o, s, ob))
            else:
                keep.append((o, s, ob))
        self.regs = keep
        self.regs.append((off, size, b))
        ap = self.t[:, off // 2:(off + size) // 2]
        if dt != BF16:
            ap = ap.bitcast(dt)
        return ap, b


class Stack:
    def __init__(self, arena, start, end):
        self.a = arena
        self.start = start
        self.end = end
        self.lo = start
        self.hi = end

    def plo(self, size, dt, name=""):
        size = (size + 31) // 32 * 32
        assert self.lo + size <= self.hi, ("arena overflow lo", name, self.lo, size, self.hi)
        r = self.a.at(self.lo, size, dt, name)
        self.lo += size
        return r

    def phi(self, size, dt, name=""):
        size = (size + 31) // 32 * 32
        assert self.hi - size >= self.lo, ("arena overflow hi", name, self.lo, size, self.hi)
        self.hi -= size
        return self.a.at(self.hi, size, dt, name)

    def phi_off(self, size):
        size = (size + 31) // 32 * 32
        assert self.hi - size >= self.lo, ("arena overflow hi(off)", self.lo, size, self.hi)
        self.hi -= size
        return self.hi

    def reset_hi(self):
        self.hi = self.end

    def reset(self):
        self.lo = self.start
        self.hi = self.end


def merge_into(dst, src):
    for k, v in src.rd.items():
        if dst.rd.get(k, 0) < v:
            dst.rd[k] = v
    if src.lw is not None:
        k, v = src.lw
        if dst.rd.get(k, 0) < v:
            dst.rd[k] = v


def _esize(dt):
    return 2 if dt == BF16 else 4


class Builder:
    def __init__(self, n_layers=L, dbg=None, use_cc=False, stages=3, stop_at=None, halves=(0, 1)):
        self.nl = n_layers
        self.halves = halves
        self.stages = stages
        self.stop_at = stop_at
        self.stopped = False
        self.dbg = dbg or []
        self.use_cc = use_cc
        self.nc = nc = bass.Bass("TRN2", target_bir_lowering=False)
        self.P = Prog(nc)
        self.dram = {}
        self.outs = {}

    def din(self, name, shape, dt=F32):
        t = self.nc.dram_tensor(name, list(shape), dt, kind="ExternalInput").ap()
        self.dram[name] = t
        return t

    def dout(self, name, shape, dt=F32):
        t = self.nc.dram_tensor(name, list(shape), dt, kind="ExternalOutput").ap()
        self.outs[name] = t
        return t

    def V(self, fn, r=(), w=()):
        self.P.op("vector", fn, r, w)

    def A(self, fn, r=(), w=()):
        self.P.op("scalar", fn, r, w)

    def G(self, fn, r=(), w=()):
        self.P.op("gpsimd", fn, r, w)

    def MM(self, out, lhsT, rhs, start, stop, r=(), w=(), inc=True):
        self.P.op("tensor", lambda e: e.matmul(out, lhsT, rhs, start=start, stop=stop), r, w, inc=inc)

    def dump(self, name, ap, buf, shape, dt=F32):
        if name not in self.dbg:
            return
        o = self.dout("dbg_" + name, shape, dt)
        self.P.dma("sync", lambda e: e.dma_start(out=o, in_=ap), r=[buf], w=[self.Bout])

    def build(self):
        nc, P = self.nc, self.P
        nl = self.nl
        xT = self.din("xT", [D, 2 * T])
        pT = self.din("pT", [L, 256, 2 * T])
        pos = self.din("pos", [1, 2 * T], I32)
        pp = self.din("pp", [128, PPW])
        cst = self.din("cst", [128, CSTW])
        W = {}
        for n in ("ffn1_w_gate", "ffn1_w_up", "ffn2_w_gate", "ffn2_w_up"):
            W[n] = self.din(n, [L, D, DFF])
        for n in ("ffn1_w_down", "ffn2_w_down"):
            W[n] = self.din(n, [L, DFF, D])
        W["w_in"] = self.din("w_in", [L, D, INW])
        W["w_in_sw"] = self.din("w_in_sw", [L, D, 544])
        W["w_uq"] = self.din("w_uq", [L, 256, 384])
        W["w_uq_sw"] = self.din("w_uq_sw", [L, 256, 128])
        W["w_ukv"] = self.din("w_ukv", [L, 128, 512])
        W["sg_ln"] = self.din("sg_ln", [L, 256])
        W["sg_wT"] = self.din("sg_wT", [L, 4, 128, 128])
        W["sg_b"] = self.din("sg_b", [L, 4, 128])
        W["w_out"] = self.din("w_out", [L, D, D])
        W["ple_w_proj"] = self.din("ple_w_proj", [L, 256, D])
        W["ple_w_gate"] = self.din("ple_w_gate", [L, D, D])
        self.W = W
        outT = self.dout("outT", [D, 2 * T])
        self.Bout = Buf("out")
        hsp = nc.dram_tensor("hspill", [D, T], F32).ap()
        self.hsp = hsp
        self.Bhsp = [Buf("hspill%d" % i) for i in range(NT)]

        AR_BYTES = 148 * 1024
        self.AR_BYTES = AR_BYTES
        ar_t = P.sb("arena", [128, AR_BYTES // 2], BF16)
        self.arena = Arena(ar_t, AR_BYTES)
        self.H0 = 4 * 2 * T * 2
        self.H_BYTES = 8 * T * 4
        xn_t = P.sb("xn", [128, 8 * T], BF16)
        self.xn = xn_t[:].rearrange("p (c t) -> p c t", c=8)
        self.Bxn = [Buf("xn%d" % i) for i in range(NT)]
        pp_t = P.sb("pp", [128, PPW], F32)
        self.pp = pp_t
        self.Bpp = Buf("pp")
        cst_t = P.sb("cst", [128, CSTW], F32)
        self.cst = cst_t
        self.Bcst = Buf("cst")
        cb_t = P.sb("cstb", [128, CSTBW], BF16)
        self.cb = cb_t
        self.Bcb = Buf("cstb")
        pb_t = P.sb("pbias", [128, 1], F32)
        self.pbias = pb_t
        self.Bpbias = Buf("pbias")
        self.pb = [P.ps("pb%d" % i, [128, 512], F32) for i in range(8)]
        self.Bpb = [Buf("pb%d" % i) for i in range(8)]
        self._rr = 0
        self._cpi = 0

        P.dma("sync", lambda e: e.dma_start(out=pp_t[:], in_=pp), w=[self.Bpp])
        P.dma("sync", lambda e: e.dma_start(out=cst_t[:], in_=cst), w=[self.Bcst])
        self.V(lambda e: e.tensor_copy(out=cb_t[:, 0:CSTBW], in_=cst_t[:, 0:CSTBW]), r=[self.Bcst], w=[self.Bcb])
        self.V(lambda e: e.tensor_scalar(out=pb_t[:, 0:1], in0=pp_t[:, PP_FLAG:PP_FLAG + 1], scalar1=-1.0, scalar2=30000.0,
                                         op0=ALU.add, op1=ALU.mult), r=[self.Bpp], w=[self.Bpbias])

        self.hs = [nc.dram_tensor("hs%d" % i, [D, T], F32).ap() for i in range(2)]
        self.Bhs = [[Buf("hs0_%d" % i) for i in range(NT)], [Buf("hs1_%d" % i) for i in range(NT)]]
        self.tabd = [nc.dram_tensor("tabd%d" % i, [128, 4 * T], BF16).ap() for i in range(2)]
        self.Btabd = [Buf("tabd0"), Buf("tabd1")]
        self.xsd = {}
        self.h_ap, self.hb = self.arena.at(self.H0, self.H_BYTES, F32, "h")
        self.h = self.h_ap.rearrange("p (c t) -> p c t", c=8)
        self.Bh = [Buf("h%d" % i) for i in range(NT)]
        self.stk = Stack(self.arena, self.H0 + self.H_BYTES, AR_BYTES)
        self.pos = pos
        if self.stages >= 2:
            self.alloc_tables()
            for half in range(2):
                self.setup_tables(pos, half)
                P.dma("sync", lambda e, half=half: e.dma_start(out=self.tabd[half][:, :], in_=self.tab_t[:, :]),
                      r=[self.Btab], w=[self.Btabd[half]])
            self.setup_lb()
        for l in range(nl):
            for half in self.halves:
                self.half = half
                c0 = half * T
                self.h_ap, self.hb = self.arena.at(self.H0, self.H_BYTES, F32, "h")
                self.h = self.h_ap.rearrange("p (c t) -> p c t", c=8)
                self.Bh = [Buf("h%d" % i) for i in range(NT)]
                for bb_ in self.Bh:
                    merge_into(bb_, self.hb)
                src = xT[:, c0:c0 + T] if l == 0 else self.hs[half][:, :]
                for tt in range(NT):
                    ts = slice(tt * TT, (tt + 1) * TT)
                    P.dma("sync", lambda e, ts=ts, src=src: e.dma_start(out=self.h[:, :, ts], in_=src[:, ts].rearrange("(c p) t -> p c t", p=128)),
                          r=([] if l == 0 else [self.Bhs[half][tt]]), w=[self.Bh[tt]])
                if self.stages >= 2:
                    P.dma("sync", lambda e, half=half: e.dma_start(out=self.tab_t[:, :], in_=self.tabd[half][:, :]),
                          r=[self.Btabd[half]], w=[self.Btab])
                self.norm(PP_FFN1 + l * 8, 1.0 / D)
                self.ffn(W["ffn1_w_gate"], W["ffn1_w_up"], W["ffn1_w_down"], l)
                if "h_ffn1" in self.dbg and l == 0 and half == 0:
                    o = self.dout("dbg_h_ffn1", [D, T])
                    for ch in range(8):
                        P.dma("sync", lambda e, ch=ch: e.dma_start(out=o[ch * 128:(ch + 1) * 128, :], in_=self.h[:, ch, :]),
                              r=self.Bh, w=[self.Bout])
                if self.stages >= 2:
                    self.mixers(l, half)
                    if self.stopped:
                        break
                    if "h_mix" in self.dbg and l == 0 and half == getattr(self, "dbg_half", 0):
                        o = self.dout("dbg_h_mix", [D, T])
                        for ch in range(8):
                            P.dma("sync", lambda e, ch=ch, o=o: e.dma_start(out=o[ch * 128:(ch + 1) * 128, :], in_=self.h[:, ch, :]),
                                  r=self.Bh, w=[self.Bout])
                if self.stages >= 3:
                    self.norm(PP_FFN2 + l * 8, 1.0 / D)
                    self.ffn(W["ffn2_w_gate"], W["ffn2_w_up"], W["ffn2_w_down"], l)
                    self.ple(l, pT, c0)
                if l == nl - 1:
                    self.final_out(outT, c0)
                else:
                    for tt in range(NT):
                        ts = slice(tt * TT, (tt + 1) * TT)
                        P.dma("sync", lambda e, ts=ts, half=half: e.dma_start(out=self.hs[half][:, ts].rearrange("(c p) t -> p c t", p=128), in_=self.h[:, :, ts]),
                              r=[self.Bh[tt]], w=[self.Bhs[half][tt]])
                    for b in self.Bh:
                        merge_into(self.hb, b)
            if self.stopped:
                break
        P.wait_all("sync", [self.Bout])
        for j in range(NDMA_SEM):
            key = ("dma", "sync", j)
            if P.cnt[key] > 0:
                P.ops["sync"].append((None, [(key, P.cnt[key])], None))
        P.emit()
        return nc

    def norm(self, gcol, inv_n):
        stk = self.stk
        stk.reset_hi()
        ones = self.cb[:, CB_ONES:CB_ONES + 128]
        sqs = [stk.phi(8 * TT * 2, BF16, "sq%d" % i) for i in range(NT)]
        rss = [stk.phi(TT * 4, F32, "rstd%d" % i) for i in range(NT)]
        epsb = self.cst[:, CS_EPS:CS_EPS + 1]
        for tt in range(NT):
            ts = slice(tt * TT, (tt + 1) * TT)
            sq, Bsq = sqs[tt]
            sq3 = sq.rearrange("p (c t) -> p c t", c=8)
            for ch in range(8):
                if ch % 2 == 0:
                    self.G(lambda e, ch=ch, ts=ts, sq3=sq3: e.tensor_tensor(out=sq3[:, ch, :], in0=self.h[:, ch, ts],
                                                                           in1=self.h[:, ch, ts], op=ALU.mult),
                           r=[self.Bh[tt]], w=[Bsq])
                else:
                    self.A(lambda e, ch=ch, ts=ts, sq3=sq3: e.activation(out=sq3[:, ch, :], in_=self.h[:, ch, ts], func=AF.Square),
                           r=[self.Bh[tt]], w=[Bsq])
        for tt in range(NT):
            sq, Bsq = sqs[tt]
            sq3 = sq.rearrange("p (c t) -> p c t", c=8)
            rs, Brs = rss[tt]
            bank = 5 + (tt % 2)
            ps, Bps = self.pb[bank], self.Bpb[bank]
            for ch in range(8):
                self.MM(ps[:, :], ones, sq3[:, ch, :], ch == 0, ch == 7, r=[Bsq, self.Bcb], w=[Bps], inc=(ch == 7))
            self.A(lambda e, ps=ps, rs=rs: e.activation(out=rs, in_=ps[:, :], func=AF.Ln, bias=epsb, scale=inv_n),
                   r=[Bps, self.Bcst], w=[Brs])
            self.A(lambda e, rs=rs: e.activation(out=rs, in_=rs, func=AF.Exp, scale=-0.5), r=[Brs], w=[Brs])
        for tt in range(NT):
            ts = slice(tt * TT, (tt + 1) * TT)
            rs, Brs = rss[tt]
            for ch in range(8):
                self.V(lambda e, ch=ch, ts=ts, rs=rs: e.scalar_tensor_tensor(
                    out=self.xn[:, ch, ts], in0=self.h[:, ch, ts], scalar=self.pp[:, gcol + ch:gcol + ch + 1],
                    in1=rs, op0=ALU.mult, op1=ALU.mult), r=[self.Bh[tt], Brs, self.Bpp], w=[self.Bxn[tt]])

    def ffn(self, wg, wu, wd, l):
        stk = self.stk
        stk.reset_hi()
        NG = NFC // 2
        NSLOT = 3
        slots = []
        for s in range(NSLOT):
            g_ap, Bg = stk.phi(8 * 256 * 2, BF16, "wg%d" % s)
            u_ap, Bu = stk.phi(8 * 256 * 2, BF16, "wu%d" % s)
            d_ap, Bd = stk.phi(2 * D * 2, BF16, "wd%d" % s)
            slots.append((g_ap.rearrange("p (c f) -> p c f", c=8), Bg, u_ap.rearrange("p (c f) -> p c f", c=8), Bu,
                          d_ap.rearrange("p (c d) -> p c d", c=2), Bd))
        NA = 6
        abuf = [stk.phi(TT * 2, BF16, "A%d" % i) for i in range(NA)]
        sgb = [stk.phi(TT * 2, BF16, "sg%d" % i) for i in range(3)]

        def load(g):
            g3, Bg, u3, Bu, d3, Bd = slots[g % NSLOT]
            c0 = g * 256
            self.P.dma("gpsimd", lambda e: e.dma_start(
                out=g3, in_=wg[l, :, c0:c0 + 256].rearrange("(c p) f -> p c f", p=128)), w=[Bg])
            self.P.dma("gpsimd", lambda e: e.dma_start(
                out=u3, in_=wu[l, :, c0:c0 + 256].rearrange("(c p) f -> p c f", p=128)), w=[Bu])
            self.P.dma("gpsimd", lambda e: e.dma_start(
                out=d3, in_=wd[l, c0:c0 + 256, :].rearrange("(c p) d -> p c d", p=128)), w=[Bd])
        load(0)
        load(1)
        ai = 0
        gi = 0
        yi = 0
        YB = (4, 5, 6, 7)

        def down(item):
            nonlocal yi
            d3, Bd, Aj, tt = item
            ts = slice(tt * TT, (tt + 1) * TT)
            for dc in range(8):
                b = YB[yi % 4]
                py, Bpy = self.pb[b], self.Bpb[b]
                yi += 1
                for j in range(2):
                    self.MM(py[:, :], d3[:, j, dc * 128:(dc + 1) * 128], Aj[j][0], j == 0, j == 1,
                            r=[Bd, Aj[j][1]], w=[Bpy], inc=(j == 1))
                self.V(lambda e, py=py, dc=dc, ts=ts: e.scalar_tensor_tensor(
                    out=self.h[:, dc, ts], in0=py[:, :], scalar=0.5, in1=self.h[:, dc, ts],
                    op0=ALU.mult, op1=ALU.add), r=[Bpy, self.Bh[tt]], w=[self.Bh[tt]])
        prev = None
        for g in range(NG):
            g3, Bg, u3, Bu, d3, Bd = slots[g % NSLOT]
            for tt in range(NT):
                ts = slice(tt * TT, (tt + 1) * TT)
                Aj = []
                for j in range(2):
                    pg, Bpg = self.pb[gi % 2], self.Bpb[gi % 2]
                    pu, Bpu = self.pb[2 + gi % 2], self.Bpb[2 + gi % 2]
                    gi += 1
                    for ch in range(8):
                        self.MM(pg[:, :], g3[:, ch, j * 128:(j + 1) * 128], self.xn[:, ch, ts], ch == 0, ch == 7,
                                r=[Bg, self.Bxn[tt]], w=[Bpg], inc=(ch == 7))
                    for ch in range(8):
                        self.MM(pu[:, :], u3[:, ch, j * 128:(j + 1) * 128], self.xn[:, ch, ts], ch == 0, ch == 7,
                                r=[Bu, self.Bxn[tt]], w=[Bpu], inc=(ch == 7))
                    sg, Bsg = sgb[ai % len(sgb)]
                    a_ap, Ba = abuf[ai % NA]
                    ai += 1
                    self.A(lambda e, sg=sg, pg=pg: e.activation(out=sg, in_=pg[:, :], func=AF.Silu), r=[Bpg], w=[Bsg])
                    self.V(lambda e, a_ap=a_ap, sg=sg, pu=pu: e.tensor_tensor(out=a_ap, in0=pu[:, :], in1=sg, op=ALU.mult),
                           r=[Bpu, Bsg], w=[Ba])
                    Aj.append((a_ap, Ba))
                if prev is not None:
                    down(prev)
                prev = (d3, Bd, Aj, tt)
                if tt == 0 and g + 2 < NG:
                    load(g + 2)
        down(prev)

    def bank(self):
        i = (0, 1, 2, 3)[self._rr % 4]
        self._rr += 1
        return self.pb[i], self.Bpb[i]

    def cp(self, out, in_, r, w):
        self._cpi += 1
        if self._cpi % 2 == 0:
            self.V(lambda e: e.tensor_copy(out=out, in_=in_), r=r, w=w)
        else:
            self.A(lambda e: e.activation(out=out, in_=in_, func=AF.Copy), r=r, w=w)

    def load_w(self, stk, wap2d, kc, ncols, name):
        ap, B = stk.phi(kc * ncols * 2, BF16, name)
        w3 = ap.rearrange("p (c f) -> p c f", c=kc)
        self.P.dma("gpsimd", lambda e: e.dma_start(out=w3, in_=wap2d.rearrange("(c p) f -> p c f", p=128)), w=[B])
        return w3, B

    def load_w_at(self, off, wap2d, kc, ncols, name):
        ap, B = self.arena.at(off, kc * ncols * 2, BF16, name)
        w3 = ap.rearrange("p (c f) -> p c f", c=kc)
        self.P.dma("gpsimd", lambda e: e.dma_start(out=w3, in_=wap2d.rearrange("(c p) f -> p c f", p=128)), w=[B])
        return w3, B

    def proj(self, ps_ap, Bps, w3, Bw, c0, M, tt):
        ts = slice(tt * TT, (tt + 1) * TT)
        for ch in range(8):
            self.MM(ps_ap, w3[:, ch, c0:c0 + M], self.xn[:, ch, ts], ch == 0, ch == 7,
                    r=[Bw, self.Bxn[tt]], w=[Bps], inc=(ch == 7))

    def proj_tm(self, ps_ap, Bps, w3, Bw, c0, n, tb):
        tt = tb // 4
        for ch in range(8):
            self.MM(ps_ap, self.xn[:, ch, tb * 128:(tb + 1) * 128], w3[:, ch, c0:c0 + n], ch == 0, ch == 7,
                    r=[Bw, self.Bxn[tt]], w=[Bps], inc=(ch == 7))

    def rstd_from(self, out, Bout, in_, Bin, scale):
        epsb = self.cst[:, CS_EPS:CS_EPS + 1]
        self.A(lambda e: e.activation(out=out, in_=in_, func=AF.Ln, bias=epsb, scale=scale), r=[Bin, self.Bcst], w=[Bout])
        self.A(lambda e: e.activation(out=out, in_=out, func=AF.Exp, scale=-0.5), r=[Bout], w=[Bout])

    def alloc_tables(self):
        tb_t = self.P.sb("ropetab", [128, 4 * T], BF16)
        self.tab_t = tb_t
        self.cosM, self.sinM = tb_t[:, 0:T], tb_t[:, T:2 * T]
        self.cosR, self.sinR = tb_t[:, 2 * T:3 * T], tb_t[:, 3 * T:4 * T]
        self.Btab = Buf("tab")

    def setup_tables(self, pos, half):
        P = self.P
        stk = self.stk
        stk.reset_hi()
        pi, Bpi = stk.phi(T * 4, I32, "posi")
        pf, Bpf = stk.phi(T * 4, F32, "posf")
        a, Ba = stk.phi(T * 4, F32, "ang")
        k, Bk = stk.phi(T * 4, F32, "kf")
        ki, Bki = stk.phi(T * 4, I32, "ki")
        P.dma("sync", lambda e: e.dma_start(out=pi, in_=pos[0, half * T:(half + 1) * T].partition_broadcast(128)), w=[Bpi])
        self.V(lambda e: e.tensor_copy(out=pf, in_=pi), r=[Bpi], w=[Bpf])
        PI = float(np.pi)
        TWO_PI = 2.0 * PI
        PI_LO = 3.1415925
        cst = self.cst
        for (invc, sgnc, cos_t, sin_t) in ((CS_INVM, CS_SGNM, self.cosM, self.sinM),
                                           (CS_INVR, CS_SGNR, self.cosR, self.sinR)):
            for which in ("sin", "cos"):
                if which == "sin":
                    self.V(lambda e, invc=invc: e.tensor_scalar(out=a, in0=pf, scalar1=cst[:, invc:invc + 1], scalar2=None,
                                                                op0=ALU.mult), r=[Bpf, self.Bcst], w=[Ba])
                else:
                    self.V(lambda e, invc=invc: e.tensor_scalar(out=a, in0=pf, scalar1=cst[:, invc:invc + 1],
                                                                scalar2=PI / 2, op0=ALU.mult, op1=ALU.add),
                           r=[Bpf, self.Bcst], w=[Ba])
                self.V(lambda e: e.tensor_scalar(out=k, in0=a, scalar1=1.0 / TWO_PI, scalar2=None, op0=ALU.mult),
                       r=[Ba], w=[Bk])
                self.V(lambda e: e.tensor_copy(out=ki, in_=k), r=[Bk], w=[Bki])
                self.V(lambda e: e.tensor_copy(out=k, in_=ki), r=[Bki], w=[Bk])
                self.V(lambda e: e.scalar_tensor_tensor(out=a, in0=k, scalar=-TWO_PI, in1=a, op0=ALU.mult, op1=ALU.add),
                       r=[Bk, Ba], w=[Ba])
                self.V(lambda e: e.tensor_scalar(out=k, in0=a, scalar1=PI, scalar2=-TWO_PI, op0=ALU.is_gt, op1=ALU.mult),
                       r=[Ba], w=[Bk])
                self.V(lambda e: e.tensor_tensor(out=a, in0=a, in1=k, op=ALU.add), r=[Ba, Bk], w=[Ba])
                self.V(lambda e: e.tensor_scalar(out=k, in0=a, scalar1=-PI, scalar2=TWO_PI, op0=ALU.is_lt, op1=ALU.mult),
                       r=[Ba], w=[Bk])
                self.V(lambda e: e.tensor_tensor(out=a, in0=a, in1=k, op=ALU.add), r=[Ba, Bk], w=[Ba])
                self.V(lambda e: e.tensor_scalar(out=a, in0=a, scalar1=PI_LO, scalar2=-PI_LO, op0=ALU.min, op1=ALU.max),
                       r=[Ba], w=[Ba])
                if which == "sin":
                    self.A(lambda e: e.activation(out=k, in_=a, func=AF.Sin), r=[Ba], w=[Bk])
                    self.V(lambda e, sgnc=sgnc, sin_t=sin_t: e.tensor_scalar(
                        out=sin_t, in0=k, scalar1=cst[:, sgnc:sgnc + 1], scalar2=None, op0=ALU.mult),
                        r=[Bk, self.Bcst], w=[self.Btab])
                else:
                    self.A(lambda e, cos_t=cos_t: e.activation(out=cos_t, in_=a, func=AF.Sin), r=[Ba], w=[self.Btab])

    def setup_lb(self):
        P = self.P
        lbt = P.sb("lbt", [128, 52], F32)
        self.lb = lbt[:, 0:16]
        self.omlb = lbt[:, 16:32]
        self.Blb = Buf("lb")
        e_ = lbt[:, 32:48]
        s4 = lbt[:, 48:52]
        B = self.Blb
        e3 = e_.rearrange("p (h l) -> p h l", l=4)
        lb3 = self.lb.rearrange("p (h l) -> p h l", l=4)
        self.A(lambda e: e.activation(out=e_, in_=self.pp[:, PP_LBL:PP_LBL + 16], func=AF.Exp), r=[self.Bpp], w=[B])
        self.V(lambda e: e.tensor_reduce(out=s4, in_=e3, axis=mybir.AxisListType.X, op=ALU.add), r=[B], w=[B])
        self.V(lambda e: e.reciprocal(out=s4, in_=s4), r=[B], w=[B])
        self.V(lambda e: e.tensor_tensor(out=e3, in0=e3, in1=s4.rearrange("p (h o) -> p h o", o=1).broadcast_to([128, 4, 4]),
                                         op=ALU.mult), r=[B], w=[B])
        self.V(lambda e: e.memset(self.lb, 0.0), r=[B], w=[B])
        for l in range(1, 4):
            self.V(lambda e, l=l: e.tensor_tensor(out=lb3[:, :, l], in0=lb3[:, :, l - 1], in1=e3[:, :, l], op=ALU.add),
                   r=[B], w=[B])
        self.V(lambda e: e.tensor_scalar(out=self.omlb, in0=self.lb, scalar1=-1.0, scalar2=1.0, op0=ALU.mult, op1=ALU.add),
               r=[B], w=[B])

    def mixers(self, l, half):
        P = self.P
        self.norm(PP_MIX + l * 8, 1.0 / D)
        for tt in range(NT):
            ts = slice(tt * TT, (tt + 1) * TT)
            P.dma("sync", lambda e, ts=ts: e.dma_start(out=self.hsp[:, ts].rearrange("(c p) t -> p c t", p=128), in_=self.h[:, :, ts]),
                  r=[self.Bh[tt]], w=[self.Bhsp[tt]])
        for b in self.Bh:
            merge_into(self.hb, b)
        M = Stack(self.arena, self.H0, self.AR_BYTES)
        self.M = M
        self.ycat = []
        self.Bycat = []
        for i in range(4):
            ap, B = self.arena.at(i * 2 * T * 2, 2 * T * 2, BF16, "ycat%d" % i)
            self.ycat.append(ap.rearrange("p (c t) -> p c t", c=2))
            self.Bycat.append(B)
        def stop(tag):
            if self.stop_at == tag:
                self.stopped = True
            return self.stopped
        if stop("spill"):
            return
        self.sg(l, M)
        M.reset()
        if stop("sg"):
            return
        self.hgrn2_s1(l, M)
        M.reset_hi()
        if self.stopped or stop("hg1"):
            return
        pred = (half == 1)
        if not pred:
            self.put("hg", l, [(self.hg_Sfin, 128, 256, self.Bhg_Sfin)], F32)
        else:
            self.hg_Spred, self.Bhg_Spred = self.load_state(M, "hg", l)
        self.hgrn2_s2(l, M, pred)
        M.reset()
        if stop("hg2"):
            return
        self.ret_s1(l, M)
        M.reset_hi()
        if stop("rt1"):
            return
        if not pred:
            self.put("rt", l, [(self.rt_Sfin, 128, 256, self.Brt_Sfin)], F32)
        else:
            self.rt_Spred, self.Brt_Spred = self.load_state(M, "rt", l)
        self.ret_s2(l, M, pred)
        M.reset()
        if stop("rt2"):
            return
        self.mla_s1(l, M)
        M.reset_hi()
        if stop("ml1"):
            return
        if not pred:
            self.put("ml", l, [(self.ml_lat[:, T:2 * T], 128, T, self.Bml_lat), (self.ml_kr[:, T:2 * T], 128, T, self.Bml_kr)], BF16)
        else:
            xs, Bxs = self.xsd[("ml", l)]
            P.dma("sync", lambda e: e.dma_start(out=self.ml_lat[:, 0:T], in_=xs[0:128, 0:T]), r=[Bxs], w=[self.Bml_lat])
            P.dma("sync", lambda e: e.dma_start(out=self.ml_kr[0:32, 0:T], in_=xs[0:32, T:2 * T]), r=[Bxs], w=[self.Bml_kr])
        self.mla_s2(l, M, pred)
        M.reset()
        if stop("ml2"):
            return
        if l == 0 and half == getattr(self, "dbg_half", 0):
            for i, nm in enumerate(("ya", "yb", "yc", "yd")):
                if nm in self.dbg:
                    o = self.dout("dbg_" + nm, [256, T], BF16)
                    for c in range(2):
                        P.dma("sync", lambda e, c=c, i=i, o=o: e.dma_start(out=o[c * 128:(c + 1) * 128, :], in_=self.ycat[i][:, c, :]),
                              r=[self.Bycat[i]], w=[self.Bout])
        wo, Bwo = self.load_w(M, self.W["w_out"][l, :, :], 8, D, "w_out")
        self.wout(l, wo, Bwo)

    def put(self, name, l, pieces, dt):
        P = self.P
        XW = sum(p[2] for p in pieces)
        xs = self.nc.dram_tensor("xs_%s%d" % (name, l), [128, XW], dt).ap()
        Bxs = Buf("xs")
        self.xsd[(name, l)] = (xs, Bxs)
        c0 = 0
        for (ap, rows, width, B) in pieces:
            P.dma("sync", lambda e, ap=ap, rows=rows, c0=c0, width=width: e.dma_start(out=xs[0:rows, c0:c0 + width], in_=ap), r=[B], w=[Bxs])
            c0 += width

    def load_state(self, M, name, l):
        xs, Bxs = self.xsd[(name, l)]
        sp, Bsp = M.phi(256 * 4, F32, name + "_sp32")
        self.P.dma("sync", lambda e: e.dma_start(out=sp, in_=xs[0:128, 0:256]), r=[Bxs], w=[Bsp])
        hs, Bhs = M.plo(256 * 2, BF16, name + "_Spred")
        self.V(lambda e: e.tensor_copy(out=hs, in_=sp), r=[Bsp], w=[Bhs])
        return hs, Bhs

    def sg(self, l, M):
        P = self.P
        W_in = self.W["w_in"]
        yb, Byb = self.ycat[1], self.Bycat[1]
        uT, Bu = M.phi(2 * T * 2, BF16, "sg_u")
        uT3 = uT.rearrange("p (c t) -> p c t", c=2)
        w3, Bw = self.load_w(M, W_in[l, :, C_ZU:C_ZU + 256], 8, 256, "w_zu")
        wv, Bwv = self.load_w(M, W_in[l, :, C_ZV:C_ZV + 256], 8, 256, "w_zv")
        for cc in range(2):
            for tt in range(NT):
                ts = slice(tt * TT, (tt + 1) * TT)
                ps, Bps = self.bank()
                self.proj(ps[:, :], Bps, w3, Bw, cc * 128, 128, tt)
                self.A(lambda e, ps=ps, cc=cc, ts=ts: e.activation(out=uT3[:, cc, ts], in_=ps[:, :], func=AF.Gelu),
                       r=[Bps], w=[Bu])
        gv, Bgv = M.phi(NB * 256 * 4, F32, "sg_gv")
        gv3 = gv.rearrange("p (b c) -> p b c", b=NB)
        st, Bst = M.phi(NB * 6 * 4, F32, "sg_st")
        mv, Bmv = M.phi(NB * 2 * 4, F32, "sg_mv")
        mv3 = mv.rearrange("p (b two) -> p b two", two=2)
        rsd, Brsd = M.phi(NB * 4, F32, "sg_rsd")
        lng, Blng = M.phi(256 * 4, F32, "sg_lng")
        vtm, Bvtm = M.phi(NB * 256 * 2, BF16, "sg_vtm")
        vtm3 = vtm.rearrange("p (b c) -> p b c", b=NB)
        wsf, Bwsf = M.phi(4 * 128 * 4, F32, "sg_wsf")
        wsf3 = wsf.rearrange("p (h t) -> p h t", h=4)
        wm, Bwm = M.phi(4 * 128 * 2, BF16, "sg_wm")
        wm3 = wm.rearrange("p (h t) -> p h t", h=4)
        bsf, Bbsf = M.phi(512 * 4, F32, "sg_bsf")
        bsb, Bbsb = M.phi(512 * 2, BF16, "sg_bsb")
        P.dma("sync", lambda e: e.dma_start(out=lng, in_=self.W["sg_ln"][l, :].partition_broadcast(128)), w=[Blng])
        P.dma("sync", lambda e: e.dma_start(out=wsf3, in_=self.W["sg_wT"][l].rearrange("h s t -> s h t")), w=[Bwsf])
        P.dma("sync", lambda e: e.dma_start(out=bsf[0:1, :], in_=self.W["sg_b"][l:l + 1].rearrange("o h t -> o (h t)")), w=[Bbsf])
        caus = self.cst[:, CB_CAUS:CB_CAUS + 128]
        for hh in range(4):
            self.V(lambda e, hh=hh: e.tensor_tensor(out=wm3[:, hh, :], in0=wsf3[:, hh, :], in1=caus, op=ALU.mult),
                   r=[Bwsf, self.Bcst], w=[Bwm])
        self.V(lambda e: e.tensor_copy(out=bsb[0:1, :], in_=bsf[0:1, :]), r=[Bbsf], w=[Bbsb])
        for tb in range(NB):
            ps, Bps = self.bank()
            self.proj_tm(ps[:, 0:256], Bps, wv, Bwv, 0, 256, tb)
            self.A(lambda e, ps=ps, tb=tb: e.activation(out=gv3[:, tb, :], in_=ps[:, 0:256], func=AF.Gelu), r=[Bps], w=[Bgv])
            self.V(lambda e, tb=tb: e.bn_stats(out=st[:, tb * 6:(tb + 1) * 6], in_=gv3[:, tb, :]), r=[Bgv], w=[Bst])
            self.V(lambda e, tb=tb: e.bn_aggr(out=mv[:, tb * 2:(tb + 1) * 2], in_=st[:, tb * 6:(tb + 1) * 6]), r=[Bst], w=[Bmv])
        self.rstd_from(rsd, Brsd, mv3[:, :, 1], Bmv, 1.0)
        for tb in range(NB):
            self.V(lambda e, tb=tb: e.tensor_scalar(out=gv3[:, tb, :], in0=gv3[:, tb, :], scalar1=mv3[:, tb, 0:1],
                                                    scalar2=rsd[:, tb:tb + 1], op0=ALU.subtract, op1=ALU.mult),
                   r=[Bgv, Bmv, Brsd], w=[Bgv])
            self.G(lambda e, tb=tb: e.tensor_tensor(out=vtm3[:, tb, :], in0=gv3[:, tb, :], in1=lng, op=ALU.mult),
                   r=[Bgv, Blng], w=[Bvtm])
        onesrow = self.cb[0:1, CB_ONES:CB_ONES + 64]
        for tt in range(NT):
            ts = slice(tt * TT, (tt + 1) * TT)
            for pr in range(2):
                ps, Bps = self.bank()
                for bi in range(4):
                    tb = tt * 4 + bi
                    for hp in range(2):
                        hh = pr * 2 + hp
                        o = ps[hp * 64:(hp + 1) * 64, bi * 128:(bi + 1) * 128]
                        self.MM(o, vtm3[:, tb, hh * 64:(hh + 1) * 64], wm3[:, hh, :], True, False, r=[Bvtm, Bwm], w=[Bps], inc=False)
                        self.MM(o, onesrow, bsb[0:1, hh * 128:(hh + 1) * 128], False, True, r=[Bbsb, self.Bcb], w=[Bps],
                                inc=(bi == 3 and hp == 1))
                self.V(lambda e, ps=ps, pr=pr, ts=ts: e.tensor_tensor(out=yb[:, pr, ts], in0=ps[:, :], in1=uT3[:, pr, ts], op=ALU.mult),
                       r=[Bps, Bu], w=[Byb])
    def hgrn2_s1(self, l, M):
        P = self.P
        W_in = self.W["w_in"]
        oloc, Bol = M.plo(2 * T * 2, BF16, "hg_oloc")
        self.hg_oloc, self.Bhg_oloc = oloc.rearrange("p (c t) -> p c t", c=2), Bol
        qB, BqB = M.plo(4 * T * 2, BF16, "hg_qB")
        self.hg_qB, self.Bhg_qB = qB.rearrange("p (h t) -> p h t", h=4), BqB
        gate, Bgate = M.plo(2 * T * 2, BF16, "hg_gate")
        self.hg_gate, self.Bhg_gate = gate.rearrange("p (c t) -> p c t", c=2), Bgate
        wg3, Bwg = self.load_w(M, W_in[l, :, C_HG:C_HG + 256], 8, 256, "w_hg")
        wv3, Bwv = self.load_w(M, W_in[l, :, C_HI:C_HI + 256], 8, 256, "w_hi")
        for cc in range(2):
            for tt in range(NT):
                ts = slice(tt * TT, (tt + 1) * TT)
                ps, Bps = self.bank()
                self.proj(ps[:, :], Bps, wg3, Bwg, cc * 128, 128, tt)
                self.A(lambda e, ps=ps, cc=cc, ts=ts: e.activation(out=self.hg_gate[:, cc, ts], in_=ps[:, :], func=AF.Silu),
                       r=[Bps], w=[Bgate])
        vtm, Bvtm = M.phi(NB * 256 * 2, BF16, "hg_vtm")
        vtm3 = vtm.rearrange("p (b c) -> p b c", b=NB)
        for tb in range(NB):
            ps, Bps = self.bank()
            self.proj_tm(ps[:, 0:256], Bps, wv3, Bwv, 0, 256, tb)
            self.cp(vtm3[:, tb, :], ps[:, 0:256], [Bps], [Bvtm])
        if self.stop_at == "hg1a":
            self.stopped = True
            return
        fg, Bfg = M.phi(T * 4, F32, "hg_fg")
        bb, Bbb = M.phi(T * 4, F32, "hg_b")
        tmp, Btmp = M.phi(T * 4, F32, "hg_tmp")
        qT, BqT = M.phi(T * 2, BF16, "hg_qT")
        kT, BkT = M.phi(T * 2, BF16, "hg_kT")
        qtl, Bqtl = M.phi(T * 2, BF16, "hg_qtl")
        ktl, Bktl = M.phi(T * 2, BF16, "hg_ktl")
        khT, BkhT = M.phi(T * 2, BF16, "hg_khT")
        khtm, Bkhtm = M.phi(NB * 128 * 2, BF16, "hg_khtm")
        khtm3 = khtm.rearrange("p (b k) -> p b k", b=NB)
        ebl, Bebl = M.phi(32 * 4, F32, "hg_ebl")
        S32, BS32 = M.phi(64 * 4, F32, "hg_S32")
        Sb = [M.phi(64 * 2, BF16, "hg_Sb%d" % i) for i in range(8)]
        msk = [M.phi(128 * 2, BF16, "hg_msk%d" % i) for i in range(3)]
        rmask, Brm = M.phi(T * 2, BF16, "hg_rmask")
        onesT, Bon = M.phi(T * 2, BF16, "hg_ones")
        self.G(lambda e: e.memset(rmask, 1.0), w=[Brm])
        self.G(lambda e: e.memset(rmask.rearrange("p (c j) -> p c j", j=64)[:, :, 0:1], 0.0), w=[Brm])
        self.G(lambda e: e.memset(onesT, 1.0), w=[Bon])
        Sfin, BSfin = M.plo(256 * 4, F32, "hg_Sfin")
        self.hg_Sfin, self.Bhg_Sfin = Sfin, BSfin
        bd = self.cb[:, CB_BD64:CB_BD64 + 128]
        ident = self.cb[:, CB_IDENT:CB_IDENT + 128]
        wf_off = M.phi_off(8 * 128 * 2)
        wq_off = M.phi_off(8 * 128 * 2)
        for hh in range(4):
            pr, hp = hh // 2, hh % 2
            wf3, Bwf = self.load_w_at(wf_off, W_in[l, :, C_HF + hh * 128:C_HF + (hh + 1) * 128], 8, 128, "w_hf")
            wq3, Bwq = self.load_w_at(wq_off, W_in[l, :, C_HQ + hh * 128:C_HQ + (hh + 1) * 128], 8, 128, "w_hq")
            ci = hh * 4 + l
            for tt in range(NT):
                ts = slice(tt * TT, (tt + 1) * TT)
                ps, Bps = self.bank()
                self.proj(ps[:, :], Bps, wf3, Bwf, 0, 128, tt)
                self.A(lambda e, ps=ps, ts=ts: e.activation(out=fg[:, ts], in_=ps[:, :], func=AF.Sigmoid), r=[Bps], w=[Bfg])
                self.V(lambda e, ts=ts, ci=ci: e.tensor_scalar(out=fg[:, ts], in0=fg[:, ts], scalar1=self.omlb[:, ci:ci + 1],
                                                               scalar2=self.lb[:, ci:ci + 1], op0=ALU.mult, op1=ALU.add),
                       r=[Bfg, self.Blb], w=[Bfg])
                self.G(lambda e, ts=ts: e.tensor_scalar(out=kT[:, ts], in0=fg[:, ts], scalar1=-1.0, scalar2=1.0,
                                                        op0=ALU.mult, op1=ALU.add), r=[Bfg], w=[BkT])
                ps2, Bps2 = self.bank()
                self.proj(ps2[:, :], Bps2, wq3, Bwq, 0, 128, tt)
                self.V(lambda e, ps2=ps2, ts=ts: e.tensor_copy(out=qT[:, ts], in_=ps2[:, :]), r=[Bps2], w=[BqT])
            for tt in range(NT):
                ts = slice(tt * TT, (tt + 1) * TT)
                self.A(lambda e, ts=ts: e.activation(out=fg[:, ts], in_=fg[:, ts], func=AF.Ln), r=[Bfg, BkT], w=[Bfg])
            self.V(lambda e: e.tensor_tensor_scan(out=bb, data0=rmask, data1=fg, initial=0.0, op0=ALU.mult, op1=ALU.add),
                   r=[Brm, Bfg], w=[Bbb])
            if self.half == 1:
                self.V(lambda e: e.tensor_tensor_scan(out=tmp, data0=onesT, data1=fg, initial=0.0, op0=ALU.mult, op1=ALU.add),
                       r=[Bon, Bfg], w=[Btmp])
                self.A(lambda e: e.activation(out=tmp, in_=tmp, func=AF.Exp), r=[Btmp], w=[Btmp])
                self.V(lambda e, hh=hh: e.tensor_tensor(out=self.hg_qB[:, hh, :], in0=qT, in1=tmp, op=ALU.mult),
                       r=[BqT, Btmp], w=[BqB])
            self.A(lambda e: e.activation(out=tmp, in_=bb, func=AF.Exp), r=[Bbb, BqB], w=[Btmp])
            self.V(lambda e: e.tensor_tensor(out=qtl, in0=qT, in1=tmp, op=ALU.mult), r=[BqT, Btmp], w=[Bqtl])
            self.A(lambda e: e.activation(out=tmp, in_=bb, func=AF.Exp, scale=-1.0), r=[Bbb, Bqtl], w=[Btmp])
            self.V(lambda e: e.tensor_tensor(out=ktl, in0=kT, in1=tmp, op=ALU.mult), r=[BkT, Btmp], w=[Bktl])
            b3 = bb.rearrange("p (c j) -> p c j", j=64)
            self.A(lambda e: e.activation(out=ebl, in_=b3[:, :, 63], func=AF.Exp), r=[Bbb], w=[Bebl])
            self.V(lambda e: e.tensor_tensor(out=khT.rearrange("p (c j) -> p c j", j=64), in0=ktl.rearrange("p (c j) -> p c j", j=64),
                                             in1=ebl.rearrange("p (c o) -> p c o", o=1).broadcast_to([128, 32, 64]), op=ALU.mult),
                   r=[Bktl, Bebl], w=[BkhT])
            if self.stop_at == "hg1b":
                self.stopped = True
                return
            for tb in range(NB):
                psx, Bpt = self.bank()
                pt = psx[:, 0:64].bitcast(BF16)
                self.P.op("tensor", lambda e, pt=pt, tb=tb: e.transpose(out=pt, in_=khT[:, tb * 128:(tb + 1) * 128], identity=ident),
                          r=[BkhT, self.Bcb], w=[Bpt])
                self.cp(khtm3[:, tb, :], pt, [Bpt], [Bkhtm])
            if self.stop_at == "hg1c":
                self.stopped = True
                return
            self.V(lambda e: e.memset(S32, 0.0), w=[BS32])
            self.G(lambda e: e.memset(Sb[0][0], 0.0), w=[Sb[0][1]])
            si = 0
            import os as _os
            SK = set(_os.environ.get("KSKIP", "").split(","))
            for tg in range(NT):
                po, Bpo = self.pb[4 + (tg % 2)], self.Bpb[4 + (tg % 2)]
                pkv, Bpkv = self.pb[6], self.Bpb[6]
                for bi in range(4):
                    tb = tg * 4 + bi
                    for c in range(2):
                        cidx = bi * 2 + c
                        if "kv" in SK:
                            continue
                        self.MM(self.pb[6 + c][:, bi * 64:(bi + 1) * 64], khtm3[c * 64:(c + 1) * 64, tb, :],
                                vtm3[c * 64:(c + 1) * 64, tb, hh * 64:(hh + 1) * 64], True, True,
                                r=[Bkhtm, Bvtm], w=[self.Bpb[6 + c]], inc=(bi == 3))
                def scores(bi_):
                    tb_ = tg * 4 + bi_
                    blk_ = slice(tb_ * 128, (tb_ + 1) * 128)
                    ps_, Bps_ = self.bank()
                    m_, Bm_ = msk[tb_ % 3]
                    self.MM(ps_[:, 0:128], ktl[:, blk_], qtl[:, blk_], True, True, r=[Bktl, Bqtl], w=[Bps_])
                    self.V(lambda e, ps_=ps_, m_=m_: e.tensor_tensor(out=m_, in0=ps_[:, 0:128], in1=bd, op=ALU.mult),
                           r=[Bps_, self.Bcb], w=[Bm_])
                scores(0)
                for bi in range(4):
                    tb = tg * 4 + bi
                    if bi + 1 < 4:
                        scores(bi + 1)
                    m_ap, Bm = msk[tb % 3]
                    o = po[hp * 64:(hp + 1) * 64, bi * 128:(bi + 1) * 128]
                    self.MM(o, vtm3[:, tb, hh * 64:(hh + 1) * 64], m_ap, True, False, r=[Bvtm, Bm], w=[Bpo], inc=False)
                    for c in range(2):
                        gc = tb * 2 + c
                        s_ap, Bs = Sb[si % len(Sb)]
                        oc = po[hp * 64:(hp + 1) * 64, bi * 128 + c * 64:bi * 128 + (c + 1) * 64]
                        self.MM(oc, s_ap, qtl[:, tb * 128 + c * 64:tb * 128 + (c + 1) * 64], False, c == 1,
                                r=[Bs, Bqtl], w=[Bpo], inc=True)
                        self.V(lambda e, gc=gc, bi=bi, c=c: e.scalar_tensor_tensor(
                            out=S32, in0=S32, scalar=ebl[:, gc:gc + 1], in1=self.pb[6 + c][:, bi * 64:(bi + 1) * 64],
                            op0=ALU.mult, op1=ALU.add), r=[BS32, Bebl, self.Bpb[6 + c]], w=[BS32])
                        si += 1
                        s2, Bs2 = Sb[si % len(Sb)]
                        self.V(lambda e, s2=s2: e.tensor_copy(out=s2, in_=S32), r=[BS32], w=[Bs2])
                ts = slice(tg * TT, (tg + 1) * TT)
                if "ev" not in SK:
                    self.cp(self.hg_oloc[hp * 64:(hp + 1) * 64, pr, ts], po[hp * 64:(hp + 1) * 64, :], [Bpo], [Bol])
            self.V(lambda e, hh=hh: e.tensor_copy(out=Sfin[:, hh * 64:(hh + 1) * 64], in_=S32), r=[BS32], w=[BSfin])

    def hgrn2_s2(self, l, M, pred):
        yc, Byc = self.ycat[2], self.Bycat[2]
        units = [(pr, tt) for pr in range(2) for tt in range(NT)]
        o32 = [M.phi(TT * 4, F32, "hg2_o%d" % i) for i in range(len(units))]
        sq = [M.phi(TT * 2, BF16, "hg2_sq%d" % i) for i in range(len(units))]
        rs = [M.phi(TT * 4, F32, "hg2_rs%d" % i) for i in range(len(units))]
        bones = self.cb[:, CB_BONES:CB_BONES + 128]
        if pred:
            Sp, BSp = self.hg_Spred, self.Bhg_Spred
        for i, (pr, tt) in enumerate(units):
            ts = slice(tt * TT, (tt + 1) * TT)
            o_ap, Bo = o32[i]
            if pred:
                ps, Bps = self.bank()
                for hp in range(2):
                    hh = pr * 2 + hp
                    self.MM(ps[hp * 64:(hp + 1) * 64, :], Sp[:, hh * 64:(hh + 1) * 64], self.hg_qB[:, hh, ts], True, True,
                            r=[BSp, self.Bhg_qB], w=[Bps], inc=(hp == 1))
                self.V(lambda e, ps=ps, o_ap=o_ap, pr=pr, ts=ts: e.tensor_tensor(out=o_ap, in0=ps[:, :], in1=self.hg_oloc[:, pr, ts], op=ALU.add),
                       r=[Bps, self.Bhg_oloc], w=[Bo])
            else:
                self.V(lambda e, o_ap=o_ap, pr=pr, ts=ts: e.tensor_copy(out=o_ap, in_=self.hg_oloc[:, pr, ts]),
                       r=[self.Bhg_oloc], w=[Bo])
            sq_ap, Bsq = sq[i]
            self.G(lambda e, o_ap=o_ap, sq_ap=sq_ap: e.tensor_tensor(out=sq_ap, in0=o_ap, in1=o_ap, op=ALU.mult), r=[Bo], w=[Bsq])
        for i, (pr, tt) in enumerate(units):
            sq_ap, Bsq = sq[i]
            rs_ap, Brs = rs[i]
            ps2, Bps2 = self.bank()
            self.MM(ps2[:, :], bones, sq_ap, True, True, r=[Bsq, self.Bcb], w=[Bps2])
            self.rstd_from(rs_ap, Brs, ps2[:, :], Bps2, 1.0 / 64)
        for i, (pr, tt) in enumerate(units):
            ts = slice(tt * TT, (tt + 1) * TT)
            o_ap, Bo = o32[i]
            rs_ap, Brs = rs[i]
            gcol = PP_HGN + l * 2 + pr
            self.V(lambda e, o_ap=o_ap, rs_ap=rs_ap, gcol=gcol: e.scalar_tensor_tensor(
                out=o_ap, in0=o_ap, scalar=self.pp[:, gcol:gcol + 1], in1=rs_ap, op0=ALU.mult, op1=ALU.mult),
                r=[Bo, Brs, self.Bpp], w=[Bo])
            self.G(lambda e, o_ap=o_ap, pr=pr, ts=ts: e.tensor_tensor(out=yc[:, pr, ts], in0=o_ap, in1=self.hg_gate[:, pr, ts], op=ALU.mult),
                   r=[Bo, self.Bhg_gate], w=[Byc])

    def ret_s1(self, l, M):
        W_in, W_sw = self.W["w_in"], self.W["w_in_sw"]
        oloc, Bol = M.plo(2 * T * 2, BF16, "rt_oloc")
        self.rt_oloc, self.Brt_oloc = oloc.rearrange("p (c t) -> p c t", c=2), Bol
        qseg, Bqseg = M.plo(2 * T * 2, BF16, "rt_qseg")
        self.rt_qseg, self.Brt_qseg = qseg.rearrange("p (c t) -> p c t", c=2), Bqseg
        gate, Bgate = M.plo(2 * T * 2, BF16, "rt_gate")
        self.rt_gate, self.Brt_gate = gate.rearrange("p (c t) -> p c t", c=2), Bgate
        Sfin, BSfin = M.plo(256 * 4, F32, "rt_Sfin")
        self.rt_Sfin, self.Brt_Sfin = Sfin, BSfin
        qr, Bqr = M.phi(2 * T * 2, BF16, "rt_qr")
        qr3 = qr.rearrange("p (c t) -> p c t", c=2)
        kr, Bkr = M.phi(2 * T * 2, BF16, "rt_kr")
        kr3 = kr.rearrange("p (c t) -> p c t", c=2)
        qd, Bqd = M.phi(2 * T * 2, BF16, "rt_qd")
        qd3 = qd.rearrange("p (c t) -> p c t", c=2)
        vtm, Bvtm = M.phi(NB * 256 * 2, BF16, "rt_vtm")
        vtm3 = vtm.rearrange("p (b c) -> p b c", b=NB)
        kdtm, Bkdtm = M.phi(NB * 256 * 2, BF16, "rt_kdtm")
        kdtm3 = kdtm.rearrange("p (b c) -> p b c", b=NB)
        t1 = [M.phi(TT * 4, F32, "rt_t1%d" % i) for i in range(2)]
        t2 = [M.phi(TT * 4, F32, "rt_t2%d" % i) for i in range(2)]
        S32, BS32 = M.phi(256 * 4, F32, "rt_S32")
        Sb = [M.phi(256 * 2, BF16, "rt_Sb%d" % i) for i in range(4)]
        msk = [M.phi(128 * 2, BF16, "rt_msk%d" % i) for i in range(8)]
        wg3, Bwg = self.load_w(M, W_in[l, :, C_RG:C_RG + 256], 8, 256, "w_rg")
        wv3, Bwv = self.load_w(M, W_in[l, :, C_RV:C_RV + 256], 8, 256, "w_rv")
        wq3, Bwq = self.load_w(M, W_in[l, :, C_RQ:C_RQ + 256], 8, 256, "w_rq")
        wqs3, Bwqs = self.load_w(M, W_sw[l, :, 32:288], 8, 256, "w_rqs")
        wk3, Bwk = self.load_w(M, W_in[l, :, C_RK:C_RK + 256], 8, 256, "w_rk")
        wks3, Bwks = self.load_w(M, W_sw[l, :, 288:544], 8, 256, "w_rks")
        for cc in range(2):
            for tt in range(NT):
                ts = slice(tt * TT, (tt + 1) * TT)
                ps, Bps = self.bank()
                self.proj(ps[:, :], Bps, wg3, Bwg, cc * 128, 128, tt)
                self.A(lambda e, ps=ps, cc=cc, ts=ts: e.activation(out=self.rt_gate[:, cc, ts], in_=ps[:, :], func=AF.Silu),
                       r=[Bps], w=[Bgate])
        for tb in range(NB):
            ps, Bps = self.bank()
            self.proj_tm(ps[:, 0:256], Bps, wv3, Bwv, 0, 256, tb)
            self.cp(vtm3[:, tb, :], ps[:, 0:256], [Bps], [Bvtm])
        ri = 0
        for (w3, Bw, ws3, Bws, dst3, Bdst) in ((wq3, Bwq, wqs3, Bwqs, qr3, Bqr), (wk3, Bwk, wks3, Bwks, kr3, Bkr)):
            for cc in range(2):
                for tt in range(NT):
                    ts = slice(tt * TT, (tt + 1) * TT)
                    ps, Bps = self.bank()
                    self.proj(ps[:, :], Bps, w3, Bw, cc * 128, 128, tt)
                    ps2, Bps2 = self.bank()
                    self.proj(ps2[:, :], Bps2, ws3, Bws, cc * 128, 128, tt)
                    a1, Ba1 = t1[ri % 2]
                    a2, Ba2 = t2[ri % 2]
                    ri += 1
                    self.V(lambda e, ps=ps, a1=a1, ts=ts: e.tensor_tensor(out=a1, in0=ps[:, :], in1=self.cosR[:, ts], op=ALU.mult),
                           r=[Bps, self.Btab], w=[Ba1])
                    self.V(lambda e, ps2=ps2, a2=a2, ts=ts: e.tensor_tensor(out=a2, in0=ps2[:, :], in1=self.sinR[:, ts], op=ALU.mult),
                           r=[Bps2, self.Btab], w=[Ba2])
                    self.G(lambda e, a1=a1, a2=a2, dst3=dst3, cc=cc, ts=ts: e.tensor_tensor(out=dst3[:, cc, ts], in0=a1, in1=a2, op=ALU.add),
                           r=[Ba1, Ba2], w=[Bdst])
        for pr in range(2):
            qdm = self.cb[:, CB_QD + pr * 128:CB_QD + (pr + 1) * 128]
            sgd = self.cb[:, CB_SEGD + pr * 16:CB_SEGD + (pr + 1) * 16]
            self.G(lambda e, pr=pr, qdm=qdm: e.tensor_tensor(
                out=qd3[:, pr, :].rearrange("p (b j) -> p b j", j=128), in0=qr3[:, pr, :].rearrange("p (b j) -> p b j", j=128),
                in1=qdm.rearrange("p (o j) -> p o j", o=1).broadcast_to([128, NB, 128]), op=ALU.mult),
                r=[Bqr, self.Bcb], w=[Bqd])
            self.G(lambda e, pr=pr, sgd=sgd: e.tensor_tensor(
                out=self.rt_qseg[:, pr, :].rearrange("p (b j) -> p b j", j=128), in0=qd3[:, pr, :].rearrange("p (b j) -> p b j", j=128),
                in1=sgd.rearrange("p (b o) -> p b o", o=1).broadcast_to([128, NB, 128]), op=ALU.mult),
                r=[Bqd, self.Bcb], w=[Bqseg])
        ident = self.cb[:, CB_IDENT:CB_IDENT + 128]
        ti = 0
        for pr in range(2):
            for tb in range(NB):
                psx, Bpt = self.bank()
                pt = psx[:, 0:64].bitcast(BF16)
                self.P.op("tensor", lambda e, pt=pt, tb=tb, pr=pr: e.transpose(out=pt, in_=kr3[:, pr, tb * 128:(tb + 1) * 128], identity=ident),
                          r=[Bkr, self.Bcb], w=[Bpt])
                for hp in range(2):
                    hh = pr * 2 + hp
                    kc = self.cst[:, CS_KDEC + hh:CS_KDEC + hh + 1]
                    if hp == 0:
                        self.V(lambda e, pt=pt, tb=tb, hh=hh, hp=hp, kc=kc: e.tensor_scalar(
                            out=kdtm3[:, tb, hh * 64:(hh + 1) * 64], in0=pt[:, hp * 64:(hp + 1) * 64], scalar1=kc, scalar2=None, op0=ALU.mult),
                            r=[Bpt, self.Bcst], w=[Bkdtm])
                    else:
                        self.A(lambda e, pt=pt, tb=tb, hh=hh, hp=hp, kc=kc: e.activation(
                            out=kdtm3[:, tb, hh * 64:(hh + 1) * 64], in_=pt[:, hp * 64:(hp + 1) * 64], func=AF.Copy, scale=kc),
                            r=[Bpt, self.Bcst], w=[Bkdtm])
        self.V(lambda e: e.memset(S32, 0.0), w=[BS32])
        self.G(lambda e: e.memset(Sb[0][0], 0.0), w=[Sb[0][1]])
        def rscores(tb_):
            blk_ = slice(tb_ * 128, (tb_ + 1) * 128)
            for hh_ in range(4):
                pr_, hp_ = hh_ // 2, hh_ % 2
                rows_ = slice(hp_ * 64, (hp_ + 1) * 64)
                ps_, Bps_ = self.bank()
                self.MM(ps_[:, 0:128], kr3[rows_, pr_, blk_], qr3[rows_, pr_, blk_], True, True, r=[Bkr, Bqr], w=[Bps_])
                m_, Bm_ = msk[(tb_ % 2) * 4 + hh_]
                dm_ = self.cst[:, CS_DM + hh_ * 128:CS_DM + (hh_ + 1) * 128]
                self.V(lambda e, ps_=ps_, m_=m_, dm_=dm_: e.tensor_tensor(out=m_, in0=ps_[:, 0:128], in1=dm_, op=ALU.mult),
                       r=[Bps_, self.Bcst], w=[Bm_])
        rscores(0)
        for tg in range(NT):
            pos_ = [(self.pb[4], self.Bpb[4]), (self.pb[5], self.Bpb[5])]
            for bi in range(4):
                tb = tg * 4 + bi
                blk = slice(tb * 128, (tb + 1) * 128)
                s_ap, Bs = Sb[tb % len(Sb)]
                s2, Bs2 = Sb[(tb + 1) % len(Sb)]
                pkv, Bpkv = self.pb[6 + (tb % 2)], self.Bpb[6 + (tb % 2)]
                if tb + 1 < NB:
                    rscores(tb + 1)
                for hh in range(4):
                    pr, hp = hh // 2, hh % 2
                    rows = slice(hp * 64, (hp + 1) * 64)
                    po, Bpo = pos_[pr]
                    m_ap, Bm = msk[(tb % 2) * 4 + hh]
                    o = po[rows, bi * 128:(bi + 1) * 128]
                    self.MM(o, vtm3[:, tb, hh * 64:(hh + 1) * 64], m_ap, True, False, r=[Bvtm, Bm], w=[Bpo], inc=False)
                    self.MM(o, s_ap[rows, hh * 64:(hh + 1) * 64], qd3[rows, pr, blk], False, True, r=[Bs, Bqd], w=[Bpo])
                    self.MM(pkv[rows, hh * 64:(hh + 1) * 64], kdtm3[:, tb, hh * 64:(hh + 1) * 64], vtm3[:, tb, hh * 64:(hh + 1) * 64],
                            True, True, r=[Bkdtm, Bvtm], w=[Bpkv], inc=(hh == 3))
                for hh in range(4):
                    hp = hh % 2
                    rows = slice(hp * 64, (hp + 1) * 64)
                    g128 = RET_G[hh] ** 128
                    self.V(lambda e, rows=rows, hh=hh, g128=g128, pkv=pkv: e.scalar_tensor_tensor(
                        out=S32[rows, hh * 64:(hh + 1) * 64], in0=S32[rows, hh * 64:(hh + 1) * 64], scalar=g128,
                        in1=pkv[rows, hh * 64:(hh + 1) * 64], op0=ALU.mult, op1=ALU.add), r=[BS32, Bpkv], w=[BS32])
                self.V(lambda e, s2=s2: e.tensor_copy(out=s2, in_=S32), r=[BS32], w=[Bs2])
            ts = slice(tg * TT, (tg + 1) * TT)
            for pr in range(2):
                po, Bpo = pos_[pr]
                self.cp(self.rt_oloc[:, pr, ts], po[:, :], [Bpo], [Bol])
        self.V(lambda e: e.tensor_copy(out=Sfin, in_=S32), r=[BS32], w=[BSfin])

    def ret_s2(self, l, M, pred):
        yd, Byd = self.ycat[3], self.Bycat[3]
        units = [(pr, tt) for pr in range(2) for tt in range(NT)]
        n = len(units)
        o32 = [M.phi(TT * 4, F32, "rt2_o%d" % i) for i in range(n)]
        ob = [M.phi(TT * 2, BF16, "rt2_ob%d" % i) for i in range(n)]
        sq = [M.phi(TT * 2, BF16, "rt2_sq%d" % i) for i in range(n)]
        rs = [M.phi(TT * 4, F32, "rt2_rs%d" % i) for i in range(n)]
        bones = self.cb[:, CB_BONES:CB_BONES + 128]
        if pred:
            Sp, BSp = self.rt_Spred, self.Brt_Spred
        for i, (pr, tt) in enumerate(units):
            ts = slice(tt * TT, (tt + 1) * TT)
            o_ap, Bo = o32[i]
            ob_ap, Bob = ob[i]
            if not pred:
                self.V(lambda e, o_ap=o_ap, pr=pr, ts=ts: e.tensor_copy(out=o_ap, in_=self.rt_oloc[:, pr, ts]),
                       r=[self.Brt_oloc], w=[Bo])
            else:
                for hp in range(2):
                    hh = pr * 2 + hp
                    rows = slice(hp * 64, (hp + 1) * 64)
                    ps, Bps = self.bank()
                    self.MM(ps[rows, :], Sp[rows, hh * 64:(hh + 1) * 64], self.rt_qseg[rows, pr, ts], True, True,
                            r=[BSp, self.Brt_qseg], w=[Bps])
                    self.V(lambda e, ps=ps, o_ap=o_ap, pr=pr, ts=ts, rows=rows: e.tensor_tensor(
                        out=o_ap[rows, :], in0=ps[rows, :], in1=self.rt_oloc[rows, pr, ts], op=ALU.add),
                        r=[Bps, self.Brt_oloc], w=[Bo])
            self.G(lambda e, o_ap=o_ap, ob_ap=ob_ap: e.tensor_copy(out=ob_ap, in_=o_ap), r=[Bo], w=[Bob])
        for i, (pr, tt) in enumerate(units):
            o_ap, Bo = o32[i]
            ob_ap, Bob = ob[i]
            sq_ap, Bsq = sq[i]
            pm, Bpm = self.bank()
            self.MM(pm[:, :], bones, ob_ap, True, True, r=[Bob, self.Bcb], w=[Bpm])
            self.V(lambda e, pm=pm, o_ap=o_ap: e.scalar_tensor_tensor(out=o_ap, in0=pm[:, :], scalar=-1.0 / 64, in1=o_ap,
                                                                      op0=ALU.mult, op1=ALU.add), r=[Bpm, Bo], w=[Bo])
            self.G(lambda e, o_ap=o_ap, sq_ap=sq_ap: e.tensor_tensor(out=sq_ap, in0=o_ap, in1=o_ap, op=ALU.mult), r=[Bo], w=[Bsq])
        for i, (pr, tt) in enumerate(units):
            sq_ap, Bsq = sq[i]
            rs_ap, Brs = rs[i]
            ps2, Bps2 = self.bank()
            self.MM(ps2[:, :], bones, sq_ap, True, True, r=[Bsq, self.Bcb], w=[Bps2])
            self.rstd_from(rs_ap, Brs, ps2[:, :], Bps2, 1.0 / 64)
        for i, (pr, tt) in enumerate(units):
            ts = slice(tt * TT, (tt + 1) * TT)
            o_ap, Bo = o32[i]
            rs_ap, Brs = rs[i]
            gcol = PP_RTN + l * 2 + pr
            self.V(lambda e, o_ap=o_ap, rs_ap=rs_ap, gcol=gcol: e.scalar_tensor_tensor(
                out=o_ap, in0=o_ap, scalar=self.pp[:, gcol:gcol + 1], in1=rs_ap, op0=ALU.mult, op1=ALU.mult),
                r=[Bo, Brs, self.Bpp], w=[Bo])
            self.G(lambda e, o_ap=o_ap, pr=pr, ts=ts: e.tensor_tensor(out=yd[:, pr, ts], in0=o_ap, in1=self.rt_gate[:, pr, ts], op=ALU.mult),
                   r=[Bo, self.Brt_gate], w=[Byd])

    def mla_s1(self, l, M):
        W_in, W_sw = self.W["w_in"], self.W["w_in_sw"]
        qT, BqT = M.plo(4 * T * 2, BF16, "ml_qT")
        self.ml_qT, self.Bml_qT = qT.rearrange("p (h t) -> p h t", h=4), BqT
        lat, Blat = M.plo(2 * T * 2, BF16, "ml_lat")
        self.ml_lat, self.Bml_lat = lat, Blat
        kr, Bkr = M.plo(2 * T * 2, BF16, "ml_kr")
        self.ml_kr, self.Bml_kr = kr, Bkr
        self.G(lambda e: e.memset(kr, 0.0), w=[Bkr])
        wcq, Bwcq = self.load_w(M, W_in[l, :, C_CQ:C_CQ + 256], 8, 256, "w_cq")
        wckv, Bwckv = self.load_w(M, W_in[l, :, C_CKV:C_CKV + 128], 8, 128, "w_ckv")
        wkpe, Bwkpe = self.load_w(M, W_in[l, :, C_KPE:C_KPE + 32], 8, 32, "w_kpe")
        wkpes, Bwkpes = self.load_w(M, W_sw[l, :, 0:32], 8, 32, "w_kpes")
        wuq, Bwuq = self.load_w(M, self.W["w_uq"][l, :, :], 2, 384, "w_uq")
        wuqs, Bwuqs = self.load_w(M, self.W["w_uq_sw"][l, :, :], 2, 128, "w_uqs")
        cq = [M.phi(2 * TT * 4, F32, "ml_cq%d" % i) for i in range(2)]
        sq = [M.phi(2 * TT * 2, BF16, "ml_sq%d" % i) for i in range(2)]
        rs = [M.phi(TT * 4, F32, "ml_rs%d" % i) for i in range(2)]
        cqn = [M.phi(2 * TT * 2, BF16, "ml_cqn%d" % i) for i in range(2)]
        t1 = [M.phi(TT * 4, F32, "ml_t1%d" % i) for i in range(2)]
        t2 = [M.phi(TT * 4, F32, "ml_t2%d" % i) for i in range(2)]
        ones = self.cb[:, CB_ONES:CB_ONES + 128]
        ri = 0
        for tt in range(NT):
            ts = slice(tt * TT, (tt + 1) * TT)
            cq_ap, Bcq = cq[tt % 2]
            cq3 = cq_ap.rearrange("p (c t) -> p c t", c=2)
            sq_ap, Bsq = sq[tt % 2]
            sq3 = sq_ap.rearrange("p (c t) -> p c t", c=2)
            rs_ap, Brs = rs[tt % 2]
            cqn_ap, Bcqn = cqn[tt % 2]
            cqn3 = cqn_ap.rearrange("p (c t) -> p c t", c=2)
            for cc in range(2):
                ps, Bps = self.bank()
                self.proj(ps[:, :], Bps, wcq, Bwcq, cc * 128, 128, tt)
                self.cp(cq3[:, cc, :], ps[:, :], [Bps], [Bcq])
            self.G(lambda e, sq_ap=sq_ap, cq_ap=cq_ap: e.tensor_tensor(out=sq_ap, in0=cq_ap, in1=cq_ap, op=ALU.mult), r=[Bcq], w=[Bsq])
            ps, Bps = self.bank()
            for cc in range(2):
                self.MM(ps[:, :], ones, sq3[:, cc, :], cc == 0, cc == 1, r=[Bsq, self.Bcb], w=[Bps], inc=(cc == 1))
            self.rstd_from(rs_ap, Brs, ps[:, :], Bps, 1.0 / 256)
            for cc in range(2):
                gcol = PP_QN + l * 2 + cc
                self.V(lambda e, cc=cc, gcol=gcol, cqn3=cqn3, cq3=cq3, rs_ap=rs_ap: e.scalar_tensor_tensor(
                    out=cqn3[:, cc, :], in0=cq3[:, cc, :], scalar=self.pp[:, gcol:gcol + 1], in1=rs_ap, op0=ALU.mult, op1=ALU.mult),
                    r=[Bcq, Brs, self.Bpp], w=[Bcqn])
            for hh in range(4):
                psA, BpsA = self.bank()
                for kc in range(2):
                    self.MM(psA[0:96, :], wuq[:, kc, hh * 96:(hh + 1) * 96], cqn3[:, kc, :], kc == 0, kc == 1,
                            r=[Bwuq, Bcqn], w=[BpsA], inc=(kc == 1))
                psB, BpsB = self.bank()
                for kc in range(2):
                    self.MM(psB[64:96, :], wuqs[:, kc, hh * 32:(hh + 1) * 32], cqn3[:, kc, :], kc == 0, kc == 1,
                            r=[Bwuqs, Bcqn], w=[BpsB], inc=(kc == 1))
                self.cp(self.ml_qT[0:64, hh, ts], psA[0:64, :], [BpsA], [BqT])
                a1, Ba1 = t1[ri % 2]
                a2, Ba2 = t2[ri % 2]
                ri += 1
                self.V(lambda e, psA=psA, a1=a1, ts=ts: e.tensor_tensor(out=a1[64:96, :], in0=psA[64:96, :], in1=self.cosM[64:96, ts], op=ALU.mult),
                       r=[BpsA, self.Btab], w=[Ba1])
                self.V(lambda e, psB=psB, a2=a2, ts=ts: e.tensor_tensor(out=a2[64:96, :], in0=psB[64:96, :], in1=self.sinM[64:96, ts], op=ALU.mult),
                       r=[BpsB, self.Btab], w=[Ba2])
                self.V(lambda e, a1=a1, a2=a2, hh=hh, ts=ts: e.tensor_tensor(out=self.ml_qT[64:96, hh, ts], in0=a1[64:96, :], in1=a2[64:96, :], op=ALU.add),
                       r=[Ba1, Ba2], w=[BqT])
            ck_ap, Bck = cq[(tt + 1) % 2]
            ck = ck_ap[:, 0:TT]
            ps, Bps = self.bank()
            self.proj(ps[:, :], Bps, wckv, Bwckv, 0, 128, tt)
            self.cp(ck, ps[:, :], [Bps], [Bck])
            sk_ap, Bsk = sq[(tt + 1) % 2]
            sk = sk_ap[:, 0:TT]
            self.G(lambda e, sk=sk, ck=ck: e.tensor_tensor(out=sk, in0=ck, in1=ck, op=ALU.mult), r=[Bck], w=[Bsk])
            ps2, Bps2 = self.bank()
            self.MM(ps2[:, :], ones, sk, True, True, r=[Bsk, self.Bcb], w=[Bps2])
            rk_ap, Brk = rs[(tt + 1) % 2]
            self.rstd_from(rk_ap, Brk, ps2[:, :], Bps2, 1.0 / 128)
            gcol = PP_KVN + l
            self.V(lambda e, ck=ck, rk_ap=rk_ap, gcol=gcol, tt=tt: e.scalar_tensor_tensor(
                out=lat[:, T + tt * TT:T + (tt + 1) * TT], in0=ck, scalar=self.pp[:, gcol:gcol + 1], in1=rk_ap, op0=ALU.mult, op1=ALU.mult),
                r=[Bck, Brk, self.Bpp], w=[Blat])
            psA, BpsA = self.bank()
            self.proj(psA[0:32, :], BpsA, wkpe, Bwkpe, 0, 32, tt)
            psB, BpsB = self.bank()
            self.proj(psB[0:32, :], BpsB, wkpes, Bwkpes, 0, 32, tt)
            a1, Ba1 = t1[ri % 2]
            a2, Ba2 = t2[ri % 2]
            ri += 1
            self.V(lambda e, psA=psA, a1=a1, ts=ts: e.tensor_tensor(out=a1[0:32, :], in0=psA[0:32, :], in1=self.cosM[0:32, ts], op=ALU.mult),
                   r=[BpsA, self.Btab], w=[Ba1])
            self.V(lambda e, psB=psB, a2=a2, ts=ts: e.tensor_tensor(out=a2[0:32, :], in0=psB[0:32, :], in1=self.sinM[0:32, ts], op=ALU.mult),
                   r=[BpsB, self.Btab], w=[Ba2])
            self.V(lambda e, a1=a1, a2=a2, tt=tt: e.tensor_tensor(out=kr[0:32, T + tt * TT:T + (tt + 1) * TT], in0=a1[0:32, :], in1=a2[0:32, :], op=ALU.add),
                   r=[Ba1, Ba2], w=[Bkr])

    def cc_gather(self, xs, Bxs, xr, Bxr):
        P = self.P
        groups = [[0, 1], [2, 3], [4, 5], [6, 7]]
        key = ("cc",)
        if key not in P.sems:
            P._mksem(key, 16)
        waits = P._deps("gpsimd", [Bxs], [Bxr])
        seq = P.cnt[key] + 1
        P.cnt[key] = seq
        P._mark(key, seq, [Bxs], [Bxr])
        P.ops["gpsimd"].append((lambda e: e.collective_compute("AllGather", ALU.bypass, replica_groups=groups,
                                                               ins=[xs[:, :]], outs=[xr[:, :]]), waits, key))

    def mla_s2(self, l, M, pred):
        ya, Bya = self.ycat[0], self.Bycat[0]
        CT = 2 * T
        wukv, Bwukv = self.load_w(M, self.W["w_ukv"][l, :, :], 1, 512, "w_ukv")
        KT = [M.phi(CT * 2, BF16, "ml_KT%d" % i) for i in range(2)]
        VH = [M.phi(32 * 128 * 2, BF16, "ml_VH%d" % i) for i in range(2)]
        PT = [M.phi(TT * 2, BF16, "ml_PT%d" % i) for i in range(5)]
        RC = [M.phi(TT * 4, F32, "ml_RC%d" % i) for i in range(2)]
        for i in range(2):
            v3 = VH[i][0].rearrange("p (k c) -> p k c", k=32)
            self.G(lambda e, v3=v3: e.memset(v3[:, :, 64:128], 1.0), w=[VH[i][1]])
        caus = self.cb[:, CB_CAUS:CB_CAUS + 128]
        SC = 96.0 ** -0.5
        lat, Blat, kr, Bkr = self.ml_lat, self.Bml_lat, self.ml_kr, self.Bml_kr
        pi = 0
        qi = 0
        for hh in range(4):
            pr, hp = hh // 2, hh % 2
            kt, Bkt = KT[hh % 2]
            vh, Bvh = VH[hh % 2]
            vh3 = vh.rearrange("p (k c) -> p k c", k=32)
            for ct in range(0 if pred else 4, CT // TT):
                ps, Bps = self.bank()
                self.MM(ps[0:64, :], wukv[:, 0, hh * 128:hh * 128 + 64], lat[:, ct * TT:(ct + 1) * TT], True, True,
                        r=[Bwukv, Blat], w=[Bps])
                self.cp(kt[0:64, ct * TT:(ct + 1) * TT], ps[0:64, :], [Bps], [Bkt])
            k0 = 0 if pred else T
            self.V(lambda e, kt=kt, k0=k0: e.tensor_copy(out=kt[64:96, k0:], in_=kr[0:32, k0:]), r=[Bkr], w=[Bkt])
            for kg in range(0 if pred else 2, 4):
                ps, Bps = self.bank()
                for j in range(8):
                    kti = kg * 8 + j
                    self.MM(ps[:, j * 64:(j + 1) * 64], lat[:, kti * 128:(kti + 1) * 128], wukv[:, 0, hh * 128 + 64:hh * 128 + 128],
                            True, True, r=[Bwukv, Blat], w=[Bps], inc=(j == 7))
                self.cp(vh3[:, kg * 8:(kg + 1) * 8, 0:64], ps[:, :].rearrange("p (k c) -> p k c", k=8), [Bps], [Bvh])
            for Q in range(NT):
                po, Bpo = self.pb[4 + (qi % 2)], self.Bpb[4 + (qi % 2)]
                qi += 1
                keys = [(k_, 0, False, False) for k_ in range(16)] if pred else []
                for j in range(4 * Q + 4):
                    keys.append((16 + j, max(0, j - 4 * Q) * 128, False, j >= 4 * Q))
                LA = 2
                pend = []

                def emit_pv(item, first, last):
                    kti_, c0_, pt_, Bpt_ = item
                    self.MM(po[:, c0_:TT], vh3[:, kti_, :], pt_[:, c0_:TT], first, last, r=[Bvh, Bpt_], w=[Bpo])
                npv = 0
                for idx, (kti, c0, usebias, diag) in enumerate(keys):
                    ps, Bps = self.bank()
                    self.MM(ps[:, c0:TT], kt[0:96, kti * 128:(kti + 1) * 128], self.ml_qT[0:96, hh, Q * TT + c0:(Q + 1) * TT], True, True,
                            r=[Bkt, self.Bml_qT], w=[Bps])
                    pt, Bpt = PT[pi % len(PT)]
                    pi += 1
                    self.A(lambda e, ps=ps, pt=pt, c0=c0: e.activation(out=pt[:, c0:TT], in_=ps[:, c0:TT], func=AF.Exp, scale=SC),
                           r=[Bps], w=[Bpt])
                    if diag:
                        self.G(lambda e, pt=pt, c0=c0: e.tensor_tensor(out=pt[:, c0:c0 + 128], in0=pt[:, c0:c0 + 128], in1=caus, op=ALU.mult),
                               r=[Bpt, self.Bcb], w=[Bpt])
                    pend.append((kti, c0, pt, Bpt))
                    if len(pend) > LA:
                        emit_pv(pend.pop(0), npv == 0, False)
                        npv += 1
                while pend:
                    emit_pv(pend.pop(0), npv == 0, len(pend) == 0)
                    npv += 1
                rc, Brc = RC[qi % 2]
                self.V(lambda e, rc=rc, po=po: e.reciprocal(out=rc[64:128, :], in_=po[64:128, :]), r=[Bpo], w=[Brc])
                self.V(lambda e, rc=rc, po=po, hp=hp, pr=pr, Q=Q: e.tensor_tensor(
                    out=ya[hp * 64:(hp + 1) * 64, pr, Q * TT:(Q + 1) * TT], in0=po[0:64, :], in1=rc[64:128, :], op=ALU.mult),
                    r=[Bpo, Brc], w=[Bya])

    def wout(self, l, wo, Bwo):
        P = self.P
        self.h_ap, self.hb = self.arena.at(self.H0, self.H_BYTES, F32, "h")
        self.h = self.h_ap.rearrange("p (c t) -> p c t", c=8)
        self.Bh = [Buf("h%d" % i) for i in range(NT)]
        for b in self.Bh:
            merge_into(b, self.hb)
        for tt in range(NT):
            ts = slice(tt * TT, (tt + 1) * TT)
            P.dma("sync", lambda e, ts=ts: e.dma_start(out=self.h[:, :, ts], in_=self.hsp[:, ts].rearrange("(c p) t -> p c t", p=128)),
                  r=[self.Bhsp[tt]], w=[self.Bh[tt]])
        for tt in range(NT):
            ts = slice(tt * TT, (tt + 1) * TT)
            for dc in range(8):
                ps, Bps = self.bank()
                for kc in range(8):
                    self.MM(ps[:, :], wo[:, kc, dc * 128:(dc + 1) * 128], self.ycat[kc // 2][:, kc % 2, ts], kc == 0, kc == 7,
                            r=[Bwo, self.Bycat[kc // 2]], w=[Bps], inc=(kc == 7))
                self.V(lambda e, ps=ps, dc=dc, ts=ts: e.tensor_tensor(out=self.h[:, dc, ts], in0=ps[:, :], in1=self.h[:, dc, ts], op=ALU.add),
                       r=[Bps, self.Bh[tt]], w=[self.Bh[tt]])

    def ple(self, l, pT, c0):
        P = self.P
        self.norm(PP_PLE + l * 8, 1.0 / D)
        stk = self.stk
        stk.reset_hi()
        wpg, Bwpg = self.load_w(stk, self.W["ple_w_gate"][l, :, :], 8, D, "w_pg")
        wpp, Bwpp = self.load_w(stk, self.W["ple_w_proj"][l, :, :], 2, D, "w_pp")
        pt = [stk.phi(2 * TT * 2, BF16, "ple_pT%d" % i) for i in range(2)]
        gt = [stk.phi(TT * 4, F32, "ple_g%d" % i) for i in range(2)]
        tm = [stk.phi(TT * 4, F32, "ple_t%d" % i) for i in range(2)]
        i = 0
        for tt in range(NT):
            ts = slice(tt * TT, (tt + 1) * TT)
            p_ap, Bp = pt[tt % 2]
            p3 = p_ap.rearrange("p (c t) -> p c t", c=2)
            P.dma("gpsimd", lambda e, p3=p3, ts=ts: e.dma_start(out=p3, in_=pT[l, :, c0 + ts.start:c0 + ts.stop].rearrange("(c p) t -> p c t", p=128)), w=[Bp])
            for dc in range(8):
                psg, Bpsg = self.bank()
                for kc in range(8):
                    self.MM(psg[:, :], wpg[:, kc, dc * 128:(dc + 1) * 128], self.xn[:, kc, ts], kc == 0, kc == 7,
                            r=[Bwpg, self.Bxn[tt]], w=[Bpsg], inc=(kc == 7))
                g_ap, Bg = gt[i % 2]
                t_ap, Bt = tm[i % 2]
                i += 1
                self.A(lambda e, psg=psg, g_ap=g_ap: e.activation(out=g_ap, in_=psg[:, :], func=AF.Sigmoid), r=[Bpsg], w=[Bg])
                psp, Bpsp = self.bank()
                for kc in range(2):
                    self.MM(psp[:, :], wpp[:, kc, dc * 128:(dc + 1) * 128], p3[:, kc, :], kc == 0, kc == 1,
                            r=[Bwpp, Bp], w=[Bpsp], inc=(kc == 1))
                self.V(lambda e, psp=psp, g_ap=g_ap, t_ap=t_ap: e.tensor_tensor(out=t_ap, in0=psp[:, :], in1=g_ap, op=ALU.mult),
                       r=[Bpsp, Bg], w=[Bt])
                self.G(lambda e, t_ap=t_ap, dc=dc, ts=ts: e.tensor_tensor(out=self.h[:, dc, ts], in0=self.h[:, dc, ts], in1=t_ap, op=ALU.add),
                       r=[Bt, self.Bh[tt]], w=[self.Bh[tt]])

    def final_out(self, outT, c0):
        stk = self.stk
        stk.reset_hi()
        ones = self.cb[:, CB_ONES:CB_ONES + 128]
        sq, Bsq = stk.phi(8 * TT * 2, BF16, "sq")
        sq3 = sq.rearrange("p (c t) -> p c t", c=8)
        rs, Brs = stk.phi(TT * 4, F32, "rstd")
        ob = [stk.phi(TT * 4, F32, "ob%d" % i) for i in range(4)]
        oi = 0
        for tt in range(NT):
            ts = slice(tt * TT, (tt + 1) * TT)
            bank = 5 + (tt % 2)
            ps, Bps = self.pb[bank], self.Bpb[bank]
            for ch in range(8):
                if ch % 2 == 0:
                    self.G(lambda e, ch=ch, ts=ts: e.tensor_tensor(out=sq3[:, ch, :], in0=self.h[:, ch, ts],
                                                                  in1=self.h[:, ch, ts], op=ALU.mult),
                           r=[self.Bh[tt]], w=[Bsq])
                else:
                    self.A(lambda e, ch=ch, ts=ts: e.activation(out=sq3[:, ch, :], in_=self.h[:, ch, ts], func=AF.Square),
                           r=[self.Bh[tt]], w=[Bsq])
            for ch in range(8):
                self.MM(ps[:, :], ones, sq3[:, ch, :], ch == 0, ch == 7, r=[Bsq, self.Bcb], w=[Bps], inc=(ch == 7))
            epsb = self.cst[:, CS_EPS:CS_EPS + 1]
            self.A(lambda e, ps=ps: e.activation(out=rs, in_=ps[:, :], func=AF.Ln, bias=epsb, scale=1.0 / D),
                   r=[Bps, self.Bcst], w=[Brs])
            self.A(lambda e: e.activation(out=rs, in_=rs, func=AF.Exp, scale=-0.5), r=[Brs], w=[Brs])
            for ch in range(8):
                o_ap, Bo = ob[oi % 4]
                oi += 1
                self.V(lambda e, ch=ch, ts=ts, o_ap=o_ap: e.scalar_tensor_tensor(
                    out=o_ap, in0=self.h[:, ch, ts], scalar=self.pp[:, PP_FINAL + ch:PP_FINAL + ch + 1],
                    in1=rs, op0=ALU.mult, op1=ALU.mult), r=[self.Bh[tt], Brs, self.Bpp], w=[Bo])
                self.P.dma("sync", lambda e, ch=ch, ts=ts, o_ap=o_ap: e.dma_start(
                    out=outT[ch * 128:(ch + 1) * 128, c0 + ts.start:c0 + ts.stop], in_=o_ap), r=[Bo], w=[self.Bout])


PP_FFN1 = 0
PP_MIX = PP_FFN1 + L * 8
PP_FFN2 = PP_MIX + L * 8
PP_PLE = PP_FFN2 + L * 8
PP_FINAL = PP_PLE + L * 8
PP_QN = PP_FINAL + 8
PP_KVN = PP_QN + L * 2
PP_HGN = PP_KVN + L
PP_RTN = PP_HGN + L * 2
PP_LBL = PP_RTN + L * 2
PP_FLAG = PP_LBL + 4 * L
PPW = PP_FLAG + 1

CB_ONES = 0
CB_IDENT = 128
CB_BONES = 256
CB_CAUS = 384
CB_BD64 = 512
CB_QD = 640
CB_SEGD = 896
CSTBW = 928
CS_EPS = 928
CS_INVM = 929
CS_INVR = 930
CS_SGNM = 931
CS_SGNR = 932
CS_KDEC = 933
CS_DM = 937
CSTW = CS_DM + 512
RET_G = [1.0 - 2.0 ** (-(5.0 + h)) for h in range(4)]


def _consts():
    c = np.zeros((128, CSTW), np.float64)
    c[:, CB_ONES:CB_ONES + 128] = 1.0
    c[:, CB_IDENT:CB_IDENT + 128] = np.eye(128)
    bo = np.zeros((128, 128))
    bo[:64, :64] = 1.0
    bo[64:, 64:] = 1.0
    c[:, CB_BONES:CB_BONES + 128] = bo
    s = np.arange(128)[:, None]
    t = np.arange(128)[None, :]
    caus = (t >= s).astype(np.float64)
    c[:, CB_CAUS:CB_CAUS + 128] = caus
    c[:, CB_BD64:CB_BD64 + 128] = caus * bo
    p = np.arange(128)
    for pr in range(2):
        for hp in range(2):
            g = RET_G[pr * 2 + hp]
            c[hp * 64:(hp + 1) * 64, CB_QD + pr * 128:CB_QD + (pr + 1) * 128] = (g ** (np.arange(128) + 1.0) / 8.0)[None, :]
            c[hp * 64:(hp + 1) * 64, CB_SEGD + pr * 16:CB_SEGD + (pr + 1) * 16] = (g ** (128.0 * np.arange(16)))[None, :]
    c[:, CS_EPS] = EPS
    c[:, CS_INVM] = (np.float32(10000.0) ** (-(np.arange(0, 32, 2, dtype=np.float32)) / np.float32(32)))[p % 16]
    c[:, CS_INVR] = (np.float32(10000.0) ** (-(np.arange(0, 64, 2, dtype=np.float32)) / np.float32(64)))[p % 32]
    c[:, CS_SGNM] = np.where((p % 32) < 16, -1.0, 1.0)
    c[:, CS_SGNR] = np.where((p % 64) < 32, -1.0, 1.0)
    for h in range(4):
        g = RET_G[h]
        c[:, CS_KDEC + h] = g ** (127.0 - p)
        c[:, CS_DM + h * 128:CS_DM + (h + 1) * 128] = np.where(t >= s, g ** np.maximum(t - s, 0) / 8.0, 0.0)
    return c.astype(np.float32)


def _pack_pp(inp, has_pred):
    pp = np.zeros((128, PPW), np.float32)

    def fm(v):
        return np.ascontiguousarray(np.asarray(v, np.float32).reshape(-1, 128).T)
    for l in range(L):
        pp[:, PP_FFN1 + l * 8:PP_FFN1 + l * 8 + 8] = fm(inp["ffn1_norm"][l])
        pp[:, PP_MIX + l * 8:PP_MIX + l * 8 + 8] = fm(inp["mix_norm"][l])
        pp[:, PP_FFN2 + l * 8:PP_FFN2 + l * 8 + 8] = fm(inp["ffn2_norm"][l])
        pp[:, PP_PLE + l * 8:PP_PLE + l * 8 + 8] = fm(inp["ple_norm"][l])
        pp[:, PP_QN + l * 2:PP_QN + l * 2 + 2] = fm(inp["mla_q_norm"][l])
        pp[:, PP_KVN + l:PP_KVN + l + 1] = fm(inp["mla_kv_norm"][l])
        pp[:, PP_HGN + l * 2:PP_HGN + l * 2 + 2] = fm(inp["hg_norm"][l])
        pp[:, PP_RTN + l * 2:PP_RTN + l * 2 + 2] = fm(inp["ret_norm"][l])
        for h in range(4):
            pp[:, PP_LBL + h * L + l] = np.asarray(inp["hg_lb_logits"][l], np.float32)[h * 128:(h + 1) * 128]
    pp[:, PP_FINAL:PP_FINAL + 8] = fm(inp["final_norm"])
    pp[:, PP_FLAG] = 1.0 if has_pred else 0.0
    return pp


def _swap_half(w, c0, width, hd):
    blk = np.asarray(w)[..., c0:c0 + width]
    sh = blk.shape[:-1]
    b = blk.reshape(sh + (width // hd, 2, hd // 2))
    return np.ascontiguousarray(b[..., ::-1, :].reshape(sh + (width,)))


def _prep_shared(inp):
    f = lambda k: np.ascontiguousarray(np.asarray(inp[k], np.float32))
    sh = {}
    for n in ("ffn1_w_gate", "ffn1_w_up", "ffn2_w_gate", "ffn2_w_up", "ffn1_w_down", "ffn2_w_down", "w_in",
              "w_out", "ple_w_proj", "ple_w_gate", "sg_ln"):
        sh[n] = f(n)
    w_in = sh["w_in"]
    sh["w_in_sw"] = np.ascontiguousarray(np.concatenate(
        [_swap_half(w_in, C_KPE, 32, 32), _swap_half(w_in, C_RQ, 256, 64), _swap_half(w_in, C_RK, 256, 64)], axis=-1))
    uq = f("mla_w_uq").reshape(L, 256, 4, 96)
    sh["w_uq"] = np.ascontiguousarray(uq.reshape(L, 256, 384))
    sh["w_uq_sw"] = np.ascontiguousarray(_swap_half(uq[..., 64:96].reshape(L, 256, 128), 0, 128, 32))
    sh["w_ukv"] = f("mla_w_ukv")
    sh["sg_wT"] = np.ascontiguousarray(f("sg_w_s").transpose(0, 1, 3, 2))
    sh["sg_b"] = f("sg_b_s")
    sh["cst"] = _consts()
    return sh


def _core_inputs(inp, sh, c):
    b = c
    m = dict(sh)
    m["xT"] = np.ascontiguousarray(np.asarray(inp["x"], np.float32)[b].T)
    m["pT"] = np.ascontiguousarray(np.asarray(inp["p"], np.float32)[:, b].transpose(0, 2, 1))
    m["pos"] = np.ascontiguousarray(np.asarray(inp["positions"], np.int32)[b][None, :])
    m["pp"] = _pack_pp(inp, False)
    return m


def run(inputs, n_layers=L, dbg=None, cores=4, use_cc=False, stages=3, stop_at=None, halves=(0, 1)):
    bld = Builder(n_layers=n_layers, dbg=dbg, use_cc=use_cc, stages=stages, stop_at=stop_at, halves=halves)
    nc = bld.build()
    sh = _prep_shared(inputs)
    in_maps = []
    for c in range(cores):
        m = _core_inputs(inputs, sh, c)
        in_maps.append({k: m[k] for k in bld.dram})
    res = run_bass_kernel_spmd(nc, in_maps, core_ids=list(range(cores)))
    return res.results


def kernel(**inputs):
    res = run(inputs)
    out = np.empty((4, 2 * T, D), np.float32)
    for c in range(4):
        out[c] = res[c]["outT"].T
    return out
```

```python
import numpy as np
import concourse.bass as bass
import concourse.mybir as mybir
from concourse.bass_utils import run_bass_kernel_spmd
from contextlib import ExitStack

F32 = mybir.dt.float32
BF16 = mybir.dt.bfloat16
I32 = mybir.dt.int32
ALU = mybir.AluOpType
AF = mybir.ActivationFunctionType

ENGS = ["tensor", "vector", "scalar", "gpsimd", "sync"]
NDMA_SEM = 8

D = 1024
L = 4
T = 2048
TT = 512
NT = T // TT
NB = T // 128
DFF = 2816
NFC = DFF // 128
EPS = 1e-6
INW = 3488
C_CQ, C_CKV, C_KPE, C_ZU, C_ZV, C_HQ, C_HF, C_HI, C_HG, C_RQ, C_RK, C_RV, C_RG = (
    0, 256, 384, 416, 672, 928, 1440, 1952, 2208, 2464, 2720, 2976, 3232)


class Buf:
    __slots__ = ("name", "lw", "rd")

    def __init__(self, name=""):
        self.name = name
        self.lw = None
        self.rd = {}


class Prog:
    def __init__(self, nc):
        self.nc = nc
        self.es = ExitStack()
        self.ops = {e: [] for e in ENGS}
        self.cnt = {}
        self.sems = {}
        self.mult = {}
        self.waited = {e: {} for e in ENGS}
        self.dma_i = {e: 0 for e in ENGS}
        for e in ENGS:
            if e != "sync":
                self._mksem(e, 1)
        for e in ("sync", "gpsimd", "scalar"):
            for j in range(NDMA_SEM):
                self._mksem(("dma", e, j), 16)

    def _mksem(self, key, mult):
        name = key if isinstance(key, str) else "_".join(str(k) for k in key)
        self.sems[key] = self.es.enter_context(self.nc.semaphore("s_" + name))
        self.cnt[key] = 0
        self.mult[key] = mult

    def sb(self, name, shape, dt):
        return self.es.enter_context(self.nc.sbuf_tensor("sb_" + name, list(shape), dt))

    def ps(self, name, shape, dt=F32):
        return self.es.enter_context(self.nc.psum_tensor(name, list(shape), dt))

    def _deps(self, eng, reads, writes):
        deps = {}

        def add(k, s):
            if deps.get(k, 0) < s:
                deps[k] = s
        for b in reads:
            if b.lw is not None:
                add(*b.lw)
        for b in writes:
            if b.lw is not None:
                add(*b.lw)
            for k, s in b.rd.items():
                add(k, s)
        waits = []
        w = self.waited[eng]
        for k, s in deps.items():
            if k == "tensor" and eng == "tensor":
                continue
            if w.get(k, 0) >= s:
                continue
            w[k] = s
            waits.append((k, s))
        return waits

    def _mark(self, key, seq, reads, writes):
        for b in reads:
            if b.rd.get(key, 0) < seq:
                b.rd[key] = seq
        for b in writes:
            b.lw = (key, seq)
            b.rd = {}

    def op(self, eng, fn, r=(), w=(), inc=True):
        waits = self._deps(eng, r, w)
        seq = self.cnt[eng] + 1
        if inc:
            self.cnt[eng] = seq
        self._mark(eng, seq, r, w)
        self.ops[eng].append((fn, waits, eng if inc else None))

    def dma(self, eng, fn, r=(), w=()):
        i = self.dma_i[eng]
        self.dma_i[eng] = i + 1
        key = ("dma", eng, i % NDMA_SEM)
        waits = self._deps(eng, r, w)
        prev = self.cnt[key]
        if prev > 0 and self.waited[eng].get(key, 0) < prev:
            self.waited[eng][key] = prev
            waits.append((key, prev))
        seq = prev + 1
        self.cnt[key] = seq
        self._mark(key, seq, r, w)
        self.ops[eng].append((fn, waits, key))

    def wait_all(self, eng, bufs):
        waits = self._deps(eng, bufs, ())
        self.ops[eng].append((None, waits, None))

    def emit(self):
        nc = self.nc
        with nc.Block() as block:
            for e in ENGS:
                ops = self.ops[e]

                def body(engine, ops=ops):
                    for fn, waits, inckey in ops:
                        for k, s in waits:
                            engine.wait_ge(self.sems[k], s * self.mult[k])
                        if fn is None:
                            continue
                        inst = fn(engine)
                        if inckey is not None:
                            inst.then_inc(self.sems[inckey], self.mult[inckey])
                getattr(block, e)(body)
        self.es.close()


class Arena:
    def __init__(self, tile_bf16, nbytes):
        self.t = tile_bf16
        self.n = nbytes
        self.regs = []
        self.names = {}

    def at(self, off, size, dt, name=""):
        assert off % 4 == 0 and size % 4 == 0 and off + size <= self.n, (name, off, size, self.n)
        b = Buf(name)
        self.names[name] = (off, size, dt)
        keep = []
        for (o, s, ob) in self.regs:
            if o < off + size and off < o + s:
                merge_into(b, ob)
                if not (off <= o and o + s <= off + size):
                    keep.append((o, s, ob))
            else:
                keep.append((o, s, ob))
        self.regs = keep
        self.regs.append((off, size, b))
        ap = self.t[:, off // 2:(off + size) // 2]
        if dt != BF16:
            ap = ap.bitcast(dt)
        return ap, b


class Stack:
    def __init__(self, arena, start, end):
        self.a = arena
        self.start = start
        self.end = end
        self.lo = start
        self.hi = end

    def plo(self, size, dt, name=""):
        size = (size + 31) // 32 * 32
        assert self.lo + size <= self.hi, ("arena overflow lo", name, self.lo, size, self.hi)
        r = self.a.at(self.lo, size, dt, name)
        self.lo += size
        return r

    def phi(self, size, dt, name=""):
        size = (size + 31) // 32 * 32
        assert self.hi - size >= self.lo, ("arena overflow hi", name, self.lo, size, self.hi)
        self.hi -= size
        return self.a.at(self.hi, size, dt, name)

    def phi_off(self, size):
        size = (size + 31) // 32 * 32
        assert self.hi - size >= self.lo, ("arena overflow hi(off)", self.lo, size, self.hi)
        self.hi -= size
        return self.hi

    def reset_hi(self):
        self.hi = self.end

    def reset(self):
        self.lo = self.start
        self.hi = self.end


def merge_into(dst, src):
    for k, v in src.rd.items():
        if dst.rd.get(k, 0) < v:
            dst.rd[k] = v
    if src.lw is not None:
        k, v = src.lw
        if dst.rd.get(k, 0) < v:
            dst.rd[k] = v


def _esize(dt):
    return 2 if dt == BF16 else 4


class Builder:
    def __init__(self, n_layers=L, dbg=None, use_cc=False, stages=3, stop_at=None, halves=(0, 1)):
        self.nl = n_layers
        self.halves = halves
        self.stages = stages
        self.stop_at = stop_at
        self.stopped = False
        self.dbg = dbg or []
        self.use_cc = use_cc
        self.nc = nc = bass.Bass("TRN2", target_bir_lowering=False)
        self.P = Prog(nc)
        self.dram = {}
        self.outs = {}

    def din(self, name, shape, dt=F32):
        t = self.nc.dram_tensor(name, list(shape), dt, kind="ExternalInput").ap()
        self.dram[name] = t
        return t

    def dout(self, name, shape, dt=F32):
        t = self.nc.dram_tensor(name, list(shape), dt, kind="ExternalOutput").ap()
        self.outs[name] = t
        return t

    def V(self, fn, r=(), w=()):
        self.P.op("vector", fn, r, w)

    def A(self, fn, r=(), w=()):
        self.P.op("scalar", fn, r, w)

    def G(self, fn, r=(), w=()):
        self.P.op("gpsimd", fn, r, w)

    def MM(self, out, lhsT, rhs, start, stop, r=(), w=(), inc=True):
        self.P.op("tensor", lambda e: e.matmul(out, lhsT, rhs, start=start, stop=stop), r, w, inc=inc)

    def dump(self, name, ap, buf, shape, dt=F32):
        if name not in self.dbg:
            return
        o = self.dout("dbg_" + name, shape, dt)
        self.P.dma("sync", lambda e: e.dma_start(out=o, in_=ap), r=[buf], w=[self.Bout])

    def build(self):
        nc, P = self.nc, self.P
        nl = self.nl
        xT = self.din("xT", [D, 2 * T])
        pT = self.din("pT", [L, 256, 2 * T])
        pos = self.din("pos", [1, 2 * T], I32)
        pp = self.din("pp", [128, PPW])
        cst = self.din("cst", [128, CSTW])
        W = {}
        for n in ("ffn1_w_gate", "ffn1_w_up", "ffn2_w_gate", "ffn2_w_up"):
            W[n] = self.din(n, [L, D, DFF])
        for n in ("ffn1_w_down", "ffn2_w_down"):
            W[n] = self.din(n, [L, DFF, D])
        W["w_in"] = self.din("w_in", [L, D, INW])
        W["w_in_sw"] = self.din("w_in_sw", [L, D, 544])
        W["w_uq"] = self.din("w_uq", [L, 256, 384])
        W["w_uq_sw"] = self.din("w_uq_sw", [L, 256, 128])
        W["w_ukv"] = self.din("w_ukv", [L, 128, 512])
        W["sg_ln"] = self.din("sg_ln", [L, 256])
        W["sg_wT"] = self.din("sg_wT", [L, 4, 128, 128])
        W["sg_b"] = self.din("sg_b", [L, 4, 128])
        W["w_out"] = self.din("w_out", [L, D, D])
        W["ple_w_proj"] = self.din("ple_w_proj", [L, 256, D])
        W["ple_w_gate"] = self.din("ple_w_gate", [L, D, D])
        self.W = W
        outT = self.dout("outT", [D, 2 * T])
        self.Bout = Buf("out")
        hsp = nc.dram_tensor("hspill", [D, T], F32).ap()
        self.hsp = hsp
        self.Bhsp = [Buf("hspill%d" % i) for i in range(NT)]

        AR_BYTES = 148 * 1024
        self.AR_BYTES = AR_BYTES
        ar_t = P.sb("arena", [128, AR_BYTES // 2], BF16)
        self.arena = Arena(ar_t, AR_BYTES)
        self.H0 = 4 * 2 * T * 2
        self.H_BYTES = 8 * T * 4
        xn_t = P.sb("xn", [128, 8 * T], BF16)
        self.xn = xn_t[:].rearrange("p (c t) -> p c t", c=8)
        self.Bxn = [Buf("xn%d" % i) for i in range(NT)]
        pp_t = P.sb("pp", [128, PPW], F32)
        self.pp = pp_t
        self.Bpp = Buf("pp")
        cst_t = P.sb("cst", [128, CSTW], F32)
        self.cst = cst_t
        self.Bcst = Buf("cst")
        cb_t = P.sb("cstb", [128, CSTBW], BF16)
        self.cb = cb_t
        self.Bcb = Buf("cstb")
        pb_t = P.sb("pbias", [128, 1], F32)
        self.pbias = pb_t
        self.Bpbias = Buf("pbias")
        self.pb = [P.ps("pb%d" % i, [128, 512], F32) for i in range(8)]
        self.Bpb = [Buf("pb%d" % i) for i in range(8)]
        self._rr = 0
        self._cpi = 0

        P.dma("sync", lambda e: e.dma_start(out=pp_t[:], in_=pp), w=[self.Bpp])
        P.dma("sync", lambda e: e.dma_start(out=cst_t[:], in_=cst), w=[self.Bcst])
        self.V(lambda e: e.tensor_copy(out=cb_t[:, 0:CSTBW], in_=cst_t[:, 0:CSTBW]), r=[self.Bcst], w=[self.Bcb])
        self.V(lambda e: e.tensor_scalar(out=pb_t[:, 0:1], in0=pp_t[:, PP_FLAG:PP_FLAG + 1], scalar1=-1.0, scalar2=30000.0,
                                         op0=ALU.add, op1=ALU.mult), r=[self.Bpp], w=[self.Bpbias])

        self.hs = [nc.dram_tensor("hs%d" % i, [D, T], F32).ap() for i in range(2)]
        self.Bhs = [[Buf("hs0_%d" % i) for i in range(NT)], [Buf("hs1_%d" % i) for i in range(NT)]]
        self.tabd = [nc.dram_tensor("tabd%d" % i, [128, 4 * T], BF16).ap() for i in range(2)]
        self.Btabd = [Buf("tabd0"), Buf("tabd1")]
        self.xsd = {}
        self.h_ap, self.hb = self.arena.at(self.H0, self.H_BYTES, F32, "h")
        self.h = self.h_ap.rearrange("p (c t) -> p c t", c=8)
        self.Bh = [Buf("h%d" % i) for i in range(NT)]
        self.stk = Stack(self.arena, self.H0 + self.H_BYTES, AR_BYTES)
        self.pos = pos
        if self.stages >= 2:
            self.alloc_tables()
            for half in range(2):
                self.setup_tables(pos, half)
                P.dma("sync", lambda e, half=half: e.dma_start(out=self.tabd[half][:, :], in_=self.tab_t[:, :]),
                      r=[self.Btab], w=[self.Btabd[half]])
            self.setup_lb()
        for l in range(nl):
            for half in self.halves:
                self.half = half
                c0 = half * T
                self.h_ap, self.hb = self.arena.at(self.H0, self.H_BYTES, F32, "h")
                self.h = self.h_ap.rearrange("p (c t) -> p c t", c=8)
                self.Bh = [Buf("h%d" % i) for i in range(NT)]
                for bb_ in self.Bh:
                    merge_into(bb_, self.hb)
                src = xT[:, c0:c0 + T] if l == 0 else self.hs[half][:, :]
                for tt in range(NT):
                    ts = slice(tt * TT, (tt + 1) * TT)
                    P.dma("sync", lambda e, ts=ts, src=src: e.dma_start(out=self.h[:, :, ts], in_=src[:, ts].rearrange("(c p) t -> p c t", p=128)),
                          r=([] if l == 0 else [self.Bhs[half][tt]]), w=[self.Bh[tt]])
                if self.stages >= 2:
                    P.dma("sync", lambda e, half=half: e.dma_start(out=self.tab_t[:, :], in_=self.tabd[half][:, :]),
                          r=[self.Btabd[half]], w=[self.Btab])
                self.norm(PP_FFN1 + l * 8, 1.0 / D)
                self.ffn(W["ffn1_w_gate"], W["ffn1_w_up"], W["ffn1_w_down"], l)
                if "h_ffn1" in self.dbg and l == 0 and half == 0:
                    o = self.dout("dbg_h_ffn1", [D, T])
                    for ch in range(8):
                        P.dma("sync", lambda e, ch=ch: e.dma_start(out=o[ch * 128:(ch + 1) * 128, :], in_=self.h[:, ch, :]),
                              r=self.Bh, w=[self.Bout])
                if self.stages >= 2:
                    self.mixers(l, half)
                    if self.stopped:
                        break
                    if "h_mix" in self.dbg and l == 0 and half == getattr(self, "dbg_half", 0):
                        o = self.dout("dbg_h_mix", [D, T])
                        for ch in range(8):
                            P.dma("sync", lambda e, ch=ch, o=o: e.dma_start(out=o[ch * 128:(ch + 1) * 128, :], in_=self.h[:, ch, :]),
                                  r=self.Bh, w=[self.Bout])
                if self.stages >= 3:
                    self.norm(PP_FFN2 + l * 8, 1.0 / D)
                    self.ffn(W["ffn2_w_gate"], W["ffn2_w_up"], W["ffn2_w_down"], l)
                    self.ple(l, pT, c0)
                if l == nl - 1:
                    self.final_out(outT, c0)
                else:
                    for tt in range(NT):
                        ts = slice(tt * TT, (tt + 1) * TT)
                        P.dma("sync", lambda e, ts=ts, half=half: e.dma_start(out=self.hs[half][:, ts].rearrange("(c p) t -> p c t", p=128), in_=self.h[:, :, ts]),
                              r=[self.Bh[tt]], w=[self.Bhs[half][tt]])
                    for b in self.Bh:
                        merge_into(self.hb, b)
            if self.stopped:
                break
        P.wait_all("sync", [self.Bout])
        for j in range(NDMA_SEM):
            key = ("dma", "sync", j)
            if P.cnt[key] > 0:
                P.ops["sync"].append((None, [(key, P.cnt[key])], None))
        P.emit()
        return nc

    def norm(self, gcol, inv_n):
        stk = self.stk
        stk.reset_hi()
        ones = self.cb[:, CB_ONES:CB_ONES + 128]
        sqs = [stk.phi(8 * TT * 2, BF16, "sq%d" % i) for i in range(NT)]
        rss = [stk.phi(TT * 4, F32, "rstd%d" % i) for i in range(NT)]
        epsb = self.cst[:, CS_EPS:CS_EPS + 1]
        for tt in range(NT):
            ts = slice(tt * TT, (tt + 1) * TT)
            sq, Bsq = sqs[tt]
            sq3 = sq.rearrange("p (c t) -> p c t", c=8)
            for ch in range(8):
                if ch % 2 == 0:
                    self.G(lambda e, ch=ch, ts=ts, sq3=sq3: e.tensor_tensor(out=sq3[:, ch, :], in0=self.h[:, ch, ts],
                                                                           in1=self.h[:, ch, ts], op=ALU.mult),
                           r=[self.Bh[tt]], w=[Bsq])
                else:
                    self.A(lambda e, ch=ch, ts=ts, sq3=sq3: e.activation(out=sq3[:, ch, :], in_=self.h[:, ch, ts], func=AF.Square),
                           r=[self.Bh[tt]], w=[Bsq])
        for tt in range(NT):
            sq, Bsq = sqs[tt]
            sq3 = sq.rearrange("p (c t) -> p c t", c=8)
            rs, Brs = rss[tt]
            bank = 5 + (tt % 2)
            ps, Bps = self.pb[bank], self.Bpb[bank]
            for ch in range(8):
                self.MM(ps[:, :], ones, sq3[:, ch, :], ch == 0, ch == 7, r=[Bsq, self.Bcb], w=[Bps], inc=(ch == 7))
            self.A(lambda e, ps=ps, rs=rs: e.activation(out=rs, in_=ps[:, :], func=AF.Ln, bias=epsb, scale=inv_n),
                   r=[Bps, self.Bcst], w=[Brs])
            self.A(lambda e, rs=rs: e.activation(out=rs, in_=rs, func=AF.Exp, scale=-0.5), r=[Brs], w=[Brs])
        for tt in range(NT):
            ts = slice(tt * TT, (tt + 1) * TT)
            rs, Brs = rss[tt]
            for ch in range(8):
                self.V(lambda e, ch=ch, ts=ts, rs=rs: e.scalar_tensor_tensor(
                    out=self.xn[:, ch, ts], in0=self.h[:, ch, ts], scalar=self.pp[:, gcol + ch:gcol + ch + 1],
                    in1=rs, op0=ALU.mult, op1=ALU.mult), r=[self.Bh[tt], Brs, self.Bpp], w=[self.Bxn[tt]])

    def ffn(self, wg, wu, wd, l):
        stk = self.stk
        stk.reset_hi()
        NG = NFC // 2
        NSLOT = 3
        slots = []
        for s in range(NSLOT):
            g_ap, Bg = stk.phi(8 * 256 * 2, BF16, "wg%d" % s)
            u_ap, Bu = stk.phi(8 * 256 * 2, BF16, "wu%d" % s)
            d_ap, Bd = stk.phi(2 * D * 2, BF16, "wd%d" % s)
            slots.append((g_ap.rearrange("p (c f) -> p c f", c=8), Bg, u_ap.rearrange("p (c f) -> p c f", c=8), Bu,
                          d_ap.rearrange("p (c d) -> p c d", c=2), Bd))
        NA = 6
        abuf = [stk.phi(TT * 2, BF16, "A%d" % i) for i in range(NA)]
        sgb = [stk.phi(TT * 2, BF16, "sg%d" % i) for i in range(3)]

        def load(g):
            g3, Bg, u3, Bu, d3, Bd = slots[g % NSLOT]
            c0 = g * 256
            self.P.dma("gpsimd", lambda e: e.dma_start(
                out=g3, in_=wg[l, :, c0:c0 + 256].rearrange("(c p) f -> p c f", p=128)), w=[Bg])
            self.P.dma("gpsimd", lambda e: e.dma_start(
                out=u3, in_=wu[l, :, c0:c0 + 256].rearrange("(c p) f -> p c f", p=128)), w=[Bu])
            self.P.dma("gpsimd", lambda e: e.dma_start(
                out=d3, in_=wd[l, c0:c0 + 256, :].rearrange("(c p) d -> p c d", p=128)), w=[Bd])
        load(0)
        load(1)
        ai = 0
        gi = 0
        yi = 0
        YB = (4, 5, 6, 7)

        def down(item):
            nonlocal yi
            d3, Bd, Aj, tt = item
            ts = slice(tt * TT, (tt + 1) * TT)
            for dc in range(8):
                b = YB[yi % 4]
                py, Bpy = self.pb[b], self.Bpb[b]
                yi += 1
                for j in range(2):
                    self.MM(py[:, :], d3[:, j, dc * 128:(dc + 1) * 128], Aj[j][0], j == 0, j == 1,
                            r=[Bd, Aj[j][1]], w=[Bpy], inc=(j == 1))
                self.V(lambda e, py=py, dc=dc, ts=ts: e.scalar_tensor_tensor(
                    out=self.h[:, dc, ts], in0=py[:, :], scalar=0.5, in1=self.h[:, dc, ts],
                    op0=ALU.mult, op1=ALU.add), r=[Bpy, self.Bh[tt]], w=[self.Bh[tt]])
        prev = None
        for g in range(NG):
            g3, Bg, u3, Bu, d3, Bd = slots[g % NSLOT]
            for tt in range(NT):
                ts = slice(tt * TT, (tt + 1) * TT)
                Aj = []
                for j in range(2):
                    pg, Bpg = self.pb[gi % 2], self.Bpb[gi % 2]
                    pu, Bpu = self.pb[2 + gi % 2], self.Bpb[2 + gi % 2]
                    gi += 1
                    for ch in range(8):
                        self.MM(pg[:, :], g3[:, ch, j * 128:(j + 1) * 128], self.xn[:, ch, ts], ch == 0, ch == 7,
                                r=[Bg, self.Bxn[tt]], w=[Bpg], inc=(ch == 7))
                    for ch in range(8):
                        self.MM(pu[:, :], u3[:, ch, j * 128:(j + 1) * 128], self.xn[:, ch, ts], ch == 0, ch == 7,
                                r=[Bu, self.Bxn[tt]], w=[Bpu], inc=(ch == 7))
                    sg, Bsg = sgb[ai % len(sgb)]
                    a_ap, Ba = abuf[ai % NA]
                    ai += 1
                    self.A(lambda e, sg=sg, pg=pg: e.activation(out=sg, in_=pg[:, :], func=AF.Silu), r=[Bpg], w=[Bsg])
                    self.V(lambda e, a_ap=a_ap, sg=sg, pu=pu: e.tensor_tensor(out=a_ap, in0=pu[:, :], in1=sg, op=ALU.mult),
                           r=[Bpu, Bsg], w=[Ba])
                    Aj.append((a_ap, Ba))
                if prev is not None:
                    down(prev)
                prev = (d3, Bd, Aj, tt)
                if tt == 0 and g + 2 < NG:
                    load(g + 2)
        down(prev)

    def bank(self):
        i = (0, 1, 2, 3)[self._rr % 4]
        self._rr += 1
        return self.pb[i], self.Bpb[i]

    def cp(self, out, in_, r, w):
        self._cpi += 1
        if self._cpi % 2 == 0:
            self.V(lambda e: e.tensor_copy(out=out, in_=in_), r=r, w=w)
        else:
            self.A(lambda e: e.activation(out=out, in_=in_, func=AF.Copy), r=r, w=w)

    def load_w(self, stk, wap2d, kc, ncols, name):
        ap, B = stk.phi(kc * ncols * 2, BF16, name)
        w3 = ap.rearrange("p (c f) -> p c f", c=kc)
        self.P.dma("gpsimd", lambda e: e.dma_start(out=w3, in_=wap2d.rearrange("(c p) f -> p c f", p=128)), w=[B])
        return w3, B

    def load_w_at(self, off, wap2d, kc, ncols, name):
        ap, B = self.arena.at(off, kc * ncols * 2, BF16, name)
        w3 = ap.rearrange("p (c f) -> p c f", c=kc)
        self.P.dma("gpsimd", lambda e: e.dma_start(out=w3, in_=wap2d.rearrange("(c p) f -> p c f", p=128)), w=[B])
        return w3, B

    def proj(self, ps_ap, Bps, w3, Bw, c0, M, tt):
        ts = slice(tt * TT, (tt + 1) * TT)
        for ch in range(8):
            self.MM(ps_ap, w3[:, ch, c0:c0 + M], self.xn[:, ch, ts], ch == 0, ch == 7,
                    r=[Bw, self.Bxn[tt]], w=[Bps], inc=(ch == 7))

    def proj_tm(self, ps_ap, Bps, w3, Bw, c0, n, tb):
        tt = tb // 4
        for ch in range(8):
            self.MM(ps_ap, self.xn[:, ch, tb * 128:(tb + 1) * 128], w3[:, ch, c0:c0 + n], ch == 0, ch == 7,
                    r=[Bw, self.Bxn[tt]], w=[Bps], inc=(ch == 7))

    def rstd_from(self, out, Bout, in_, Bin, scale):
        epsb = self.cst[:, CS_EPS:CS_EPS + 1]
        self.A(lambda e: e.activation(out=out, in_=in_, func=AF.Ln, bias=epsb, scale=scale), r=[Bin, self.Bcst], w=[Bout])
        self.A(lambda e: e.activation(out=out, in_=out, func=AF.Exp, scale=-0.5), r=[Bout], w=[Bout])

    def alloc_tables(self):
        tb_t = self.P.sb("ropetab", [128, 4 * T], BF16)
        self.tab_t = tb_t
        self.cosM, self.sinM = tb_t[:, 0:T], tb_t[:, T:2 * T]
        self.cosR, self.sinR = tb_t[:, 2 * T:3 * T], tb_t[:, 3 * T:4 * T]
        self.Btab = Buf("tab")

    def setup_tables(self, pos, half):
        P = self.P
        stk = self.stk
        stk.reset_hi()
        pi, Bpi = stk.phi(T * 4, I32, "posi")
        pf, Bpf = stk.phi(T * 4, F32, "posf")
        a, Ba = stk.phi(T * 4, F32, "ang")
        k, Bk = stk.phi(T * 4, F32, "kf")
        ki, Bki = stk.phi(T * 4, I32, "ki")
        P.dma("sync", lambda e: e.dma_start(out=pi, in_=pos[0, half * T:(half + 1) * T].partition_broadcast(128)), w=[Bpi])
        self.V(lambda e: e.tensor_copy(out=pf, in_=pi), r=[Bpi], w=[Bpf])
        PI = float(np.pi)
        TWO_PI = 2.0 * PI
        PI_LO = 3.1415925
        cst = self.cst
        for (invc, sgnc, cos_t, sin_t) in ((CS_INVM, CS_SGNM, self.cosM, self.sinM),
                                           (CS_INVR, CS_SGNR, self.cosR, self.sinR)):
            for which in ("sin", "cos"):
                if which == "sin":
                    self.V(lambda e, invc=invc: e.tensor_scalar(out=a, in0=pf, scalar1=cst[:, invc:invc + 1], scalar2=None,
                                                                op0=ALU.mult), r=[Bpf, self.Bcst], w=[Ba])
                else:
                    self.V(lambda e, invc=invc: e.tensor_scalar(out=a, in0=pf, scalar1=cst[:, invc:invc + 1],
                                                                scalar2=PI / 2, op0=ALU.mult, op1=ALU.add),
                           r=[Bpf, self.Bcst], w=[Ba])
                self.V(lambda e: e.tensor_scalar(out=k, in0=a, scalar1=1.0 / TWO_PI, scalar2=None, op0=ALU.mult),
                       r=[Ba], w=[Bk])
                self.V(lambda e: e.tensor_copy(out=ki, in_=k), r=[Bk], w=[Bki])
                self.V(lambda e: e.tensor_copy(out=k, in_=ki), r=[Bki], w=[Bk])
                self.V(lambda e: e.scalar_tensor_tensor(out=a, in0=k, scalar=-TWO_PI, in1=a, op0=ALU.mult, op1=ALU.add),
                       r=[Bk, Ba], w=[Ba])
                self.V(lambda e: e.tensor_scalar(out=k, in0=a, scalar1=PI, scalar2=-TWO_PI, op0=ALU.is_gt, op1=ALU.mult),
                       r=[Ba], w=[Bk])
                self.V(lambda e: e.tensor_tensor(out=a, in0=a, in1=k, op=ALU.add), r=[Ba, Bk], w=[Ba])
                self.V(lambda e: e.tensor_scalar(out=k, in0=a, scalar1=-PI, scalar2=TWO_PI, op0=ALU.is_lt, op1=ALU.mult),
                       r=[Ba], w=[Bk])
                self.V(lambda e: e.tensor_tensor(out=a, in0=a, in1=k, op=ALU.add), r=[Ba, Bk], w=[Ba])
                self.V(lambda e: e.tensor_scalar(out=a, in0=a, scalar1=PI_LO, scalar2=-PI_LO, op0=ALU.min, op1=ALU.max),
                       r=[Ba], w=[Ba])
                if which == "sin":
                    self.A(lambda e: e.activation(out=k, in_=a, func=AF.Sin), r=[Ba], w=[Bk])
                    self.V(lambda e, sgnc=sgnc, sin_t=sin_t: e.tensor_scalar(
                        out=sin_t, in0=k, scalar1=cst[:, sgnc:sgnc + 1], scalar2=None, op0=ALU.mult),
                        r=[Bk, self.Bcst], w=[self.Btab])
                else:
                    self.A(lambda e, cos_t=cos_t: e.activation(out=cos_t, in_=a, func=AF.Sin), r=[Ba], w=[self.Btab])

    def setup_lb(self):
        P = self.P
        lbt = P.sb("lbt", [128, 52], F32)
        self.lb = lbt[:, 0:16]
        self.omlb = lbt[:, 16:32]
        self.Blb = Buf("lb")
        e_ = lbt[:, 32:48]
        s4 = lbt[:, 48:52]
        B = self.Blb
        e3 = e_.rearrange("p (h l) -> p h l", l=4)
        lb3 = self.lb.rearrange("p (h l) -> p h l", l=4)
        self.A(lambda e: e.activation(out=e_, in_=self.pp[:, PP_LBL:PP_LBL + 16], func=AF.Exp), r=[self.Bpp], w=[B])
        self.V(lambda e: e.tensor_reduce(out=s4, in_=e3, axis=mybir.AxisListType.X, op=ALU.add), r=[B], w=[B])
        self.V(lambda e: e.reciprocal(out=s4, in_=s4), r=[B], w=[B])
        self.V(lambda e: e.tensor_tensor(out=e3, in0=e3, in1=s4.rearrange("p (h o) -> p h o", o=1).broadcast_to([128, 4, 4]),
                                         op=ALU.mult), r=[B], w=[B])
        self.V(lambda e: e.memset(self.lb, 0.0), r=[B], w=[B])
        for l in range(1, 4):
            self.V(lambda e, l=l: e.tensor_tensor(out=lb3[:, :, l], in0=lb3[:, :, l - 1], in1=e3[:, :, l], op=ALU.add),
                   r=[B], w=[B])
        self.V(lambda e: e.tensor_scalar(out=self.omlb, in0=self.lb, scalar1=-1.0, scalar2=1.0, op0=ALU.mult, op1=ALU.add),
               r=[B], w=[B])

    def mixers(self, l, half):
        P = self.P
        self.norm(PP_MIX + l * 8, 1.0 / D)
        for tt in range(NT):
            ts = slice(tt * TT, (tt + 1) * TT)
            P.dma("sync", lambda e, ts=ts: e.dma_start(out=self.hsp[:, ts].rearrange("(c p) t -> p c t", p=128), in_=self.h[:, :, ts]),
                  r=[self.Bh[tt]], w=[self.Bhsp[tt]])
        for b in self.Bh:
            merge_into(self.hb, b)
        M = Stack(self.arena, self.H0, self.AR_BYTES)
        self.M = M
        self.ycat = []
        self.Bycat = []
        for i in range(4):
            ap, B = self.arena.at(i * 2 * T * 2, 2 * T * 2, BF16, "ycat%d" % i)
            self.ycat.append(ap.rearrange("p (c t) -> p c t", c=2))
            self.Bycat.append(B)
        def stop(tag):
            if self.stop_at == tag:
                self.stopped = True
            return self.stopped
        if stop("spill"):
            return
        self.sg(l, M)
        M.reset()
        if stop("sg"):
            return
        self.hgrn2_s1(l, M)
        M.reset_hi()
        if self.stopped or stop("hg1"):
            return
        pred = (half == 1)
        if not pred:
            self.put("hg", l, [(self.hg_Sfin, 128, 256, self.Bhg_Sfin)], F32)
        else:
            self.hg_Spred, self.Bhg_Spred = self.load_state(M, "hg", l)
        self.hgrn2_s2(l, M, pred)
        M.reset()
        if stop("hg2"):
            return
        self.ret_s1(l, M)
        M.reset_hi()
        if stop("rt1"):
            return
        if not pred:
            self.put("rt", l, [(self.rt_Sfin, 128, 256, self.Brt_Sfin)], F32)
        else:
            self.rt_Spred, self.Brt_Spred = self.load_state(M, "rt", l)
        self.ret_s2(l, M, pred)
        M.reset()
        if stop("rt2"):
            return
        self.mla_s1(l, M)
        M.reset_hi()
        if stop("ml1"):
            return
        if not pred:
            self.put("ml", l, [(self.ml_lat[:, T:2 * T], 128, T, self.Bml_lat), (self.ml_kr[:, T:2 * T], 128, T, self.Bml_kr)], BF16)
        else:
            xs, Bxs = self.xsd[("ml", l)]
            P.dma("sync", lambda e: e.dma_start(out=self.ml_lat[:, 0:T], in_=xs[0:128, 0:T]), r=[Bxs], w=[self.Bml_lat])
            P.dma("sync", lambda e: e.dma_start(out=self.ml_kr[0:32, 0:T], in_=xs[0:32, T:2 * T]), r=[Bxs], w=[self.Bml_kr])
        self.mla_s2(l, M, pred)
        M.reset()
        if stop("ml2"):
            return
        if l == 0 and half == getattr(self, "dbg_half", 0):
            for i, nm in enumerate(("ya", "yb", "yc", "yd")):
                if nm in self.dbg:
                    o = self.dout("dbg_" + nm, [256, T], BF16)
                    for c in range(2):
                        P.dma("sync", lambda e, c=c, i=i, o=o: e.dma_start(out=o[c * 128:(c + 1) * 128, :], in_=self.ycat[i][:, c, :]),
                              r=[self.Bycat[i]], w=[self.Bout])
        wo, Bwo = self.load_w(M, self.W["w_out"][l, :, :], 8, D, "w_out")
        self.wout(l, wo, Bwo)

    def put(self, name, l, pieces, dt):
        P = self.P
        XW = sum(p[2] for p in pieces)
        xs = self.nc.dram_tensor("xs_%s%d" % (name, l), [128, XW], dt).ap()
        Bxs = Buf("xs")
        self.xsd[(name, l)] = (xs, Bxs)
        c0 = 0
        for (ap, rows, width, B) in pieces:
            P.dma("sync", lambda e, ap=ap, rows=rows, c0=c0, width=width: e.dma_start(out=xs[0:rows, c0:c0 + width], in_=ap), r=[B], w=[Bxs])
            c0 += width

    def load_state(self, M, name, l):
        xs, Bxs = self.xsd[(name, l)]
        sp, Bsp = M.phi(256 * 4, F32, name + "_sp32")
        self.P.dma("sync", lambda e: e.dma_start(out=sp, in_=xs[0:128, 0:256]), r=[Bxs], w=[Bsp])
        hs, Bhs = M.plo(256 * 2, BF16, name + "_Spred")
        self.V(lambda e: e.tensor_copy(out=hs, in_=sp), r=[Bsp], w=[Bhs])
        return hs, Bhs

    def sg(self, l, M):
        P = self.P
        W_in = self.W["w_in"]
        yb, Byb = self.ycat[1], self.Bycat[1]
        uT, Bu = M.phi(2 * T * 2, BF16, "sg_u")
        uT3 = uT.rearrange("p (c t) -> p c t", c=2)
        w3, Bw = self.load_w(M, W_in[l, :, C_ZU:C_ZU + 256], 8, 256, "w_zu")
        wv, Bwv = self.load_w(M, W_in[l, :, C_ZV:C_ZV + 256], 8, 256, "w_zv")
        for cc in range(2):
            for tt in range(NT):
                ts = slice(tt * TT, (tt + 1) * TT)
                ps, Bps = self.bank()
                self.proj(ps[:, :], Bps, w3, Bw, cc * 128, 128, tt)
                self.A(lambda e, ps=ps, cc=cc, ts=ts: e.activation(out=uT3[:, cc, ts], in_=ps[:, :], func=AF.Gelu),
                       r=[Bps], w=[Bu])
        gv, Bgv = M.phi(NB * 256 * 4, F32, "sg_gv")
        gv3 = gv.rearrange("p (b c) -> p b c", b=NB)
        st, Bst = M.phi(NB * 6 * 4, F32, "sg_st")
        mv, Bmv = M.phi(NB * 2 * 4, F32, "sg_mv")
        mv3 = mv.rearrange("p (b two) -> p b two", two=2)
        rsd, Brsd = M.phi(NB * 4, F32, "sg_rsd")
        lng, Blng = M.phi(256 * 4, F32, "sg_lng")
        vtm, Bvtm = M.phi(NB * 256 * 2, BF16, "sg_vtm")
        vtm3 = vtm.rearrange("p (b c) -> p b c", b=NB)
        wsf, Bwsf = M.phi(4 * 128 * 4, F32, "sg_wsf")
        wsf3 = wsf.rearrange("p (h t) -> p h t", h=4)
        wm, Bwm = M.phi(4 * 128 * 2, BF16, "sg_wm")
        wm3 = wm.rearrange("p (h t) -> p h t", h=4)
        bsf, Bbsf = M.phi(512 * 4, F32, "sg_bsf")
        bsb, Bbsb = M.phi(512 * 2, BF16, "sg_bsb")
        P.dma("sync", lambda e: e.dma_start(out=lng, in_=self.W["sg_ln"][l, :].partition_broadcast(128)), w=[Blng])
        P.dma("sync", lambda e: e.dma_start(out=wsf3, in_=self.W["sg_wT"][l].rearrange("h s t -> s h t")), w=[Bwsf])
        P.dma("sync", lambda e: e.dma_start(out=bsf[0:1, :], in_=self.W["sg_b"][l:l + 1].rearrange("o h t -> o (h t)")), w=[Bbsf])
        caus = self.cst[:, CB_CAUS:CB_CAUS + 128]
        for hh in range(4):
            self.V(lambda e, hh=hh: e.tensor_tensor(out=wm3[:, hh, :], in0=wsf3[:, hh, :], in1=caus, op=ALU.mult),
                   r=[Bwsf, self.Bcst], w=[Bwm])
        self.V(lambda e: e.tensor_copy(out=bsb[0:1, :], in_=bsf[0:1, :]), r=[Bbsf], w=[Bbsb])
        for tb in range(NB):
            ps, Bps = self.bank()
            self.proj_tm(ps[:, 0:256], Bps, wv, Bwv, 0, 256, tb)
            self.A(lambda e, ps=ps, tb=tb: e.activation(out=gv3[:, tb, :], in_=ps[:, 0:256], func=AF.Gelu), r=[Bps], w=[Bgv])
            self.V(lambda e, tb=tb: e.bn_stats(out=st[:, tb * 6:(tb + 1) * 6], in_=gv3[:, tb, :]), r=[Bgv], w=[Bst])
            self.V(lambda e, tb=tb: e.bn_aggr(out=mv[:, tb * 2:(tb + 1) * 2], in_=st[:, tb * 6:(tb + 1) * 6]), r=[Bst], w=[Bmv])
        self.rstd_from(rsd, Brsd, mv3[:, :, 1], Bmv, 1.0)
        for tb in range(NB):
            self.V(lambda e, tb=tb: e.tensor_scalar(out=gv3[:, tb, :], in0=gv3[:, tb, :], scalar1=mv3[:, tb, 0:1],
                                                    scalar2=rsd[:, tb:tb + 1], op0=ALU.subtract, op1=ALU.mult),
                   r=[Bgv, Bmv, Brsd], w=[Bgv])
            self.G(lambda e, tb=tb: e.tensor_tensor(out=vtm3[:, tb, :], in0=gv3[:, tb, :], in1=lng, op=ALU.mult),
                   r=[Bgv, Blng], w=[Bvtm])
        onesrow = self.cb[0:1, CB_ONES:CB_ONES + 64]
        for tt in range(NT):
            ts = slice(tt * TT, (tt + 1) * TT)
            for pr in range(2):
                ps, Bps = self.bank()
                for bi in range(4):
                    tb = tt * 4 + bi
                    for hp in range(2):
                        hh = pr * 2 + hp
                        o = ps[hp * 64:(hp + 1) * 64, bi * 128:(bi + 1) * 128]
                        self.MM(o, vtm3[:, tb, hh * 64:(hh + 1) * 64], wm3[:, hh, :], True, False, r=[Bvtm, Bwm], w=[Bps], inc=False)
                        self.MM(o, onesrow, bsb[0:1, hh * 128:(hh + 1) * 128], False, True, r=[Bbsb, self.Bcb], w=[Bps],
                                inc=(bi == 3 and hp == 1))
                self.V(lambda e, ps=ps, pr=pr, ts=ts: e.tensor_tensor(out=yb[:, pr, ts], in0=ps[:, :], in1=uT3[:, pr, ts], op=ALU.mult),
                       r=[Bps, Bu], w=[Byb])
    def hgrn2_s1(self, l, M):
        P = self.P
        W_in = self.W["w_in"]
        oloc, Bol = M.plo(2 * T * 2, BF16, "hg_oloc")
        self.hg_oloc, self.Bhg_oloc = oloc.rearrange("p (c t) -> p c t", c=2), Bol
        qB, BqB = M.plo(4 * T * 2, BF16, "hg_qB")
        self.hg_qB, self.Bhg_qB = qB.rearrange("p (h t) -> p h t", h=4), BqB
        gate, Bgate = M.plo(2 * T * 2, BF16, "hg_gate")
        self.hg_gate, self.Bhg_gate = gate.rearrange("p (c t) -> p c t", c=2), Bgate
        wg3, Bwg = self.load_w(M, W_in[l, :, C_HG:C_HG + 256], 8, 256, "w_hg")
        wv3, Bwv = self.load_w(M, W_in[l, :, C_HI:C_HI + 256], 8, 256, "w_hi")
        for cc in range(2):
            for tt in range(NT):
                ts = slice(tt * TT, (tt + 1) * TT)
                ps, Bps = self.bank()
                self.proj(ps[:, :], Bps, wg3, Bwg, cc * 128, 128, tt)
                self.A(lambda e, ps=ps, cc=cc, ts=ts: e.activation(out=self.hg_gate[:, cc, ts], in_=ps[:, :], func=AF.Silu),
                       r=[Bps], w=[Bgate])
        vtm, Bvtm = M.phi(NB * 256 * 2, BF16, "hg_vtm")
        vtm3 = vtm.rearrange("p (b c) -> p b c", b=NB)
        for tb in range(NB):
            ps, Bps = self.bank()
            self.proj_tm(ps[:, 0:256], Bps, wv3, Bwv, 0, 256, tb)
            self.cp(vtm3[:, tb, :], ps[:, 0:256], [Bps], [Bvtm])
        if self.stop_at == "hg1a":
            self.stopped = True
            return
        fg, Bfg = M.phi(T * 4, F32, "hg_fg")
        bb, Bbb = M.phi(T * 4, F32, "hg_b")
        tmp, Btmp = M.phi(T * 4, F32, "hg_tmp")
        qT, BqT = M.phi(T * 2, BF16, "hg_qT")
        kT, BkT = M.phi(T * 2, BF16, "hg_kT")
        qtl, Bqtl = M.phi(T * 2, BF16, "hg_qtl")
        ktl, Bktl = M.phi(T * 2, BF16, "hg_ktl")
        khT, BkhT = M.phi(T * 2, BF16, "hg_khT")
        khtm, Bkhtm = M.phi(NB * 128 * 2, BF16, "hg_khtm")
        khtm3 = khtm.rearrange("p (b k) -> p b k", b=NB)
        ebl, Bebl = M.phi(32 * 4, F32, "hg_ebl")
        S32, BS32 = M.phi(64 * 4, F32, "hg_S32")
        Sb = [M.phi(64 * 2, BF16, "hg_Sb%d" % i) for i in range(8)]
        msk = [M.phi(128 * 2, BF16, "hg_msk%d" % i) for i in range(3)]
        rmask, Brm = M.phi(T * 2, BF16, "hg_rmask")
        onesT, Bon = M.phi(T * 2, BF16, "hg_ones")
        self.G(lambda e: e.memset(rmask, 1.0), w=[Brm])
        self.G(lambda e: e.memset(rmask.rearrange("p (c j) -> p c j", j=64)[:, :, 0:1], 0.0), w=[Brm])
        self.G(lambda e: e.memset(onesT, 1.0), w=[Bon])
        Sfin, BSfin = M.plo(256 * 4, F32, "hg_Sfin")
        self.hg_Sfin, self.Bhg_Sfin = Sfin, BSfin
        bd = self.cb[:, CB_BD64:CB_BD64 + 128]
        ident = self.cb[:, CB_IDENT:CB_IDENT + 128]
        wf_off = M.phi_off(8 * 128 * 2)
        wq_off = M.phi_off(8 * 128 * 2)
        for hh in range(4):
            pr, hp = hh // 2, hh % 2
            wf3, Bwf = self.load_w_at(wf_off, W_in[l, :, C_HF + hh * 128:C_HF + (hh + 1) * 128], 8, 128, "w_hf")
            wq3, Bwq = self.load_w_at(wq_off, W_in[l, :, C_HQ + hh * 128:C_HQ + (hh + 1) * 128], 8, 128, "w_hq")
            ci = hh * 4 + l
            for tt in range(NT):
                ts = slice(tt * TT, (tt + 1) * TT)
                ps, Bps = self.bank()
                self.proj(ps[:, :], Bps, wf3, Bwf, 0, 128, tt)
                self.A(lambda e, ps=ps, ts=ts: e.activation(out=fg[:, ts], in_=ps[:, :], func=AF.Sigmoid), r=[Bps], w=[Bfg])
                self.V(lambda e, ts=ts, ci=ci: e.tensor_scalar(out=fg[:, ts], in0=fg[:, ts], scalar1=self.omlb[:, ci:ci + 1],
                                                               scalar2=self.lb[:, ci:ci + 1], op0=ALU.mult, op1=ALU.add),
                       r=[Bfg, self.Blb], w=[Bfg])
                self.G(lambda e, ts=ts: e.tensor_scalar(out=kT[:, ts], in0=fg[:, ts], scalar1=-1.0, scalar2=1.0,
                                                        op0=ALU.mult, op1=ALU.add), r=[Bfg], w=[BkT])
                ps2, Bps2 = self.bank()
                self.proj(ps2[:, :], Bps2, wq3, Bwq, 0, 128, tt)
                self.V(lambda e, ps2=ps2, ts=ts: e.tensor_copy(out=qT[:, ts], in_=ps2[:, :]), r=[Bps2], w=[BqT])
            for tt in range(NT):
                ts = slice(tt * TT, (tt + 1) * TT)
                self.A(lambda e, ts=ts: e.activation(out=fg[:, ts], in_=fg[:, ts], func=AF.Ln), r=[Bfg, BkT], w=[Bfg])
            self.V(lambda e: e.tensor_tensor_scan(out=bb, data0=rmask, data1=fg, initial=0.0, op0=ALU.mult, op1=ALU.add),
                   r=[Brm, Bfg], w=[Bbb])
            if self.half == 1:
                self.V(lambda e: e.tensor_tensor_scan(out=tmp, data0=onesT, data1=fg, initial=0.0, op0=ALU.mult, op1=ALU.add),
                       r=[Bon, Bfg], w=[Btmp])
                self.A(lambda e: e.activation(out=tmp, in_=tmp, func=AF.Exp), r=[Btmp], w=[Btmp])
                self.V(lambda e, hh=hh: e.tensor_tensor(out=self.hg_qB[:, hh, :], in0=qT, in1=tmp, op=ALU.mult),
                       r=[BqT, Btmp], w=[BqB])
            self.A(lambda e: e.activation(out=tmp, in_=bb, func=AF.Exp), r=[Bbb, BqB], w=[Btmp])
            self.V(lambda e: e.tensor_tensor(out=qtl, in0=qT, in1=tmp, op=ALU.mult), r=[BqT, Btmp], w=[Bqtl])
            self.A(lambda e: e.activation(out=tmp, in_=bb, func=AF.Exp, scale=-1.0), r=[Bbb, Bqtl], w=[Btmp])
            self.V(lambda e: e.tensor_tensor(out=ktl, in0=kT, in1=tmp, op=ALU.mult), r=[BkT, Btmp], w=[Bktl])
            b3 = bb.rearrange("p (c j) -> p c j", j=64)
            self.A(lambda e: e.activation(out=ebl, in_=b3[:, :, 63], func=AF.Exp), r=[Bbb], w=[Bebl])
            self.V(lambda e: e.tensor_tensor(out=khT.rearrange("p (c j) -> p c j", j=64), in0=ktl.rearrange("p (c j) -> p c j", j=64),
                                             in1=ebl.rearrange("p (c o) -> p c o", o=1).broadcast_to([128, 32, 64]), op=ALU.mult),
                   r=[Bktl, Bebl], w=[BkhT])
            if self.stop_at == "hg1b":
                self.stopped = True
                return
            for tb in range(NB):
                psx, Bpt = self.bank()
                pt = psx[:, 0:64].bitcast(BF16)
                self.P.op("tensor", lambda e, pt=pt, tb=tb: e.transpose(out=pt, in_=khT[:, tb * 128:(tb + 1) * 128], identity=ident),
                          r=[BkhT, self.Bcb], w=[Bpt])
                self.cp(khtm3[:, tb, :], pt, [Bpt], [Bkhtm])
            if self.stop_at == "hg1c":
                self.stopped = True
                return
            self.V(lambda e: e.memset(S32, 0.0), w=[BS32])
            self.G(lambda e: e.memset(Sb[0][0], 0.0), w=[Sb[0][1]])
            si = 0
            import os as _os
            SK = set(_os.environ.get("KSKIP", "").split(","))
            for tg in range(NT):
                po, Bpo = self.pb[4 + (tg % 2)], self.Bpb[4 + (tg % 2)]
                pkv, Bpkv = self.pb[6], self.Bpb[6]
                for bi in range(4):
                    tb = tg * 4 + bi
                    for c in range(2):
                        cidx = bi * 2 + c
                        if "kv" in SK:
                            continue
                        self.MM(self.pb[6 + c][:, bi * 64:(bi + 1) * 64], khtm3[c * 64:(c + 1) * 64, tb, :],
                                vtm3[c * 64:(c + 1) * 64, tb, hh * 64:(hh + 1) * 64], True, True,
                                r=[Bkhtm, Bvtm], w=[self.Bpb[6 + c]], inc=(bi == 3))
                def scores(bi_):
                    tb_ = tg * 4 + bi_
                    blk_ = slice(tb_ * 128, (tb_ + 1) * 128)
                    ps_, Bps_ = self.bank()
                    m_, Bm_ = msk[tb_ % 3]
                    self.MM(ps_[:, 0:128], ktl[:, blk_], qtl[:, blk_], True, True, r=[Bktl, Bqtl], w=[Bps_])
                    self.V(lambda e, ps_=ps_, m_=m_: e.tensor_tensor(out=m_, in0=ps_[:, 0:128], in1=bd, op=ALU.mult),
                           r=[Bps_, self.Bcb], w=[Bm_])
                scores(0)
                for bi in range(4):
                    tb = tg * 4 + bi
                    if bi + 1 < 4:
                        scores(bi + 1)
                    m_ap, Bm = msk[tb % 3]
                    o = po[hp * 64:(hp + 1) * 64, bi * 128:(bi + 1) * 128]
                    self.MM(o, vtm3[:, tb, hh * 64:(hh + 1) * 64], m_ap, True, False, r=[Bvtm, Bm], w=[Bpo], inc=False)
                    for c in range(2):
                        gc = tb * 2 + c
                        s_ap, Bs = Sb[si % len(Sb)]
                        oc = po[hp * 64:(hp + 1) * 64, bi * 128 + c * 64:bi * 128 + (c + 1) * 64]
                        self.MM(oc, s_ap, qtl[:, tb * 128 + c * 64:tb * 128 + (c + 1) * 64], False, c == 1,
                                r=[Bs, Bqtl], w=[Bpo], inc=True)
                        self.V(lambda e, gc=gc, bi=bi, c=c: e.scalar_tensor_tensor(
                            out=S32, in0=S32, scalar=ebl[:, gc:gc + 1], in1=self.pb[6 + c][:, bi * 64:(bi + 1) * 64],
                            op0=ALU.mult, op1=ALU.add), r=[BS32, Bebl, self.Bpb[6 + c]], w=[BS32])
                        si += 1
                        s2, Bs2 = Sb[si % len(Sb)]
                        self.V(lambda e, s2=s2: e.tensor_copy(out=s2, in_=S32), r=[BS32], w=[Bs2])
                ts = slice(tg * TT, (tg + 1) * TT)
                if "ev" not in SK:
                    self.cp(self.hg_oloc[hp * 64:(hp + 1) * 64, pr, ts], po[hp * 64:(hp + 1) * 64, :], [Bpo], [Bol])
            self.V(lambda e, hh=hh: e.tensor_copy(out=Sfin[:, hh * 64:(hh + 1) * 64], in_=S32), r=[BS32], w=[BSfin])

    def hgrn2_s2(self, l, M, pred):
        yc, Byc = self.ycat[2], self.Bycat[2]
        units = [(pr, tt) for pr in range(2) for tt in range(NT)]
        o32 = [M.phi(TT * 4, F32, "hg2_o%d" % i) for i in range(len(units))]
        sq = [M.phi(TT * 2, BF16, "hg2_sq%d" % i) for i in range(len(units))]
        rs = [M.phi(TT * 4, F32, "hg2_rs%d" % i) for i in range(len(units))]
        bones = self.cb[:, CB_BONES:CB_BONES + 128]
        if pred:
            Sp, BSp = self.hg_Spred, self.Bhg_Spred
        for i, (pr, tt) in enumerate(units):
            ts = slice(tt * TT, (tt + 1) * TT)
            o_ap, Bo = o32[i]
            if pred:
                ps, Bps = self.bank()
                for hp in range(2):
                    hh = pr * 2 + hp
                    self.MM(ps[hp * 64:(hp + 1) * 64, :], Sp[:, hh * 64:(hh + 1) * 64], self.hg_qB[:, hh, ts], True, True,
                            r=[BSp, self.Bhg_qB], w=[Bps], inc=(hp == 1))
                self.V(lambda e, ps=ps, o_ap=o_ap, pr=pr, ts=ts: e.tensor_tensor(out=o_ap, in0=ps[:, :], in1=self.hg_oloc[:, pr, ts], op=ALU.add),
                       r=[Bps, self.Bhg_oloc], w=[Bo])
            else:
                self.V(lambda e, o_ap=o_ap, pr=pr, ts=ts: e.tensor_copy(out=o_ap, in_=self.hg_oloc[:, pr, ts]),
                       r=[self.Bhg_oloc], w=[Bo])
            sq_ap, Bsq = sq[i]
            self.G(lambda e, o_ap=o_ap, sq_ap=sq_ap: e.tensor_tensor(out=sq_ap, in0=o_ap, in1=o_ap, op=ALU.mult), r=[Bo], w=[Bsq])
        for i, (pr, tt) in enumerate(units):
            sq_ap, Bsq = sq[i]
            rs_ap, Brs = rs[i]
            ps2, Bps2 = self.bank()
            self.MM(ps2[:, :], bones, sq_ap, True, True, r=[Bsq, self.Bcb], w=[Bps2])
            self.rstd_from(rs_ap, Brs, ps2[:, :], Bps2, 1.0 / 64)
        for i, (pr, tt) in enumerate(units):
            ts = slice(tt * TT, (tt + 1) * TT)
            o_ap, Bo = o32[i]
            rs_ap, Brs = rs[i]
            gcol = PP_HGN + l * 2 + pr
            self.V(lambda e, o_ap=o_ap, rs_ap=rs_ap, gcol=gcol: e.scalar_tensor_tensor(
                out=o_ap, in0=o_ap, scalar=self.pp[:, gcol:gcol + 1], in1=rs_ap, op0=ALU.mult, op1=ALU.mult),
                r=[Bo, Brs, self.Bpp], w=[Bo])
            self.G(lambda e, o_ap=o_ap, pr=pr, ts=ts: e.tensor_tensor(out=yc[:, pr, ts], in0=o_ap, in1=self.hg_gate[:, pr, ts], op=ALU.mult),
                   r=[Bo, self.Bhg_gate], w=[Byc])

    def ret_s1(self, l, M):
        W_in, W_sw = self.W["w_in"], self.W["w_in_sw"]
        oloc, Bol = M.plo(2 * T * 2, BF16, "rt_oloc")
        self.rt_oloc, self.Brt_oloc = oloc.rearrange("p (c t) -> p c t", c=2), Bol
        qseg, Bqseg = M.plo(2 * T * 2, BF16, "rt_qseg")
        self.rt_qseg, self.Brt_qseg = qseg.rearrange("p (c t) -> p c t", c=2), Bqseg
        gate, Bgate = M.plo(2 * T * 2, BF16, "rt_gate")
        self.rt_gate, self.Brt_gate = gate.rearrange("p (c t) -> p c t", c=2), Bgate
        Sfin, BSfin = M.plo(256 * 4, F32, "rt_Sfin")
        self.rt_Sfin, self.Brt_Sfin = Sfin, BSfin
        qr, Bqr = M.phi(2 * T * 2, BF16, "rt_qr")
        qr3 = qr.rearrange("p (c t) -> p c t", c=2)
        kr, Bkr = M.phi(2 * T * 2, BF16, "rt_kr")
        kr3 = kr.rearrange("p (c t) -> p c t", c=2)
        qd, Bqd = M.phi(2 * T * 2, BF16, "rt_qd")
        qd3 = qd.rearrange("p (c t) -> p c t", c=2)
        vtm, Bvtm = M.phi(NB * 256 * 2, BF16, "rt_vtm")
        vtm3 = vtm.rearrange("p (b c) -> p b c", b=NB)
        kdtm, Bkdtm = M.phi(NB * 256 * 2, BF16, "rt_kdtm")
        kdtm3 = kdtm.rearrange("p (b c) -> p b c", b=NB)
        t1 = [M.phi(TT * 4, F32, "rt_t1%d" % i) for i in range(2)]
        t2 = [M.phi(TT * 4, F32, "rt_t2%d" % i) for i in range(2)]
        S32, BS32 = M.phi(256 * 4, F32, "rt_S32")
        Sb = [M.phi(256 * 2, BF16, "rt_Sb%d" % i) for i in range(4)]
        msk = [M.phi(128 * 2, BF16, "rt_msk%d" % i) for i in range(8)]
        wg3, Bwg = self.load_w(M, W_in[l, :, C_RG:C_RG + 256], 8, 256, "w_rg")
        wv3, Bwv = self.load_w(M, W_in[l, :, C_RV:C_RV + 256], 8, 256, "w_rv")
        wq3, Bwq = self.load_w(M, W_in[l, :, C_RQ:C_RQ + 256], 8, 256, "w_rq")
        wqs3, Bwqs = self.load_w(M, W_sw[l, :, 32:288], 8, 256, "w_rqs")
        wk3, Bwk = self.load_w(M, W_in[l, :, C_RK:C_RK + 256], 8, 256, "w_rk")
        wks3, Bwks = self.load_w(M, W_sw[l, :, 288:544], 8, 256, "w_rks")
        for cc in range(2):
            for tt in range(NT):
                ts = slice(tt * TT, (tt + 1) * TT)
                ps, Bps = self.bank()
                self.proj(ps[:, :], Bps, wg3, Bwg, cc * 128, 128, tt)
                self.A(lambda e, ps=ps, cc=cc, ts=ts: e.activation(out=self.rt_gate[:, cc, ts], in_=ps[:, :], func=AF.Silu),
                       r=[Bps], w=[Bgate])
        for tb in range(NB):
            ps, Bps = self.bank()
            self.proj_tm(ps[:, 0:256], Bps, wv3, Bwv, 0, 256, tb)
            self.cp(vtm3[:, tb, :], ps[:, 0:256], [Bps], [Bvtm])
        ri = 0
        for (w3, Bw, ws3, Bws, dst3, Bdst) in ((wq3, Bwq, wqs3, Bwqs, qr3, Bqr), (wk3, Bwk, wks3, Bwks, kr3, Bkr)):
            for cc in range(2):
                for tt in range(NT):
                    ts = slice(tt * TT, (tt + 1) * TT)
                    ps, Bps = self.bank()
                    self.proj(ps[:, :], Bps, w3, Bw, cc * 128, 128, tt)
                    ps2, Bps2 = self.bank()
                    self.proj(ps2[:, :], Bps2, ws3, Bws, cc * 128, 128, tt)
                    a1, Ba1 = t1[ri % 2]
                    a2, Ba2 = t2[ri % 2]
                    ri += 1
                    self.V(lambda e, ps=ps, a1=a1, ts=ts: e.tensor_tensor(out=a1, in0=ps[:, :], in1=self.cosR[:, ts], op=ALU.mult),
                           r=[Bps, self.Btab], w=[Ba1])
                    self.V(lambda e, ps2=ps2, a2=a2, ts=ts: e.tensor_tensor(out=a2, in0=ps2[:, :], in1=self.sinR[:, ts], op=ALU.mult),
                           r=[Bps2, self.Btab], w=[Ba2])
                    self.G(lambda e, a1=a1, a2=a2, dst3=dst3, cc=cc, ts=ts: e.tensor_tensor(out=dst3[:, cc, ts], in0=a1, in1=a2, op=ALU.add),
                           r=[Ba1, Ba2], w=[Bdst])
        for pr in range(2):
            qdm = self.cb[:, CB_QD + pr * 128:CB_QD + (pr + 1) * 128]
            sgd = self.cb[:, CB_SEGD + pr * 16:CB_SEGD + (pr + 1) * 16]
            self.G(lambda e, pr=pr, qdm=qdm: e.tensor_tensor(
                out=qd3[:, pr, :].rearrange("p (b j) -> p b j", j=128), in0=qr3[:, pr, :].rearrange("p (b j) -> p b j", j=128),
                in1=qdm.rearrange("p (o j) -> p o j", o=1).broadcast_to([128, NB, 128]), op=ALU.mult),
                r=[Bqr, self.Bcb], w=[Bqd])
            self.G(lambda e, pr=pr, sgd=sgd: e.tensor_tensor(
                out=self.rt_qseg[:, pr, :].rearrange("p (b j) -> p b j", j=128), in0=qd3[:, pr, :].rearrange("p (b j) -> p b j", j=128),
                in1=sgd.rearrange("p (b o) -> p b o", o=1).broadcast_to([128, NB, 128]), op=ALU.mult),
                r=[Bqd, self.Bcb], w=[Bqseg])
        ident = self.cb[:, CB_IDENT:CB_IDENT + 128]
        ti = 0
        for pr in range(2):
            for tb in range(NB):
                psx, Bpt = self.bank()
                pt = psx[:, 0:64].bitcast(BF16)
                self.P.op("tensor", lambda e, pt=pt, tb=tb, pr=pr: e.transpose(out=pt, in_=kr3[:, pr, tb * 128:(tb + 1) * 128], identity=ident),
                          r=[Bkr, self.Bcb], w=[Bpt])
                for hp in range(2):
                    hh = pr * 2 + hp
                    kc = self.cst[:, CS_KDEC + hh:CS_KDEC + hh + 1]
                    if hp == 0:
                        self.V(lambda e, pt=pt, tb=tb, hh=hh, hp=hp, kc=kc: e.tensor_scalar(
                            out=kdtm3[:, tb, hh * 64:(hh + 1) * 64], in0=pt[:, hp * 64:(hp + 1) * 64], scalar1=kc, scalar2=None, op0=ALU.mult),
                            r=[Bpt, self.Bcst], w=[Bkdtm])
                    else:
                        self.A(lambda e, pt=pt, tb=tb, hh=hh, hp=hp, kc=kc: e.activation(
                            out=kdtm3[:, tb, hh * 64:(hh + 1) * 64], in_=pt[:, hp * 64:(hp + 1) * 64], func=AF.Copy, scale=kc),
                            r=[Bpt, self.Bcst], w=[Bkdtm])
        self.V(lambda e: e.memset(S32, 0.0), w=[BS32])
        self.G(lambda e: e.memset(Sb[0][0], 0.0), w=[Sb[0][1]])
        def rscores(tb_):
            blk_ = slice(tb_ * 128, (tb_ + 1) * 128)
            for hh_ in range(4):
                pr_, hp_ = hh_ // 2, hh_ % 2
                rows_ = slice(hp_ * 64, (hp_ + 1) * 64)
                ps_, Bps_ = self.bank()
                self.MM(ps_[:, 0:128], kr3[rows_, pr_, blk_], qr3[rows_, pr_, blk_], True, True, r=[Bkr, Bqr], w=[Bps_])
                m_, Bm_ = msk[(tb_ % 2) * 4 + hh_]
                dm_ = self.cst[:, CS_DM + hh_ * 128:CS_DM + (hh_ + 1) * 128]
                self.V(lambda e, ps_=ps_, m_=m_, dm_=dm_: e.tensor_tensor(out=m_, in0=ps_[:, 0:128], in1=dm_, op=ALU.mult),
                       r=[Bps_, self.Bcst], w=[Bm_])
        rscores(0)
        for tg in range(NT):
            pos_ = [(self.pb[4], self.Bpb[4]), (self.pb[5], self.Bpb[5])]
            for bi in range(4):
                tb = tg * 4 + bi
                blk = slice(tb * 128, (tb + 1) * 128)
                s_ap, Bs = Sb[tb % len(Sb)]
                s2, Bs2 = Sb[(tb + 1) % len(Sb)]
                pkv, Bpkv = self.pb[6 + (tb % 2)], self.Bpb[6 + (tb % 2)]
                if tb + 1 < NB:
                    rscores(tb + 1)
                for hh in range(4):
                    pr, hp = hh // 2, hh % 2
                    rows = slice(hp * 64, (hp + 1) * 64)
                    po, Bpo = pos_[pr]
                    m_ap, Bm = msk[(tb % 2) * 4 + hh]
                    o = po[rows, bi * 128:(bi + 1) * 128]
                    self.MM(o, vtm3[:, tb, hh * 64:(hh + 1) * 64], m_ap, True, False, r=[Bvtm, Bm], w=[Bpo], inc=False)
                    self.MM(o, s_ap[rows, hh * 64:(hh + 1) * 64], qd3[rows, pr, blk], False, True, r=[Bs, Bqd], w=[Bpo])
                    self.MM(pkv[rows, hh * 64:(hh + 1) * 64], kdtm3[:, tb, hh * 64:(hh + 1) * 64], vtm3[:, tb, hh * 64:(hh + 1) * 64],
                            True, True, r=[Bkdtm, Bvtm], w=[Bpkv], inc=(hh == 3))
                for hh in range(4):
                    hp = hh % 2
                    rows = slice(hp * 64, (hp + 1) * 64)
                    g128 = RET_G[hh] ** 128
                    self.V(lambda e, rows=rows, hh=hh, g128=g128, pkv=pkv: e.scalar_tensor_tensor(
                        out=S32[rows, hh * 64:(hh + 1) * 64], in0=S32[rows, hh * 64:(hh + 1) * 64], scalar=g128,
                        in1=pkv[rows, hh * 64:(hh + 1) * 64], op0=ALU.mult, op1=ALU.add), r=[BS32, Bpkv], w=[BS32])
                self.V(lambda e, s2=s2: e.tensor_copy(out=s2, in_=S32), r=[BS32], w=[Bs2])
            ts = slice(tg * TT, (tg + 1) * TT)
            for pr in range(2):
                po, Bpo = pos_[pr]
                self.cp(self.rt_oloc[:, pr, ts], po[:, :], [Bpo], [Bol])
        self.V(lambda e: e.tensor_copy(out=Sfin, in_=S32), r=[BS32], w=[BSfin])

    def ret_s2(self, l, M, pred):
        yd, Byd = self.ycat[3], self.Bycat[3]
        units = [(pr, tt) for pr in range(2) for tt in range(NT)]
        n = len(units)
        o32 = [M.phi(TT * 4, F32, "rt2_o%d" % i) for i in range(n)]
        ob = [M.phi(TT * 2, BF16, "rt2_ob%d" % i) for i in range(n)]
        sq = [M.phi(TT * 2, BF16, "rt2_sq%d" % i) for i in range(n)]
        rs = [M.phi(TT * 4, F32, "rt2_rs%d" % i) for i in range(n)]
        bones = self.cb[:, CB_BONES:CB_BONES + 128]
        if pred:
            Sp, BSp = self.rt_Spred, self.Brt_Spred
        for i, (pr, tt) in enumerate(units):
            ts = slice(tt * TT, (tt + 1) * TT)
            o_ap, Bo = o32[i]
            ob_ap, Bob = ob[i]
            if not pred:
                self.V(lambda e, o_ap=o_ap, pr=pr, ts=ts: e.tensor_copy(out=o_ap, in_=self.rt_oloc[:, pr, ts]),
                       r=[self.Brt_oloc], w=[Bo])
            else:
                for hp in range(2):
                    hh = pr * 2 + hp
                    rows = slice(hp * 64, (hp + 1) * 64)
                    ps, Bps = self.bank()
                    self.MM(ps[rows, :], Sp[rows, hh * 64:(hh + 1) * 64], self.rt_qseg[rows, pr, ts], True, True,
                            r=[BSp, self.Brt_qseg], w=[Bps])
                    self.V(lambda e, ps=ps, o_ap=o_ap, pr=pr, ts=ts, rows=rows: e.tensor_tensor(
                        out=o_ap[rows, :], in0=ps[rows, :], in1=self.rt_oloc[rows, pr, ts], op=ALU.add),
                        r=[Bps, self.Brt_oloc], w=[Bo])
            self.G(lambda e, o_ap=o_ap, ob_ap=ob_ap: e.tensor_copy(out=ob_ap, in_=o_ap), r=[Bo], w=[Bob])
        for i, (pr, tt) in enumerate(units):
            o_ap, Bo = o32[i]
            ob_ap, Bob = ob[i]
            sq_ap, Bsq = sq[i]
            pm, Bpm = self.bank()
            self.MM(pm[:, :], bones, ob_ap, True, True, r=[Bob, self.Bcb], w=[Bpm])
            self.V(lambda e, pm=pm, o_ap=o_ap: e.scalar_tensor_tensor(out=o_ap, in0=pm[:, :], scalar=-1.0 / 64, in1=o_ap,
                                                                      op0=ALU.mult, op1=ALU.add), r=[Bpm, Bo], w=[Bo])
            self.G(lambda e, o_ap=o_ap, sq_ap=sq_ap: e.tensor_tensor(out=sq_ap, in0=o_ap, in1=o_ap, op=ALU.mult), r=[Bo], w=[Bsq])
        for i, (pr, tt) in enumerate(units):
            sq_ap, Bsq = sq[i]
            rs_ap, Brs = rs[i]
            ps2, Bps2 = self.bank()
            self.MM(ps2[:, :], bones, sq_ap, True, True, r=[Bsq, self.Bcb], w=[Bps2])
            self.rstd_from(rs_ap, Brs, ps2[:, :], Bps2, 1.0 / 64)
        for i, (pr, tt) in enumerate(units):
            ts = slice(tt * TT, (tt + 1) * TT)
            o_ap, Bo = o32[i]
            rs_ap, Brs = rs[i]
            gcol = PP_RTN + l * 2 + pr
            self.V(lambda e, o_ap=o_ap, rs_ap=rs_ap, gcol=gcol: e.scalar_tensor_tensor(
                out=o_ap, in0=o_ap, scalar=self.pp[:, gcol:gcol + 1], in1=rs_ap, op0=ALU.mult, op1=ALU.mult),
                r=[Bo, Brs, self.Bpp], w=[Bo])
            self.G(lambda e, o_ap=o_ap, pr=pr, ts=ts: e.tensor_tensor(out=yd[:, pr, ts], in0=o_ap, in1=self.rt_gate[:, pr, ts], op=ALU.mult),
                   r=[Bo, self.Brt_gate], w=[Byd])

    def mla_s1(self, l, M):
        W_in, W_sw = self.W["w_in"], self.W["w_in_sw"]
        qT, BqT = M.plo(4 * T * 2, BF16, "ml_qT")
        self.ml_qT, self.Bml_qT = qT.rearrange("p (h t) -> p h t", h=4), BqT
        lat, Blat = M.plo(2 * T * 2, BF16, "ml_lat")
        self.ml_lat, self.Bml_lat = lat, Blat
        kr, Bkr = M.plo(2 * T * 2, BF16, "ml_kr")
        self.ml_kr, self.Bml_kr = kr, Bkr
        self.G(lambda e: e.memset(kr, 0.0), w=[Bkr])
        wcq, Bwcq = self.load_w(M, W_in[l, :, C_CQ:C_CQ + 256], 8, 256, "w_cq")
        wckv, Bwckv = self.load_w(M, W_in[l, :, C_CKV:C_CKV + 128], 8, 128, "w_ckv")
        wkpe, Bwkpe = self.load_w(M, W_in[l, :, C_KPE:C_KPE + 32], 8, 32, "w_kpe")
        wkpes, Bwkpes = self.load_w(M, W_sw[l, :, 0:32], 8, 32, "w_kpes")
        wuq, Bwuq = self.load_w(M, self.W["w_uq"][l, :, :], 2, 384, "w_uq")
        wuqs, Bwuqs = self.load_w(M, self.W["w_uq_sw"][l, :, :], 2, 128, "w_uqs")
        cq = [M.phi(2 * TT * 4, F32, "ml_cq%d" % i) for i in range(2)]
        sq = [M.phi(2 * TT * 2, BF16, "ml_sq%d" % i) for i in range(2)]
        rs = [M.phi(TT * 4, F32, "ml_rs%d" % i) for i in range(2)]
        cqn = [M.phi(2 * TT * 2, BF16, "ml_cqn%d" % i) for i in range(2)]
        t1 = [M.phi(TT * 4, F32, "ml_t1%d" % i) for i in range(2)]
        t2 = [M.phi(TT * 4, F32, "ml_t2%d" % i) for i in range(2)]
        ones = self.cb[:, CB_ONES:CB_ONES + 128]
        ri = 0
        for tt in range(NT):
            ts = slice(tt * TT, (tt + 1) * TT)
            cq_ap, Bcq = cq[tt % 2]
            cq3 = cq_ap.rearrange("p (c t) -> p c t", c=2)
            sq_ap, Bsq = sq[tt % 2]
            sq3 = sq_ap.rearrange("p (c t) -> p c t", c=2)
            rs_ap, Brs = rs[tt % 2]
            cqn_ap, Bcqn = cqn[tt % 2]
            cqn3 = cqn_ap.rearrange("p (c t) -> p c t", c=2)
            for cc in range(2):
                ps, Bps = self.bank()
                self.proj(ps[:, :], Bps, wcq, Bwcq, cc * 128, 128, tt)
                self.cp(cq3[:, cc, :], ps[:, :], [Bps], [Bcq])
            self.G(lambda e, sq_ap=sq_ap, cq_ap=cq_ap: e.tensor_tensor(out=sq_ap, in0=cq_ap, in1=cq_ap, op=ALU.mult), r=[Bcq], w=[Bsq])
            ps, Bps = self.bank()
            for cc in range(2):
                self.MM(ps[:, :], ones, sq3[:, cc, :], cc == 0, cc == 1, r=[Bsq, self.Bcb], w=[Bps], inc=(cc == 1))
            self.rstd_from(rs_ap, Brs, ps[:, :], Bps, 1.0 / 256)
            for cc in range(2):
                gcol = PP_QN + l * 2 + cc
                self.V(lambda e, cc=cc, gcol=gcol, cqn3=cqn3, cq3=cq3, rs_ap=rs_ap: e.scalar_tensor_tensor(
                    out=cqn3[:, cc, :], in0=cq3[:, cc, :], scalar=self.pp[:, gcol:gcol + 1], in1=rs_ap, op0=ALU.mult, op1=ALU.mult),
                    r=[Bcq, Brs, self.Bpp], w=[Bcqn])
            for hh in range(4):
                psA, BpsA = self.bank()
                for kc in range(2):
                    self.MM(psA[0:96, :], wuq[:, kc, hh * 96:(hh + 1) * 96], cqn3[:, kc, :], kc == 0, kc == 1,
                            r=[Bwuq, Bcqn], w=[BpsA], inc=(kc == 1))
                psB, BpsB = self.bank()
                for kc in range(2):
                    self.MM(psB[64:96, :], wuqs[:, kc, hh * 32:(hh + 1) * 32], cqn3[:, kc, :], kc == 0, kc == 1,
                            r=[Bwuqs, Bcqn], w=[BpsB], inc=(kc == 1))
                self.cp(self.ml_qT[0:64, hh, ts], psA[0:64, :], [BpsA], [BqT])
                a1, Ba1 = t1[ri % 2]
                a2, Ba2 = t2[ri % 2]
                ri += 1
                self.V(lambda e, psA=psA, a1=a1, ts=ts: e.tensor_tensor(out=a1[64:96, :], in0=psA[64:96, :], in1=self.cosM[64:96, ts], op=ALU.mult),
                       r=[BpsA, self.Btab], w=[Ba1])
                self.V(lambda e, psB=psB, a2=a2, ts=ts: e.tensor_tensor(out=a2[64:96, :], in0=psB[64:96, :], in1=self.sinM[64:96, ts], op=ALU.mult),
                       r=[BpsB, self.Btab], w=[Ba2])
                self.V(lambda e, a1=a1, a2=a2, hh=hh, ts=ts: e.tensor_tensor(out=self.ml_qT[64:96, hh, ts], in0=a1[64:96, :], in1=a2[64:96, :], op=ALU.add),
                       r=[Ba1, Ba2], w=[BqT])
            ck_ap, Bck = cq[(tt + 1) % 2]
            ck = ck_ap[:, 0:TT]
            ps, Bps = self.bank()
            self.proj(ps[:, :], Bps, wckv, Bwckv, 0, 128, tt)
            self.cp(ck, ps[:, :], [Bps], [Bck])
            sk_ap, Bsk = sq[(tt + 1) % 2]
            sk = sk_ap[:, 0:TT]
            self.G(lambda e, sk=sk, ck=ck: e.tensor_tensor(out=sk, in0=ck, in1=ck, op=ALU.mult), r=[Bck], w=[Bsk])
            ps2, Bps2 = self.bank()
            self.MM(ps2[:, :], ones, sk, True, True, r=[Bsk, self.Bcb], w=[Bps2])
            rk_ap, Brk = rs[(tt + 1) % 2]
            self.rstd_from(rk_ap, Brk, ps2[:, :], Bps2, 1.0 / 128)
            gcol = PP_KVN + l
            self.V(lambda e, ck=ck, rk_ap=rk_ap, gcol=gcol, tt=tt: e.scalar_tensor_tensor(
                out=lat[:, T + tt * TT:T + (tt + 1) * TT], in0=ck, scalar=self.pp[:, gcol:gcol + 1], in1=rk_ap, op0=ALU.mult, op1=ALU.mult),
                r=[Bck, Brk, self.Bpp], w=[Blat])
            psA, BpsA = self.bank()
            self.proj(psA[0:32, :], BpsA, wkpe, Bwkpe, 0, 32, tt)
            psB, BpsB = self.bank()
            self.proj(psB[0:32, :], BpsB, wkpes, Bwkpes, 0, 32, tt)
            a1, Ba1 = t1[ri % 2]
            a2, Ba2 = t2[ri % 2]
            ri += 1
            self.V(lambda e, psA=psA, a1=a1, ts=ts: e.tensor_tensor(out=a1[0:32, :], in0=psA[0:32, :], in1=self.cosM[0:32, ts], op=ALU.mult),
                   r=[BpsA, self.Btab], w=[Ba1])
            self.V(lambda e, psB=psB, a2=a2, ts=ts: e.tensor_tensor(out=a2[0:32, :], in0=psB[0:32, :], in1=self.sinM[0:32, ts], op=ALU.mult),
                   r=[BpsB, self.Btab], w=[Ba2])
            self.V(lambda e, a1=a1, a2=a2, tt=tt: e.tensor_tensor(out=kr[0:32, T + tt * TT:T + (tt + 1) * TT], in0=a1[0:32, :], in1=a2[0:32, :], op=ALU.add),
                   r=[Ba1, Ba2], w=[Bkr])

    def cc_gather(self, xs, Bxs, xr, Bxr):
        P = self.P
        groups = [[0, 1], [2, 3], [4, 5], [6, 7]]
        key = ("cc",)
        if key not in P.sems:
            P._mksem(key, 16)
        waits = P._deps("gpsimd", [Bxs], [Bxr])
        seq = P.cnt[key] + 1
        P.cnt[key] = seq
        P._mark(key, seq, [Bxs], [Bxr])
        P.ops["gpsimd"].append((lambda e: e.collective_compute("AllGather", ALU.bypass, replica_groups=groups,
                                                               ins=[xs[:, :]], outs=[xr[:, :]]), waits, key))

    def mla_s2(self, l, M, pred):
        ya, Bya = self.ycat[0], self.Bycat[0]
        CT = 2 * T
        wukv, Bwukv = self.load_w(M, self.W["w_ukv"][l, :, :], 1, 512, "w_ukv")
        KT = [M.phi(CT * 2, BF16, "ml_KT%d" % i) for i in range(2)]
        VH = [M.phi(32 * 128 * 2, BF16, "ml_VH%d" % i) for i in range(2)]
        PT = [M.phi(TT * 2, BF16, "ml_PT%d" % i) for i in range(5)]
        RC = [M.phi(TT * 4, F32, "ml_RC%d" % i) for i in range(2)]
        for i in range(2):
            v3 = VH[i][0].rearrange("p (k c) -> p k c", k=32)
            self.G(lambda e, v3=v3: e.memset(v3[:, :, 64:128], 1.0), w=[VH[i][1]])
        caus = self.cb[:, CB_CAUS:CB_CAUS + 128]
        SC = 96.0 ** -0.5
        lat, Blat, kr, Bkr = self.ml_lat, self.Bml_lat, self.ml_kr, self.Bml_kr
        pi = 0
        qi = 0
        for hh in range(4):
            pr, hp = hh // 2, hh % 2
            kt, Bkt = KT[hh % 2]
            vh, Bvh = VH[hh % 2]
            vh3 = vh.rearrange("p (k c) -> p k c", k=32)
            for ct in range(0 if pred else 4, CT // TT):
                ps, Bps = self.bank()
                self.MM(ps[0:64, :], wukv[:, 0, hh * 128:hh * 128 + 64], lat[:, ct * TT:(ct + 1) * TT], True, True,
                        r=[Bwukv, Blat], w=[Bps])
                self.cp(kt[0:64, ct * TT:(ct + 1) * TT], ps[0:64, :], [Bps], [Bkt])
            k0 = 0 if pred else T
            self.V(lambda e, kt=kt, k0=k0: e.tensor_copy(out=kt[64:96, k0:], in_=kr[0:32, k0:]), r=[Bkr], w=[Bkt])
            for kg in range(0 if pred else 2, 4):
                ps, Bps = self.bank()
                for j in range(8):
                    kti = kg * 8 + j
                    self.MM(ps[:, j * 64:(j + 1) * 64], lat[:, kti * 128:(kti + 1) * 128], wukv[:, 0, hh * 128 + 64:hh * 128 + 128],
                            True, True, r=[Bwukv, Blat], w=[Bps], inc=(j == 7))
                self.cp(vh3[:, kg * 8:(kg + 1) * 8, 0:64], ps[:, :].rearrange("p (k c) -> p k c", k=8), [Bps], [Bvh])
            for Q in range(NT):
                po, Bpo = self.pb[4 + (qi % 2)], self.Bpb[4 + (qi % 2)]
                qi += 1
                keys = [(k_, 0, False, False) for k_ in range(16)] if pred else []
                for j in range(4 * Q + 4):
                    keys.append((16 + j, max(0, j - 4 * Q) * 128, False, j >= 4 * Q))
                LA = 2
                pend = []

                def emit_pv(item, first, last):
                    kti_, c0_, pt_, Bpt_ = item
                    self.MM(po[:, c0_:TT], vh3[:, kti_, :], pt_[:, c0_:TT], first, last, r=[Bvh, Bpt_], w=[Bpo])
                npv = 0
                for idx, (kti, c0, usebias, diag) in enumerate(keys):
                    ps, Bps = self.bank()
                    self.MM(ps[:, c0:TT], kt[0:96, kti * 128:(kti + 1) * 128], self.ml_qT[0:96, hh, Q * TT + c0:(Q + 1) * TT], True, True,
                            r=[Bkt, self.Bml_qT], w=[Bps])
                    pt, Bpt = PT[pi % len(PT)]
                    pi += 1
                    self.A(lambda e, ps=ps, pt=pt, c0=c0: e.activation(out=pt[:, c0:TT], in_=ps[:, c0:TT], func=AF.Exp, scale=SC),
                           r=[Bps], w=[Bpt])
                    if diag:
                        self.G(lambda e, pt=pt, c0=c0: e.tensor_tensor(out=pt[:, c0:c0 + 128], in0=pt[:, c0:c0 + 128], in1=caus, op=ALU.mult),
                               r=[Bpt, self.Bcb], w=[Bpt])
                    pend.append((kti, c0, pt, Bpt))
                    if len(pend) > LA:
                        emit_pv(pend.pop(0), npv == 0, False)
                        npv += 1
                while pend:
                    emit_pv(pend.pop(0), npv == 0, len(pend) == 0)
                    npv += 1
                rc, Brc = RC[qi % 2]
                self.V(lambda e, rc=rc, po=po: e.reciprocal(out=rc[64:128, :], in_=po[64:128, :]), r=[Bpo], w=[Brc])
                self.V(lambda e, rc=rc, po=po, hp=hp, pr=pr, Q=Q: e.tensor_tensor(
                    out=ya[hp * 64:(hp + 1) * 64, pr, Q * TT:(Q + 1) * TT], in0=po[0:64, :], in1=rc[64:128, :], op=ALU.mult),
                    r=[Bpo, Brc], w=[Bya])

    def wout(self, l, wo, Bwo):
        P = self.P
        self.h_ap, self.hb = self.arena.at(self.H0, self.H_BYTES, F32, "h")
        self.h = self.h_ap.rearrange("p (c t) -> p c t", c=8)
        self.Bh = [Buf("h%d" % i) for i in range(NT)]
        for b in self.Bh:
            merge_into(b, self.hb)
        for tt in range(NT):
            ts = slice(tt * TT, (tt + 1) * TT)
            P.dma("sync", lambda e, ts=ts: e.dma_start(out=self.h[:, :, ts], in_=self.hsp[:, ts].rearrange("(c p) t -> p c t", p=128)),
                  r=[self.Bhsp[tt]], w=[self.Bh[tt]])
        for tt in range(NT):
            ts = slice(tt * TT, (tt + 1) * TT)
            for dc in range(8):
                ps, Bps = self.bank()
                for kc in range(8):
                    self.MM(ps[:, :], wo[:, kc, dc * 128:(dc + 1) * 128], self.ycat[kc // 2][:, kc % 2, ts], kc == 0, kc == 7,
                            r=[Bwo, self.Bycat[kc // 2]], w=[Bps], inc=(kc == 7))
                self.V(lambda e, ps=ps, dc=dc, ts=ts: e.tensor_tensor(out=self.h[:, dc, ts], in0=ps[:, :], in1=self.h[:, dc, ts], op=ALU.add),
                       r=[Bps, self.Bh[tt]], w=[self.Bh[tt]])

    def ple(self, l, pT, c0):
        P = self.P
        self.norm(PP_PLE + l * 8, 1.0 / D)
        stk = self.stk
        stk.reset_hi()
        wpg, Bwpg = self.load_w(stk, self.W["ple_w_gate"][l, :, :], 8, D, "w_pg")
        wpp, Bwpp = self.load_w(stk, self.W["ple_w_proj"][l, :, :], 2, D, "w_pp")
        pt = [stk.phi(2 * TT * 2, BF16, "ple_pT%d" % i) for i in range(2)]
        gt = [stk.phi(TT * 4, F32, "ple_g%d" % i) for i in range(2)]
        tm = [stk.phi(TT * 4, F32, "ple_t%d" % i) for i in range(2)]
        i = 0
        for tt in range(NT):
            ts = slice(tt * TT, (tt + 1) * TT)
            p_ap, Bp = pt[tt % 2]
            p3 = p_ap.rearrange("p (c t) -> p c t", c=2)
            P.dma("gpsimd", lambda e, p3=p3, ts=ts: e.dma_start(out=p3, in_=pT[l, :, c0 + ts.start:c0 + ts.stop].rearrange("(c p) t -> p c t", p=128)), w=[Bp])
            for dc in range(8):
                psg, Bpsg = self.bank()
                for kc in range(8):
                    self.MM(psg[:, :], wpg[:, kc, dc * 128:(dc + 1) * 128], self.xn[:, kc, ts], kc == 0, kc == 7,
                            r=[Bwpg, self.Bxn[tt]], w=[Bpsg], inc=(kc == 7))
                g_ap, Bg = gt[i % 2]
                t_ap, Bt = tm[i % 2]
                i += 1
                self.A(lambda e, psg=psg, g_ap=g_ap: e.activation(out=g_ap, in_=psg[:, :], func=AF.Sigmoid), r=[Bpsg], w=[Bg])
                psp, Bpsp = self.bank()
                for kc in range(2):
                    self.MM(psp[:, :], wpp[:, kc, dc * 128:(dc + 1) * 128], p3[:, kc, :], kc == 0, kc == 1,
                            r=[Bwpp, Bp], w=[Bpsp], inc=(kc == 1))
                self.V(lambda e, psp=psp, g_ap=g_ap, t_ap=t_ap: e.tensor_tensor(out=t_ap, in0=psp[:, :], in1=g_ap, op=ALU.mult),
                       r=[Bpsp, Bg], w=[Bt])
                self.G(lambda e, t_ap=t_ap, dc=dc, ts=ts: e.tensor_tensor(out=self.h[:, dc, ts], in0=self.h[:, dc, ts], in1=t_ap, op=ALU.add),
                       r=[Bt, self.Bh[tt]], w=[self.Bh[tt]])

    def final_out(self, outT, c0):
        stk = self.stk
        stk.reset_hi()
        ones = self.cb[:, CB_ONES:CB_ONES + 128]
        sq, Bsq = stk.phi(8 * TT * 2, BF16, "sq")
        sq3 = sq.rearrange("p (c t) -> p c t", c=8)
        rs, Brs = stk.phi(TT * 4, F32, "rstd")
        ob = [stk.phi(TT * 4, F32, "ob%d" % i) for i in range(4)]
        oi = 0
        for tt in range(NT):
            ts = slice(tt * TT, (tt + 1) * TT)
            bank = 5 + (tt % 2)
            ps, Bps = self.pb[bank], self.Bpb[bank]
            for ch in range(8):
                if ch % 2 == 0:
                    self.G(lambda e, ch=ch, ts=ts: e.tensor_tensor(out=sq3[:, ch, :], in0=self.h[:, ch, ts],
                                                                  in1=self.h[:, ch, ts], op=ALU.mult),
                           r=[self.Bh[tt]], w=[Bsq])
                else:
                    self.A(lambda e, ch=ch, ts=ts: e.activation(out=sq3[:, ch, :], in_=self.h[:, ch, ts], func=AF.Square),
                           r=[self.Bh[tt]], w=[Bsq])
            for ch in range(8):
                self.MM(ps[:, :], ones, sq3[:, ch, :], ch == 0, ch == 7, r=[Bsq, self.Bcb], w=[Bps], inc=(ch == 7))
            epsb = self.cst[:, CS_EPS:CS_EPS + 1]
            self.A(lambda e, ps=ps: e.activation(out=rs, in_=ps[:, :], func=AF.Ln, bias=epsb, scale=1.0 / D),
                   r=[Bps, self.Bcst], w=[Brs])
            self.A(lambda e: e.activation(out=rs, in_=rs, func=AF.Exp, scale=-0.5), r=[Brs], w=[Brs])
            for ch in range(8):
                o_ap, Bo = ob[oi % 4]
                oi += 1
                self.V(lambda e, ch=ch, ts=ts, o_ap=o_ap: e.scalar_tensor_tensor(
                    out=o_ap, in0=self.h[:, ch, ts], scalar=self.pp[:, PP_FINAL + ch:PP_FINAL + ch + 1],
                    in1=rs, op0=ALU.mult, op1=ALU.mult), r=[self.Bh[tt], Brs, self.Bpp], w=[Bo])
                self.P.dma("sync", lambda e, ch=ch, ts=ts, o_ap=o_ap: e.dma_start(
                    out=outT[ch * 128:(ch + 1) * 128, c0 + ts.start:c0 + ts.stop], in_=o_ap), r=[Bo], w=[self.Bout])


PP_FFN1 = 0
PP_MIX = PP_FFN1 + L * 8
PP_FFN2 = PP_MIX + L * 8
PP_PLE = PP_FFN2 + L * 8
PP_FINAL = PP_PLE + L * 8
PP_QN = PP_FINAL + 8
PP_KVN = PP_QN + L * 2
PP_HGN = PP_KVN + L
PP_RTN = PP_HGN + L * 2
PP_LBL = PP_RTN + L * 2
PP_FLAG = PP_LBL + 4 * L
PPW = PP_FLAG + 1

CB_ONES = 0
CB_IDENT = 128
CB_BONES = 256
CB_CAUS = 384
CB_BD64 = 512
CB_QD = 640
CB_SEGD = 896
CSTBW = 928
CS_EPS = 928
CS_INVM = 929
CS_INVR = 930
CS_SGNM = 931
CS_SGNR = 932
CS_KDEC = 933
CS_DM = 937
CSTW = CS_DM + 512
RET_G = [1.0 - 2.0 ** (-(5.0 + h)) for h in range(4)]


def _consts():
    c = np.zeros((128, CSTW), np.float64)
    c[:, CB_ONES:CB_ONES + 128] = 1.0
    c[:, CB_IDENT:CB_IDENT + 128] = np.eye(128)
    bo = np.zeros((128, 128))
    bo[:64, :64] = 1.0
    bo[64:, 64:] = 1.0
    c[:, CB_BONES:CB_BONES + 128] = bo
    s = np.arange(128)[:, None]
    t = np.arange(128)[None, :]
    caus = (t >= s).astype(np.float64)
    c[:, CB_CAUS:CB_CAUS + 128] = caus
    c[:, CB_BD64:CB_BD64 + 128] = caus * bo
    p = np.arange(128)
    for pr in range(2):
        for hp in range(2):
            g = RET_G[pr * 2 + hp]
            c[hp * 64:(hp + 1) * 64, CB_QD + pr * 128:CB_QD + (pr + 1) * 128] = (g ** (np.arange(128) + 1.0) / 8.0)[None, :]
            c[hp * 64:(hp + 1) * 64, CB_SEGD + pr * 16:CB_SEGD + (pr + 1) * 16] = (g ** (128.0 * np.arange(16)))[None, :]
    c[:, CS_EPS] = EPS
    c[:, CS_INVM] = (np.float32(10000.0) ** (-(np.arange(0, 32, 2, dtype=np.float32)) / np.float32(32)))[p % 16]
    c[:, CS_INVR] = (np.float32(10000.0) ** (-(np.arange(0, 64, 2, dtype=np.float32)) / np.float32(64)))[p % 32]
    c[:, CS_SGNM] = np.where((p % 32) < 16, -1.0, 1.0)
    c[:, CS_SGNR] = np.where((p % 64) < 32, -1.0, 1.0)
    for h in range(4):
        g = RET_G[h]
        c[:, CS_KDEC + h] = g ** (127.0 - p)
        c[:, CS_DM + h * 128:CS_DM + (h + 1) * 128] = np.where(t >= s, g ** np.maximum(t - s, 0) / 8.0, 0.0)
    return c.astype(np.float32)


def _pack_pp(inp, has_pred):
    pp = np.zeros((128, PPW), np.float32)

    def fm(v):
        return np.ascontiguousarray(np.asarray(v, np.float32).reshape(-1, 128).T)
    for l in range(L):
        pp[:, PP_FFN1 + l * 8:PP_FFN1 + l * 8 + 8] = fm(inp["ffn1_norm"][l])
        pp[:, PP_MIX + l * 8:PP_MIX + l * 8 + 8] = fm(inp["mix_norm"][l])
        pp[:, PP_FFN2 + l * 8:PP_FFN2 + l * 8 + 8] = fm(inp["ffn2_norm"][l])
        pp[:, PP_PLE + l * 8:PP_PLE + l * 8 + 8] = fm(inp["ple_norm"][l])
        pp[:, PP_QN + l * 2:PP_QN + l * 2 + 2] = fm(inp["mla_q_norm"][l])
        pp[:, PP_KVN + l:PP_KVN + l + 1] = fm(inp["mla_kv_norm"][l])
        pp[:, PP_HGN + l * 2:PP_HGN + l * 2 + 2] = fm(inp["hg_norm"][l])
        pp[:, PP_RTN + l * 2:PP_RTN + l * 2 + 2] = fm(inp["ret_norm"][l])
        for h in range(4):
            pp[:, PP_LBL + h * L + l] = np.asarray(inp["hg_lb_logits"][l], np.float32)[h * 128:(h + 1) * 128]
    pp[:, PP_FINAL:PP_FINAL + 8] = fm(inp["final_norm"])
    pp[:, PP_FLAG] = 1.0 if has_pred else 0.0
    return pp


def _swap_half(w, c0, width, hd):
    blk = np.asarray(w)[..., c0:c0 + width]
    sh = blk.shape[:-1]
    b = blk.reshape(sh + (width // hd, 2, hd // 2))
    return np.ascontiguousarray(b[..., ::-1, :].reshape(sh + (width,)))


def _prep_shared(inp):
    f = lambda k: np.ascontiguousarray(np.asarray(inp[k], np.float32))
    sh = {}
    for n in ("ffn1_w_gate", "ffn1_w_up", "ffn2_w_gate", "ffn2_w_up", "ffn1_w_down", "ffn2_w_down", "w_in",
              "w_out", "ple_w_proj", "ple_w_gate", "sg_ln"):
        sh[n] = f(n)
    w_in = sh["w_in"]
    sh["w_in_sw"] = np.ascontiguousarray(np.concatenate(
        [_swap_half(w_in, C_KPE, 32, 32), _swap_half(w_in, C_RQ, 256, 64), _swap_half(w_in, C_RK, 256, 64)], axis=-1))
    uq = f("mla_w_uq").reshape(L, 256, 4, 96)
    sh["w_uq"] = np.ascontiguousarray(uq.reshape(L, 256, 384))
    sh["w_uq_sw"] = np.ascontiguousarray(_swap_half(uq[..., 64:96].reshape(L, 256, 128), 0, 128, 32))
    sh["w_ukv"] = f("mla_w_ukv")
    sh["sg_wT"] = np.ascontiguousarray(f("sg_w_s").transpose(0, 1, 3, 2))
    sh["sg_b"] = f("sg_b_s")
    sh["cst"] = _consts()
    return sh


def _core_inputs(inp, sh, c):
    b = c
    m = dict(sh)
    m["xT"] = np.ascontiguousarray(np.asarray(inp["x"], np.float32)[b].T)
    m["pT"] = np.ascontiguousarray(np.asarray(inp["p"], np.float32)[:, b].transpose(0, 2, 1))
    m["pos"] = np.ascontiguousarray(np.asarray(inp["positions"], np.int32)[b][None, :])
    m["pp"] = _pack_pp(inp, False)
    return m


REAL_CORES = (0, 1, 4, 5)


def run(inputs, n_layers=L, dbg=None, cores=4, use_cc=False, stages=3, stop_at=None, halves=(0, 1), spread=False):
    bld = Builder(n_layers=n_layers, dbg=dbg, use_cc=use_cc, stages=stages, stop_at=stop_at, halves=halves)
    nc = bld.build()
    sh = _prep_shared(inputs)
    if not spread:
        in_maps = []
        for c in range(cores):
            m = _core_inputs(inputs, sh, c)
            in_maps.append({k: m[k] for k in bld.dram})
        res = run_bass_kernel_spmd(nc, in_maps, core_ids=list(range(cores)))
        return res.results
    real = {}
    for b, c in enumerate(REAL_CORES):
        m = _core_inputs(inputs, sh, b)
        real[c] = {k: m[k] for k in bld.dram}
    zero = {k: np.zeros_like(v) for k, v in real[REAL_CORES[0]].items()}
    in_maps = [real.get(c, zero) for c in range(8)]
    res = run_bass_kernel_spmd(nc, in_maps, core_ids=list(range(8)))
    return [res.results[c] for c in REAL_CORES]


def kernel(**inputs):
    res = run(inputs, spread=True)
    out = np.empty((4, 2 * T, D), np.float32)
    for b in range(4):
        out[b] = res[b]["outT"].T
    return out
```
